# Optimizing a Trainium2 kernel written in Bass

```python
import math
import jax
import jax.numpy as jnp
from jax import lax
import numpy as np

D_MODEL = 2048
BATCH = 8
SEQ = 2048
DEPTH = 2

GRID_W = 64
CTX_LEN = 256
HEAD_DIM = 64
ROPE_THETA = 10000.0
NORM_EPS = 1e-6
N_MIXERS = 4
GROUP_W = D_MODEL // N_MIXERS

GLA_DV = 64
GLA_DK = GLA_DV // 2
GLA_HEADS = GROUP_W // GLA_DV
GLA_GATE_RANK = 16
GLA_GATE_TEMP = 16.0
GLA_CHUNK = 16
SWA_HEADS = GROUP_W // HEAD_DIM
SWA_KV_HEADS = SWA_HEADS // 4
SWA_WINDOW = 128
SWA_BLOCK = 128
HYENA_CH = GROUP_W
HYENA_ORDER = 2
HYENA_BANDS = 16
HYENA_EMB = 1 + 2 * HYENA_BANDS
HYENA_HIDDEN = 64
HYENA_TARGET = 1e-2
HYENA_MIN_DECAY = math.log(1.0 / HYENA_TARGET) / 1.5
HYENA_MAX_DECAY = math.log(1.0 / HYENA_TARGET) / 0.3
DIFF_QK_DIM = HEAD_DIM
DIFF_V_DIM = 2 * HEAD_DIM
DIFF_HEADS = GROUP_W // DIFF_V_DIM
DIFF_BLOCK = 128
N_EXPERTS = 16
N_GROUPS = 4
EXPERTS_PER_GROUP = N_EXPERTS // N_GROUPS
TOP_K = 2
D_EXPERT = 1024

COL_SIZES = (GLA_HEADS * GLA_DK, GLA_HEADS * GLA_DK, GLA_HEADS * GLA_DV, GLA_HEADS * GLA_DV, 2 * GLA_GATE_RANK,
             SWA_HEADS * HEAD_DIM, SWA_KV_HEADS * HEAD_DIM, SWA_KV_HEADS * HEAD_DIM,
             (HYENA_ORDER + 1) * HYENA_CH,
             DIFF_HEADS * 2 * DIFF_QK_DIM, DIFF_HEADS * 2 * DIFF_QK_DIM, DIFF_HEADS * DIFF_V_DIM)
D_IN = sum(COL_SIZES)
COL_OFFSETS = tuple(int(o) for o in np.cumsum(COL_SIZES)[:-1])
D_MIX = GLA_HEADS * GLA_DV + SWA_HEADS * HEAD_DIM + HYENA_CH + DIFF_HEADS * DIFF_V_DIM

kernel_name = "hybrid_parallel_heads_moe_dit"

F32 = jnp.float32


def rmsnorm(x, w):
    xf = x.astype(F32)
    y = xf * lax.rsqrt(jnp.mean(xf * xf, axis=-1, keepdims=True) + NORM_EPS)
    return (y * w.astype(F32)).astype(x.dtype)


def rope_tables(row_ids, col_ids):
    n = HEAD_DIM // 4
    inv = ROPE_THETA ** (-jnp.arange(n, dtype=F32) / n)
    ar = row_ids[:, None] * inv
    ac = col_ids[:, None] * inv
    return (jnp.cos(ar), jnp.sin(ar), jnp.cos(ac), jnp.sin(ac))


def rope_2d(x, rope):
    cos_r, sin_r, cos_c, sin_c = rope
    shp = (1, x.shape[1]) + (1,) * (x.ndim - 3) + (cos_r.shape[-1],)

    def rot(xh, cos, sin):
        cos = cos.reshape(shp).astype(xh.dtype)
        sin = sin.reshape(shp).astype(xh.dtype)
        x1, x2 = jnp.split(xh, 2, axis=-1)
        return jnp.concatenate([x1 * cos - x2 * sin, x2 * cos + x1 * sin], axis=-1)

    xr, xcol = jnp.split(x, 2, axis=-1)
    return jnp.concatenate([rot(xr, cos_r, sin_r), rot(xcol, cos_c, sin_c)], axis=-1)


def gla_scan(q, k, v, log_a, s0, with_output):
    B, L, H, dk = k.shape
    dv = v.shape[-1]
    C = GLA_CHUNK
    n = L // C
    r = lambda t: t.reshape(B, n, C, H, t.shape[-1]).astype(F32)
    q, k, v, log_a = r(q), r(k), r(v), r(log_a)
    b = jnp.cumsum(log_a, axis=2)
    b_last = b[:, :, -1]
    chunk_kv = jnp.einsum('bnchd,bnchv->bnhdv', k * jnp.exp(b_last[:, :, None] - b), v)
    chunk_decay = jnp.exp(b_last)

    def step(s, inp):
        dec, kv = inp
        return dec[..., None] * s + kv, s

    s_final, s_start = lax.scan(step, s0, (jnp.moveaxis(chunk_decay, 1, 0), jnp.moveaxis(chunk_kv, 1, 0)))
    if not with_output:
        return None, s_final
    s_start = jnp.moveaxis(s_start, 0, 1)
    o_inter = jnp.einsum('bnchd,bnhdv->bnchv', q * jnp.exp(b), s_start)
    tri = jnp.tril(jnp.ones((C, C), dtype=bool))
    diff = b[:, :, :, None] - b[:, :, None, :]
    decay = jnp.exp(jnp.where(tri[None, None, :, :, None, None], diff, -jnp.inf))
    attn = jnp.einsum('bnthd,bnshd,bntshd->bnhts', q, k, decay)
    o_intra = jnp.einsum('bnhts,bnshv->bnthv', attn, v)
    return (o_inter + o_intra).reshape(B, L, H, dv), s_final


def gla_mixer(cols_c, cols_l, p, update_ctx):
    def prep(cols):
        q, k, v, g, a = cols
        B, L, _ = q.shape
        q = q.reshape(B, L, GLA_HEADS, GLA_DK) * (GLA_DK ** -0.5)
        k = k.reshape(B, L, GLA_HEADS, GLA_DK)
        v = v.reshape(B, L, GLA_HEADS, GLA_DV)
        z = jnp.einsum('bldr,drk->bldk', a.reshape(B, L, 2, GLA_GATE_RANK), p['gla_gate_w']) + p['gla_gate_b']
        la = (jax.nn.log_sigmoid(z.astype(F32)) / GLA_GATE_TEMP).reshape(B, L, 2, GLA_HEADS, GLA_DK)
        return q, k, v, g, la[:, :, 0], la[:, :, 1]

    qc, kc, vc, gc, lfc, lbc = prep(cols_c)
    ql, kl, vl, gl, lfl, lbl = prep(cols_l)
    B = ql.shape[0]
    s0 = jnp.zeros((B, GLA_HEADS, GLA_DK, GLA_DV), F32)
    fl = lambda t: jnp.flip(t, axis=1)
    oc_f, s_f = gla_scan(qc, kc, vc, lfc, s0, update_ctx)
    oc_b, s_b = gla_scan(fl(qc), fl(kc), fl(vc), fl(lbc), s0, update_ctx)
    ol_f, _ = gla_scan(ql, kl, vl, lfl, s_f, True)
    ol_b, _ = gla_scan(fl(ql), fl(kl), fl(vl), fl(lbl), s_b, True)

    def finish(o, g):
        o = rmsnorm(o.astype(g.dtype), p['gla_norm_w'])
        return o.reshape(g.shape) * jax.nn.silu(g)

    out_l = finish(ol_f + fl(ol_b), gl)
    out_c = finish(oc_f + fl(oc_b), gc) if update_ctx else None
    return out_c, out_l


def swa_latent(q, k, v, kc, vc, sink):
    B, S, Hq, hd = q.shape
    Hkv = k.shape[2]
    G = Hq // Hkv
    W = SWA_BLOCK
    nb = S // W
    Lc = kc.shape[1]
    scale = hd ** -0.5
    qb = q.reshape(B, nb, W, Hkv, G, hd)

    def band(t):
        tp = jnp.pad(t, ((0, 0), (W, W), (0, 0), (0, 0))).reshape(B, nb + 2, W, Hkv, hd)
        return jnp.concatenate([tp[:, :-2], tp[:, 1:-1], tp[:, 2:]], axis=2)

    kb, vb = band(k), band(v)
    s_loc = jnp.einsum('bnqhgd,bnkhd->bnhgqk', qb, kb).astype(F32) * scale
    blk = jnp.arange(nb)[:, None] * W
    qpos = blk + jnp.arange(W)[None, :]
    kpos = blk - W + jnp.arange(3 * W)[None, :]
    valid = ((jnp.abs(qpos[:, :, None] - kpos[:, None, :]) <= SWA_WINDOW)
             & (kpos >= 0)[:, None, :] & (kpos < S)[:, None, :])
    s_loc = jnp.where(valid[None, :, None, None], s_loc, -jnp.inf)
    s_ctx = jnp.einsum('bnqhgd,bkhd->bnhgqk', qb, kc).astype(F32) * scale
    s_sink = jnp.broadcast_to(sink.astype(F32).reshape(1, 1, Hkv, G, 1, 1), s_ctx.shape[:-1] + (1,))
    prob = jax.nn.softmax(jnp.concatenate([s_sink, s_ctx, s_loc], axis=-1), axis=-1).astype(v.dtype)
    o = (jnp.einsum('bnhgqk,bkhd->bnqhgd', prob[..., 1:1 + Lc], vc)
         + jnp.einsum('bnhgqk,bnkhd->bnqhgd', prob[..., 1 + Lc:], vb))
    return o.reshape(B, S, Hq * hd)


def swa_context(q, k, v, sink):
    B, L, Hq, hd = q.shape
    Hkv = k.shape[2]
    G = Hq // Hkv
    s = jnp.einsum('bqhgd,bkhd->bhgqk', q.reshape(B, L, Hkv, G, hd), k).astype(F32) * (hd ** -0.5)
    s_sink = jnp.broadcast_to(sink.astype(F32).reshape(1, Hkv, G, 1, 1), s.shape[:-1] + (1,))
    prob = jax.nn.softmax(jnp.concatenate([s_sink, s], axis=-1), axis=-1)[..., 1:].astype(v.dtype)
    return jnp.einsum('bhgqk,bkhd->bqhgd', prob, v).reshape(B, L, Hq * hd)


def swa_mixer(cols_c, cols_l, p, rope, update_ctx):
    qc, kc, vc = cols_c
    ql, kl, vl = cols_l
    heads = lambda t, n: t.reshape(t.shape[0], t.shape[1], n, HEAD_DIM)
    qn = lambda t: rmsnorm(heads(t, SWA_HEADS), p['swa_q_norm_w'])
    kn = lambda t: rmsnorm(heads(t, SWA_KV_HEADS), p['swa_k_norm_w'])
    kc_, vc_ = kn(kc), heads(vc, SWA_KV_HEADS)
    out_l = swa_latent(rope_2d(qn(ql), rope), rope_2d(kn(kl), rope), heads(vl, SWA_KV_HEADS),
                       kc_, vc_, p['swa_sink'])
    out_c = swa_context(qn(qc), kc_, vc_, p['swa_sink']) if update_ctx else None
    return out_c, out_l


def short_conv(u, w, b):
    up = jnp.pad(u, ((0, 0), (1, 1), (0, 0)))
    return up[:, :-2] * w[0] + up[:, 1:-1] * w[1] + up[:, 2:] * w[2] + b


def hyena_filter_spectrum(L, p):
    t = jnp.linspace(0.0, 1.0, L, dtype=F32)[:, None]
    w = (2.0 * math.pi / L) * jnp.arange(L, dtype=F32)[:, None]
    bands = jnp.linspace(1e-4, HYENA_BANDS - 1, HYENA_BANDS, dtype=F32)
    z = jnp.concatenate([t, jnp.cos(w * bands), -jnp.sin(w * bands)], axis=-1)
    fr = p['hyena_ffn_freq'].astype(F32)
    h = jnp.sin(fr[0] * (z @ p['hyena_ffn_w1'].astype(F32) + p['hyena_ffn_b1'].astype(F32)))
    h = jnp.sin(fr[1] * (h @ p['hyena_ffn_w2'].astype(F32) + p['hyena_ffn_b2'].astype(F32)))
    h = (h @ p['hyena_ffn_w3'].astype(F32)).reshape(L, HYENA_ORDER, 2, HYENA_CH)
    deltas = jnp.linspace(HYENA_MIN_DECAY, HYENA_MAX_DECAY, HYENA_CH, dtype=F32)
    h = h * jnp.exp(-t * deltas)[:, None, None, :]
    h = h / jnp.sum(jnp.abs(h), axis=(0, 2), keepdims=True)
    full = jnp.concatenate([h[:, :, 0], jnp.zeros((1, HYENA_ORDER, HYENA_CH), F32),
                            jnp.flip(h[1:, :, 1], axis=0)], axis=0)
    return jnp.fft.rfft(full, axis=0)


def fft_conv(u, spec):
    L = u.shape[1]
    U = jnp.fft.rfft(u.astype(F32), n=2 * L, axis=1)
    return jnp.fft.irfft(U * spec[None], n=2 * L, axis=1)[:, :L].astype(u.dtype)


def hyena_mixer(u_c, u_l, p, update_ctx):
    def run(u):
        z = short_conv(u, p['hyena_conv_w'], p['hyena_conv_b'])
        x1, x2, y = jnp.split(z, 3, axis=-1)
        spec = hyena_filter_spectrum(u.shape[1], p)
        for o, gate in enumerate((x1, x2)):
            y = gate * (fft_conv(y, spec[:, o]) + y * p['hyena_bias'][o])
        return y

    out_l = run(u_l)
    out_c = run(u_c) if update_ctx else None
    return out_c, out_l


def diff_attend(q, k, v, lam):
    s = jnp.einsum('bqhmd,bkhmd->bhmqk', q, k).astype(F32) * (q.shape[-1] ** -0.5)
    prob = jax.nn.softmax(s, axis=-1)
    wgt = prob[:, :, 0] - lam * prob[:, :, 1]
    return jnp.einsum('bhqk,bkhe->bqhe', wgt.astype(v.dtype), v)


def diff_attend_blocks(q, k, v, lam):
    B, S, H, M, d = q.shape
    nb = S // DIFF_BLOCK
    qb = jnp.moveaxis(q.reshape(B, nb, DIFF_BLOCK, H, M, d), 1, 0)
    o = lax.map(lambda qq: diff_attend(qq, k, v, lam), qb)
    return jnp.moveaxis(o, 0, 1).reshape(B, S, H, -1)


def diff_mixer(cols_c, cols_l, p, rope, lam_init, update_ctx):
    dl = p['diff_lambda'].astype(F32)
    lam = jnp.exp(jnp.sum(dl[0] * dl[1])) - jnp.exp(jnp.sum(dl[2] * dl[3])) + lam_init
    qk = lambda t: t.reshape(t.shape[0], t.shape[1], DIFF_HEADS, 2, DIFF_QK_DIM)
    vh = lambda t: t.reshape(t.shape[0], t.shape[1], DIFF_HEADS, DIFF_V_DIM)
    qn = lambda t: rmsnorm(qk(t), p['diff_q_norm_w'])
    kn = lambda t: rmsnorm(qk(t), p['diff_k_norm_w'])
    qc, kc, vc = cols_c
    ql, kl, vl = cols_l
    kc_, vc_ = kn(kc), vh(vc)
    k_all = jnp.concatenate([kc_, rope_2d(kn(kl), rope)], axis=1)
    v_all = jnp.concatenate([vc_, vh(vl)], axis=1)

    def post(o):
        o = rmsnorm(o, p['diff_subln_w']) * (1.0 - lam_init)
        return o.reshape(o.shape[0], o.shape[1], DIFF_HEADS * DIFF_V_DIM)

    out_l = post(diff_attend_blocks(rope_2d(qn(ql), rope), k_all, v_all, lam))
    out_c = post(diff_attend(qn(qc), kc_, vc_, lam)) if update_ctx else None
    return out_c, out_l


def moe(h, router_w, router_bias, w_gate, w_up, w_down):
    Bh, Lh, D = h.shape
    t = h.reshape(-1, D)
    probs = jax.nn.softmax((t @ router_w).astype(F32), axis=-1)
    sel = (probs + router_bias.astype(F32)).reshape(-1, N_GROUPS, EXPERTS_PER_GROUP)
    group = jnp.argmax(jnp.max(sel, axis=-1), axis=-1)
    sel_g = jnp.take_along_axis(sel, group[:, None, None], axis=1)[:, 0]
    _, idx = lax.top_k(sel_g, TOP_K)
    expert = group[:, None] * EXPERTS_PER_GROUP + idx
    wts = jnp.take_along_axis(probs, expert, axis=-1)
    wts = wts / jnp.sum(wts, axis=-1, keepdims=True)
    comb = jnp.sum(jax.nn.one_hot(expert, N_EXPERTS, dtype=F32) * wts[..., None], axis=1).astype(t.dtype)
    y = jnp.zeros_like(t)
    for e in range(N_EXPERTS):
        he = jax.nn.silu(t @ w_gate[e]) * (t @ w_up[e])
        y = y + comb[:, e:e + 1] * (he @ w_down[e])
    return y.reshape(Bh, Lh, D)


def hybrid_layer(x, xc, c, c_ctx, p, router_w, router_bias, rope, lam_init, update_ctx):
    Lc = xc.shape[1]
    mod_l = jnp.split(jax.nn.silu(c) @ p['ada_w'] + p['ada_b'], 6, axis=-1)
    mod_c = jnp.split(jax.nn.silu(c_ctx) @ p['ada_w'] + p['ada_b'], 6, axis=-1)
    sh_l, sc_l, ga_l, shf_l, scf_l, gf_l = [m[:, None, :] for m in mod_l]
    sh_c, sc_c, ga_c, shf_c, scf_c, gf_c = mod_c

    h_l = rmsnorm(x, p['norm1_w']) * (1.0 + sc_l) + sh_l
    h_c = rmsnorm(xc, p['norm1_w']) * (1.0 + sc_c) + sh_c
    u = jnp.concatenate([h_c, h_l], axis=1) @ p['w_in']
    cols_c = jnp.split(u[:, :Lc], COL_OFFSETS, axis=-1)
    cols_l = jnp.split(u[:, Lc:], COL_OFFSETS, axis=-1)

    a_c, a_l = gla_mixer(cols_c[0:5], cols_l[0:5], p, update_ctx)
    b_c, b_l = swa_mixer(cols_c[5:8], cols_l[5:8], p, rope, update_ctx)
    y_c, y_l = hyena_mixer(cols_c[8], cols_l[8], p, update_ctx)
    d_c, d_l = diff_mixer(cols_c[9:12], cols_l[9:12], p, rope, lam_init, update_ctx)

    x = x + ga_l * (jnp.concatenate([a_l, b_l, y_l, d_l], axis=-1) @ p['w_out'])
    f_l = rmsnorm(x, p['norm2_w']) * (1.0 + scf_l) + shf_l
    if update_ctx:
        xc = xc + ga_c * (jnp.concatenate([a_c, b_c, y_c, d_c], axis=-1) @ p['w_out'])
        f_c = rmsnorm(xc, p['norm2_w']) * (1.0 + scf_c) + shf_c
        f = moe(jnp.concatenate([f_c, f_l], axis=1), router_w, router_bias,
                p['expert_w_gate'], p['expert_w_up'], p['expert_w_down'])
        xc = xc + gf_c * f[:, :Lc]
        x = x + gf_l * f[:, Lc:]
    else:
        x = x + gf_l * moe(f_l, router_w, router_bias, p['expert_w_gate'], p['expert_w_up'], p['expert_w_down'])
    return x, xc


def setup_inputs(seed: int = 0) -> dict:
    key = jax.random.key(seed)
    ks = iter(jax.random.split(key, 48))
    nrm = lambda shape, scale: jax.random.normal(next(ks), shape, F32) * scale
    gain = lambda shape: 1.0 + nrm(shape, 0.02)
    D = D_MODEL
    return {
        "x": nrm((BATCH, SEQ, D), 1.0),
        "c": nrm((BATCH, D), 1.0),
        "ctx": nrm((BATCH, CTX_LEN, D), 1.0),
        "c_ctx": nrm((D,), 1.0),
        "norm1_w": gain((DEPTH, D)),
        "norm2_w": gain((DEPTH, D)),
        "ada_w": nrm((DEPTH, D, 6 * D), 0.5 * D ** -0.5),
        "ada_b": nrm((DEPTH, 6 * D), 0.01),
        "w_in": nrm((DEPTH, D, D_IN), D ** -0.5),
        "w_out": nrm((DEPTH, D_MIX, D), D_MIX ** -0.5),
        "gla_gate_w": nrm((DEPTH, 2, GLA_GATE_RANK, GLA_HEADS * GLA_DK), GLA_GATE_RANK ** -0.5),
        "gla_gate_b": nrm((DEPTH, 2, GLA_HEADS * GLA_DK), 0.1),
        "gla_norm_w": gain((DEPTH, GLA_DV)),
        "swa_q_norm_w": gain((DEPTH, HEAD_DIM)),
        "swa_k_norm_w": gain((DEPTH, HEAD_DIM)),
        "swa_sink": nrm((DEPTH, SWA_HEADS), 0.5),
        "hyena_conv_w": nrm((DEPTH, 3, (HYENA_ORDER + 1) * HYENA_CH), 3 ** -0.5),
        "hyena_conv_b": nrm((DEPTH, (HYENA_ORDER + 1) * HYENA_CH), 0.01),
        "hyena_ffn_w1": nrm((DEPTH, HYENA_EMB, HYENA_HIDDEN), HYENA_EMB ** -0.5),
        "hyena_ffn_b1": nrm((DEPTH, HYENA_HIDDEN), 0.1),
        "hyena_ffn_w2": nrm((DEPTH, HYENA_HIDDEN, HYENA_HIDDEN), HYENA_HIDDEN ** -0.5),
        "hyena_ffn_b2": nrm((DEPTH, HYENA_HIDDEN), 0.1),
        "hyena_ffn_w3": nrm((DEPTH, HYENA_HIDDEN, HYENA_ORDER * 2 * HYENA_CH), HYENA_HIDDEN ** -0.5),
        "hyena_ffn_freq": 1.0 + nrm((DEPTH, 2, HYENA_HIDDEN), 0.1),
        "hyena_bias": nrm((DEPTH, HYENA_ORDER, HYENA_CH), 1.0),
        "diff_q_norm_w": gain((DEPTH, DIFF_QK_DIM)),
        "diff_k_norm_w": gain((DEPTH, DIFF_QK_DIM)),
        "diff_lambda": nrm((DEPTH, 4, DIFF_QK_DIM), 0.1),
        "diff_subln_w": gain((DEPTH, DIFF_V_DIM)),
        "router_w": nrm((D, N_EXPERTS), D ** -0.5),
        "router_bias": nrm((N_EXPERTS,), 0.01),
        "expert_w_gate": nrm((DEPTH, N_EXPERTS, D, D_EXPERT), D ** -0.5),
        "expert_w_up": nrm((DEPTH, N_EXPERTS, D, D_EXPERT), D ** -0.5),
        "expert_w_down": nrm((DEPTH, N_EXPERTS, D_EXPERT, D), D_EXPERT ** -0.5),
    }


def reference(x, c, ctx, c_ctx, norm1_w, norm2_w, ada_w, ada_b, w_in, w_out, gla_gate_w, gla_gate_b, gla_norm_w,
              swa_q_norm_w, swa_k_norm_w, swa_sink, hyena_conv_w, hyena_conv_b, hyena_ffn_w1, hyena_ffn_b1,
              hyena_ffn_w2, hyena_ffn_b2, hyena_ffn_w3, hyena_ffn_freq, hyena_bias, diff_q_norm_w, diff_k_norm_w,
              diff_lambda, diff_subln_w, router_w, router_bias, expert_w_gate, expert_w_up, expert_w_down):
    S = x.shape[1]
    rows = S // GRID_W
    row_ids = jnp.repeat(jnp.arange(rows), GRID_W).astype(F32)
    col_ids = jnp.tile(jnp.arange(GRID_W), rows).astype(F32)
    rope = rope_tables(row_ids, col_ids)
    xc = ctx
    for l in range(DEPTH):
        p = dict(norm1_w=norm1_w[l], norm2_w=norm2_w[l], ada_w=ada_w[l], ada_b=ada_b[l], w_in=w_in[l],
                 w_out=w_out[l], gla_gate_w=gla_gate_w[l], gla_gate_b=gla_gate_b[l], gla_norm_w=gla_norm_w[l],
                 swa_q_norm_w=swa_q_norm_w[l], swa_k_norm_w=swa_k_norm_w[l], swa_sink=swa_sink[l],
                 hyena_conv_w=hyena_conv_w[l], hyena_conv_b=hyena_conv_b[l], hyena_ffn_w1=hyena_ffn_w1[l],
                 hyena_ffn_b1=hyena_ffn_b1[l], hyena_ffn_w2=hyena_ffn_w2[l], hyena_ffn_b2=hyena_ffn_b2[l],
                 hyena_ffn_w3=hyena_ffn_w3[l], hyena_ffn_freq=hyena_ffn_freq[l], hyena_bias=hyena_bias[l],
                 diff_q_norm_w=diff_q_norm_w[l], diff_k_norm_w=diff_k_norm_w[l], diff_lambda=diff_lambda[l],
                 diff_subln_w=diff_subln_w[l], expert_w_gate=expert_w_gate[l], expert_w_up=expert_w_up[l],
                 expert_w_down=expert_w_down[l])
        lam_init = 0.8 - 0.6 * math.exp(-0.3 * l)
        x, xc = hybrid_layer(x, xc, c, c_ctx, p, router_w, router_bias, rope, lam_init, l < DEPTH - 1)
    return x
```

```python
import math
import numpy as np
import ml_dtypes
import concourse.bass as bass
import concourse.mybir as mybir
from concourse.bass_utils import run_bass_kernel_spmd

F32 = mybir.dt.float32
BF16 = mybir.dt.bfloat16
ALU = mybir.AluOpType
AF = mybir.ActivationFunctionType
AX = mybir.AxisListType

D = 2048
S = 2048
LC = 256
T = S + LC
NT = T // 128
DIN = 5408
DEPTH = 2
EPS = 1e-6
NE = 16
DFF = 1024
C_GQ, C_GK, C_GV, C_GG, C_GA = 0, 256, 512, 1024, 1536
C_SQ, C_SK, C_SV = 1568, 2080, 2208
C_HY = 2336
C_DQ, C_DK, C_DV = 3872, 4384, 4896


class Buf:
    __slots__ = ("t", "name", "w", "r")

    def __init__(self, t, name=""):
        self.t = t
        self.name = name
        self.w = {}
        self.r = {}

    def __getitem__(self, idx):
        return self.t[idx]


class K:
    def __init__(self, nc, n_dma_sems=48):
        self.nc = nc
        self.engs = {"pe": nc.tensor, "act": nc.scalar, "dve": nc.vector, "pool": nc.gpsimd, "sp": nc.sync}
        self._ctx = []
        self.sems = {}
        self.count = {}
        self.semobj = {}
        for n in self.engs:
            self.sems[n] = self._sem("e_" + n)
            self.count[n] = 0
            self.semobj[("e", n)] = self.sems[n]
        self.dma_sems = []
        for i in range(n_dma_sems):
            s = self._sem("d%d" % i)
            self.dma_sems.append([s, 0])
            self.semobj[("d", i)] = s
        self.dma_rr = 0
        self.waited = {n: {} for n in self.engs}
        self.pending = {}
        self.epoch = {}
        self.n_inst = 0
        self.n_wait = 0

    def _sem(self, name):
        cm = self.nc.semaphore(name)
        s = cm.__enter__()
        self._ctx.append((cm, None))
        return s

    def _newbuf(self, t, name):
        b = Buf(t, name)
        b.r = dict(self.pending)
        return b

    def sbuf(self, name, shape, dtype):
        self.n_alloc = getattr(self, "n_alloc", 0) + 1
        name = "%s_%d" % (name, self.n_alloc)
        cm = self.nc.sbuf_tensor(name, list(shape), dtype)
        t = cm.__enter__()
        b = self._newbuf(t, name)
        self._ctx.append((cm, b))
        return b

    def psum(self, name, shape, dtype=F32):
        self.n_alloc = getattr(self, "n_alloc", 0) + 1
        name = "%s_%d" % (name, self.n_alloc)
        cm = self.nc.psum_tensor(name, list(shape), dtype)
        t = cm.__enter__()
        b = self._newbuf(t, name)
        self._ctx.append((cm, b))
        return b

    def dram(self, name, shape, dtype, kind="Internal"):
        t = self.nc.dram_tensor(name, list(shape), dtype, kind=kind)
        return Buf(t.ap(), name)

    def mark(self):
        return len(self._ctx)

    def release(self, mark):
        while len(self._ctx) > mark:
            cm, b = self._ctx.pop()
            if b is not None:
                for evs in (b.w, b.r):
                    for key, (val, en) in evs.items():
                        if self.pending.get(key, (0, ""))[0] < val:
                            self.pending[key] = (val, "rel")
            cm.__exit__(None, None, None)

    def _need(self, eng, reads, writes):
        need = {}
        is_dma = eng.startswith("dma:")

        def add(evs, same_ok, skip_dma=False):
            for key, (val, en) in evs.items():
                if en == eng and same_ok:
                    continue
                if skip_dma and en == "dma":
                    continue
                if need.get(key, 0) < val:
                    need[key] = val

        for b in reads:
            add(b.w, False)
        for b in writes:
            add(b.w, True, skip_dma=is_dma)
            add(b.r, True)
        return need

    def _do_waits(self, eng, need):
        e = self.engs[eng]
        wd = self.waited[eng]
        for key, val in need.items():
            if wd.get(key, 0) >= val:
                continue
            e.wait_ge(self.semobj[key], val)
            wd[key] = val
            self.n_wait += 1

    def op(self, eng, reads, writes, fn):
        need = self._need(eng, reads, writes)
        self._do_waits(eng, need)
        if self.count[eng] >= 30000:
            self.epoch[eng] = self.epoch.get(eng, 0) + 1
            self.sems[eng] = self._sem("e_%s_%d" % (eng, self.epoch[eng]))
            self.count[eng] = 0
            self.semobj[("e", eng, self.epoch[eng])] = self.sems[eng]
        ins = fn()
        self.count[eng] += 1
        ins.then_inc(self.sems[eng], 1)
        ep = self.epoch.get(eng, 0)
        key = ("e", eng) if ep == 0 else ("e", eng, ep)
        ev = (self.count[eng], eng)
        for b in reads:
            b.r[key] = ev
        for b in writes:
            b.w = {key: ev}
            b.r = {}
        self.n_inst += 1
        return ins

    def dma(self, queue, out_buf, out_ap, in_buf, in_ap, **kw):
        i = self.dma_rr
        self.dma_rr = (self.dma_rr + 1) % len(self.dma_sems)
        d = self.dma_sems[i]
        key = ("d", i)
        need = self._need("dma:" + queue, [in_buf], [out_buf])
        if d[1] > 0 and need.get(key, 0) < d[1]:
            need[key] = d[1]
        self._do_waits(queue, need)
        ins = self.engs[queue].dma_start(out=out_ap, in_=in_ap, **kw)
        d[1] += 16
        ins.then_inc(d[0], 16)
        ev = (d[1], "dma")
        in_buf.r[key] = ev
        if out_buf.r or any(en != "dma" for (_, en) in out_buf.w.values()):
            out_buf.w = {}
        out_buf.w[key] = ev
        out_buf.r = {}
        self.n_inst += 1
        return ins

    def finish(self, out_bufs):
        need = {}
        for b in out_bufs:
            for key, (val, _) in b.w.items():
                if need.get(key, 0) < val:
                    need[key] = val
        self._do_waits("sp", need)
        need2 = {}
        for n in self.engs:
            if n != "sp" and self.count[n] > 0:
                ep = self.epoch.get(n, 0)
                need2[("e", n) if ep == 0 else ("e", n, ep)] = self.count[n]
        self._do_waits("sp", need2)
        for i, d in enumerate(self.dma_sems):
            if d[1] > 0:
                self._do_waits("sp", {("d", i): d[1]})

    def close(self):
        self.release(0)


class Ctx:
    pass


def _mm(k, out_b, out_ap, l_b, l_ap, r_b, r_ap, start, stop):
    nc = k.nc
    return k.op("pe", [l_b, r_b], [out_b], lambda: nc.tensor.matmul(out_ap, lhsT=l_ap, rhs=r_ap, start=start, stop=stop))


def _tr(k, out_b, out_ap, in_b, in_ap, ident_b, ident_ap):
    nc = k.nc
    return k.op("pe", [in_b, ident_b], [out_b], lambda: nc.tensor.transpose(out_ap, in_ap, ident_ap))


def _copy(k, eng, out_b, out_ap, in_b, in_ap):
    nc = k.nc
    if eng == "act":
        return k.op("act", [in_b], [out_b], lambda: nc.scalar.copy(out=out_ap, in_=in_ap))
    e = nc.vector if eng == "dve" else nc.gpsimd
    return k.op(eng, [in_b], [out_b], lambda: e.tensor_copy(out=out_ap, in_=in_ap))


def _rsqrt(k, out_b, out_ap, in_b, in_ap, eps, scale=1.0):
    nc = k.nc
    k.op("act", [in_b], [out_b], lambda: nc.scalar.activation(out=out_ap, in_=in_ap, func=AF.Sqrt, bias=eps, scale=scale))
    k.op("dve", [out_b], [out_b], lambda: nc.vector.reciprocal(out=out_ap, in_=out_ap))


def make_consts(k, cx):
    nc = k.nc
    cx.ident = k.sbuf("ident", [128, 128], BF16)
    cx.ident32 = k.sbuf("ident32", [128, 128], F32)
    for t in (cx.ident, cx.ident32):
        k.op("pool", [], [t], lambda: nc.gpsimd.memset(t[:, :], 1.0))
        k.op("pool", [t], [t], lambda: nc.gpsimd.affine_select(out=t[:, :], in_=t[:, :], pattern=[[-1, 128]],
                                                               compare_op=ALU.is_equal, fill=0.0, base=0,
                                                               channel_multiplier=1))


def stage_mod(k, cx, W):
    nc = k.nc
    m0 = k.mark()
    cv = k.sbuf("cv", [128, 2, 16], F32)
    csT = k.sbuf("csT", [128, 16, 2], BF16)
    for r in range(2):
        k.dma("sp", cv, cv[:, r, :], W["cvec"], W["cvec"][r, :].rearrange("(p k) -> p k", k=16))
    k.op("act", [cv], [csT], lambda: nc.scalar.activation(out=csT[:, :, :].rearrange("p k r -> p r k"), in_=cv[:, :, :],
                                                           func=AF.Silu))
    wb = [k.sbuf("adaw%d" % i, [128, 16, 512], BF16) for i in range(2)]
    bb = [k.sbuf("adab%d" % i, [2, 512], F32) for i in range(2)]
    ob = [k.sbuf("modo%d" % i, [2, 512], F32) for i in range(2)]
    ps = [k.psum("modps%d" % i, [128, 512], F32) for i in range(2)]
    it = 0
    for l in range(DEPTH):
        for n in range(24):
            s = it % 2
            it += 1
            wt = wb[s]
            k.dma("pool", wt, wt[:, :, :], W["ada_w"],
                  W["ada_w"][l, :, n * 512:(n + 1) * 512].rearrange("(p k) n -> p k n", k=16))
            k.dma("sp", bb[s], bb[s][:, :], W["ada_b"], W["ada_b"][l:l + 1, n * 512:(n + 1) * 512].broadcast_to([2, 512]))
            for kk in range(16):
                _mm(k, ps[s], ps[s][0:2, :], csT, csT[:, kk, :], wt, wt[:, kk, :], kk == 0, kk == 15)
            k.op("dve", [ps[s], bb[s]], [ob[s]], lambda: nc.vector.tensor_tensor(out=ob[s][:, :], in0=ps[s][0:2, :],
                                                                                  in1=bb[s][:, :], op=ALU.add))
            k.dma("sp", W["mod"], W["mod"][l, :, n * 512:(n + 1) * 512], ob[s], ob[s][:, :])
    k.release(m0)


def _bc_load(k, name, src_b, src_ap_row, n, dtype=F32, parts=128, queue="sp"):
    t = k.sbuf(name, [parts, n], dtype)
    k.dma(queue, t, t[:, :], src_b, src_ap_row.broadcast_to([parts, n]))
    return t


def stage_inproj(k, cx, W, l, xsrc):
    nc = k.nc
    m0 = k.mark()
    wsc = []
    shb = []
    for r in range(2):
        a = _bc_load(k, "wsc%d" % r, W["mod"], W["mod"][l, r:r + 1, 2048:4096], D)
        b = _bc_load(k, "shb%d" % r, W["mod"], W["mod"][l, r:r + 1, 0:2048], D)
        wsc.append(a)
        shb.append(b)
    m1 = k.mark()
    nw = _bc_load(k, "nw", W["norm1_w"], W["norm1_w"][l:l + 1, :], D)
    for r in range(2):
        a = wsc[r]
        k.op("dve", [a, nw], [a], lambda: nc.vector.scalar_tensor_tensor(out=a[:, :], in0=a[:, :], scalar=1.0, in1=nw[:, :],
                                                                         op0=ALU.add, op1=ALU.mult))
        k.op("act", [a], [a], lambda: nc.scalar.mul(out=a[:, :], in_=a[:, :], mul=math.sqrt(D)))
    k.release(m1)
    hT = [k.sbuf("hT%d" % i, [128, 16, 128], BF16) for i in range(NT)]
    xt = [k.sbuf("xt%d" % i, [128, D], F32) for i in range(2)]
    hb = [k.sbuf("hb%d" % i, [128, D], BF16) for i in range(2)]
    junk = k.sbuf("junk", [128, D], BF16)
    ss = k.sbuf("ss", [128, NT], F32)
    rs = k.sbuf("rs", [128, NT], F32)
    tps = [k.psum("tp%d" % i, [128, 4, 128], BF16) for i in range(2)]
    pss = [k.psum("pps%d" % i, [128, 512], F32) for i in range(4)]
    wbuf = [k.sbuf("winb%d" % i, [128, 16, 512], BF16) for i in range(2)]
    uos = [k.sbuf("uo%d" % i, [128, 512], BF16) for i in range(4)]
    k.op("dve", [], [ss], lambda: nc.vector.memset(ss[:, :], 0.0))
    def load_w(g):
        c0 = g * 512
        cw = min(512, DIN - c0)
        wt = wbuf[g % 2]
        k.dma("pool", wt, wt[:, :, 0:cw], W["w_in"], W["w_in"][l, :, c0:c0 + cw].rearrange("(k p) n -> p k n", p=128))
    load_w(0)
    for i in range(NT):
        xb = xt[i % 2]
        h = hb[i % 2]
        sb, sap = xsrc(i)
        k.dma("sp", xb, xb[:, :], sb, sap)
        r = 1 if i < 2 else 0
        k.op("act", [xb], [junk, ss], lambda: nc.scalar.activation(out=junk[:, :], in_=xb[:, :], func=AF.Square,
                                                                    accum_out=ss[:, i:i + 1]))
        _rsqrt(k, rs, rs[:, i:i + 1], ss, ss[:, i:i + 1], EPS * D)
        k.op("dve", [xb, rs, wsc[r]], [xb], lambda: nc.vector.scalar_tensor_tensor(out=xb[:, :], in0=xb[:, :],
                                                                                   scalar=rs[:, i:i + 1], in1=wsc[r][:, :],
                                                                                   op0=ALU.mult, op1=ALU.mult))
        k.op("pool", [xb, shb[r]], [h], lambda: nc.gpsimd.tensor_tensor(out=h[:, :], in0=xb[:, :], in1=shb[r][:, :],
                                                                         op=ALU.add))
        for kq in range(4):
            tp = tps[(4 * i + kq) % 2]
            for j in range(4):
                kk = 4 * kq + j
                _tr(k, tp, tp[:, j, :], h, h[:, kk * 128:(kk + 1) * 128], cx.ident, cx.ident[:, :])
            _copy(k, "act" if kq % 2 else "dve", hT[i], hT[i][:, 4 * kq:4 * kq + 4, :], tp, tp[:, :, :])
    n = 0
    for g in range(11):
        c0 = g * 512
        cw = min(512, DIN - c0)
        wt = wbuf[g % 2]
        if g + 1 < 11:
            load_w(g + 1)
        for i in range(NT):
            ps = pss[n % 4]
            uo = uos[n % 4]
            for kk in range(16):
                _mm(k, ps, ps[:, 0:cw], hT[i], hT[i][:, kk, :], wt, wt[:, kk, 0:cw], kk == 0, kk == 15)
            _copy(k, "act" if n % 2 else "dve", uo, uo[:, 0:cw], ps, ps[:, 0:cw])
            k.dma("sp", W["u"], W["u"][i * 128:(i + 1) * 128, c0:c0 + cw], uo, uo[:, 0:cw])
            n += 1
    k.release(m0)


def stage_outproj_norm_router(k, cx, W, l, xsrc, tiles):
    nc = k.nc
    m0 = k.mark()
    wo = k.sbuf("wo", [128, 16, D], BF16)
    for q in range(4):
        k.dma("pool", wo, wo[:, 4 * q:4 * q + 4, :], W["w_out"],
              W["w_out"][l, q * 512:(q + 1) * 512, :].rearrange("(k p) n -> p k n", p=128))
    rows = sorted(set(1 if i < 2 else 0 for i in tiles))
    ga = {}
    wsc = {}
    shf = {}
    for r in rows:
        ga[r] = _bc_load(k, "ga%d" % r, W["mod"], W["mod"][l, r:r + 1, 4096:6144], D)
        wsc[r] = _bc_load(k, "wscf%d" % r, W["mod"], W["mod"][l, r:r + 1, 8192:10240], D)
        shf[r] = _bc_load(k, "shf%d" % r, W["mod"], W["mod"][l, r:r + 1, 6144:8192], D)
    xt = [k.sbuf("oxt%d" % i, [128, D], F32) for i in range(2)]
    nw = xt[1]
    k.dma("sp", nw, nw[:, :], W["norm2_w"], W["norm2_w"][l:l + 1, :].broadcast_to([128, D]))
    for r in rows:
        a = wsc[r]
        k.op("dve", [a, nw], [a], lambda: nc.vector.scalar_tensor_tensor(out=a[:, :], in0=a[:, :], scalar=1.0, in1=nw[:, :],
                                                                         op0=ALU.add, op1=ALU.mult))
        k.op("act", [a], [a], lambda: nc.scalar.mul(out=a[:, :], in_=a[:, :], mul=math.sqrt(D)))
    rw = k.sbuf("rw", [128, 16, NE], F32)
    k.dma("sp", rw, rw[:, :, :], W["router_w"], W["router_w"].t.rearrange("(k p) e -> p k e", p=128))
    rwh = k.sbuf("rwh", [128, 16, NE], BF16)
    rwl = k.sbuf("rwl", [128, 16, NE], BF16)
    k.op("dve", [rw], [rwh], lambda: nc.vector.tensor_copy(out=rwh[:, :, :], in_=rw[:, :, :]))
    k.op("dve", [rw, rwh], [rwl], lambda: nc.vector.tensor_tensor(out=rwl[:, :, :], in0=rw[:, :, :], in1=rwh[:, :, :],
                                                                  op=ALU.subtract))
    rbias = _bc_load(k, "rbias", W["router_bias"], W["router_bias"][0:1, :], NE)
    mt = [k.sbuf("mt%d" % i, [128, D], BF16) for i in range(2)]
    mT = [k.sbuf("mT%d" % i, [128, 16, 128], BF16) for i in range(2)]
    ft_ = [k.sbuf("ft%d" % i, [128, D], F32) for i in range(2)]
    fh_ = [k.sbuf("fh%d" % i, [128, D], BF16) for i in range(2)]
    fl_ = [k.sbuf("fl%d" % i, [128, D], BF16) for i in range(2)]
    fTl_ = [k.sbuf("fTl%d" % i, [128, 16, 128], BF16) for i in range(2)]
    fTb = [k.sbuf("fTb%d" % i, [128, 16, 128], BF16) for i in range(2)]
    junk = k.sbuf("ojunk", [128, D], BF16)
    ss = k.sbuf("oss", [128, NT], F32)
    rs = k.sbuf("ors", [128, NT], F32)
    sm = k.sbuf("rsm", [128, 256], F32)
    tps = [k.psum("otp%d" % i, [128, 4, 128], BF16) for i in range(2)]
    pss = [k.psum("ops%d" % i, [128, 512], F32) for i in range(3)]
    psr = k.psum("opsr", [128, 512], F32)
    k.op("dve", [], [ss], lambda: nc.vector.memset(ss[:, :], 0.0))
    n = 0
    for ii, i in enumerate(tiles):
        r = 1 if i < 2 else 0
        xb = xt[ii % 2]
        m = mt[ii % 2]
        mTt = mT[ii % 2]
        ft, fh, fl, fTl = ft_[ii % 2], fh_[ii % 2], fl_[ii % 2], fTl_[ii % 2]
        sb, sap = xsrc(i)
        k.dma("sp", xb, xb[:, :], sb, sap)
        k.dma("sp", m, m[:, :], W["mix"], W["mix"][i * 128:(i + 1) * 128, :])
        for kq in range(4):
            tp = tps[kq % 2]
            for j in range(4):
                kk = 4 * kq + j
                _tr(k, tp, tp[:, j, :], m, m[:, kk * 128:(kk + 1) * 128], cx.ident, cx.ident[:, :])
            _copy(k, "act" if kq % 2 else "dve", mTt, mTt[:, 4 * kq:4 * kq + 4, :], tp, tp[:, :, :])
        for g in range(4):
            ps = pss[n % 3]
            n += 1
            for kk in range(16):
                _mm(k, ps, ps[:, :], mTt, mTt[:, kk, :], wo, wo[:, kk, g * 512:(g + 1) * 512], kk == 0, kk == 15)
            gs = slice(g * 512, (g + 1) * 512)
            k.op("dve", [ps, ga[r]], [ft], lambda: nc.vector.tensor_tensor(out=ft[:, gs], in0=ps[:, :], in1=ga[r][:, gs],
                                                                          op=ALU.mult))
            k.op("pool", [xb, ft], [xb], lambda: nc.gpsimd.tensor_tensor(out=xb[:, gs], in0=xb[:, gs], in1=ft[:, gs],
                                                                         op=ALU.add))
        k.dma("sp", W["xres"], W["xres"][i * 128:(i + 1) * 128, :], xb, xb[:, :])
        k.op("act", [xb], [junk, ss], lambda: nc.scalar.activation(out=junk[:, :], in_=xb[:, :], func=AF.Square,
                                                                    accum_out=ss[:, i:i + 1]))
        _rsqrt(k, rs, rs[:, i:i + 1], ss, ss[:, i:i + 1], EPS * D)
        k.op("dve", [xb, rs, wsc[r]], [ft], lambda: nc.vector.scalar_tensor_tensor(out=ft[:, :], in0=xb[:, :],
                                                                                   scalar=rs[:, i:i + 1], in1=wsc[r][:, :],
                                                                                   op0=ALU.mult, op1=ALU.mult))
        k.op("pool", [ft, shf[r]], [ft], lambda: nc.gpsimd.tensor_tensor(out=ft[:, :], in0=ft[:, :], in1=shf[r][:, :],
                                                                          op=ALU.add))
        k.op("act", [ft], [fh], lambda: nc.scalar.copy(out=fh[:, :], in_=ft[:, :]))
        k.op("dve", [ft, fh], [fl], lambda: nc.vector.tensor_tensor(out=fl[:, :], in0=ft[:, :], in1=fh[:, :], op=ALU.subtract))
        fb = fTb[ii % 2]
        nq = 0
        for (src, dst) in ((fh, fb), (fl, fTl)):
            for kq in range(4):
                tp = tps[nq % 2]
                nq += 1
                for j in range(4):
                    kk = 4 * kq + j
                    _tr(k, tp, tp[:, j, :], src, src[:, kk * 128:(kk + 1) * 128], cx.ident, cx.ident[:, :])
                _copy(k, "act" if kq % 2 else "dve", dst, dst[:, 4 * kq:4 * kq + 4, :], tp, tp[:, :, :])
        k.dma("sp", W["fT"], W["fT"][:, :, i * 128:(i + 1) * 128], fb, fb[:, :, :])
        terms = [(fb, rwh), (fTl, rwh), (fb, rwl)]
        nmm = 0
        for (fx_, w_) in terms:
            for kk in range(16):
                _mm(k, psr, psr[:, 0:NE], fx_, fx_[:, kk, :], w_, w_[:, kk, :], nmm == 0, nmm == 47)
                nmm += 1
        _router(k, cx, W, psr, sm, rbias, i)
    k.release(m0)


def _router(k, cx, W, psr, sm, rbias, i):
    nc = k.nc
    v = nc.vector
    lg = psr[:, 0:NE]
    mx, nmx, se, rse = sm[:, 0:1], sm[:, 1:2], sm[:, 2:3], sm[:, 3:4]
    best, top1, top2, den = sm[:, 4:5], sm[:, 5:6], sm[:, 6:7], sm[:, 7:8]
    e = sm[:, 16:32]
    probs = sm[:, 32:48]
    sel = sm[:, 48:64]
    gmax = sm[:, 64:68]
    gsel = sm[:, 68:72]
    pen = sm[:, 72:76]
    selm = sm[:, 80:96]
    m1 = sm[:, 96:112]
    selm2 = sm[:, 112:128]
    m2 = sm[:, 128:144]
    wu = sm[:, 144:160]
    comb = sm[:, 160:176]

    def dv(reads_ps, fn):
        return k.op("dve", [sm] + ([psr] if reads_ps else []) + [rbias], [sm], fn)

    dv(True, lambda: v.tensor_reduce(out=mx, in_=lg, axis=AX.X, op=ALU.max))
    dv(False, lambda: v.tensor_scalar(out=nmx, in0=mx, scalar1=-1.0, scalar2=None, op0=ALU.mult))
    dv(False, lambda: v.memset(se, 0.0))
    k.op("act", [psr, sm], [sm], lambda: nc.scalar.activation(out=e, in_=lg, func=AF.Exp, bias=nmx, scale=1.0, accum_out=se))
    dv(False, lambda: v.reciprocal(out=rse, in_=se))
    dv(False, lambda: v.tensor_scalar(out=probs, in0=e, scalar1=rse, scalar2=None, op0=ALU.mult))
    dv(False, lambda: v.tensor_tensor(out=sel, in0=probs, in1=rbias[:, :], op=ALU.add))
    dv(False, lambda: v.tensor_reduce(out=gmax, in_=sel.rearrange("p (g e) -> p g e", e=4), axis=AX.X, op=ALU.max))
    dv(False, lambda: v.tensor_reduce(out=best, in_=gmax, axis=AX.X, op=ALU.max))
    dv(False, lambda: v.tensor_scalar(out=gsel, in0=gmax, scalar1=best, scalar2=None, op0=ALU.is_ge))
    dv(False, lambda: v.tensor_scalar(out=pen, in0=gsel, scalar1=-1.0, scalar2=1e30, op0=ALU.add, op1=ALU.mult))
    dv(False, lambda: v.tensor_tensor(out=selm.rearrange("p (g e) -> p g e", e=4), in0=sel.rearrange("p (g e) -> p g e", e=4),
                                      in1=pen.unsqueeze(2).broadcast_to([128, 4, 4]), op=ALU.add))
    dv(False, lambda: v.tensor_reduce(out=top1, in_=selm, axis=AX.X, op=ALU.max))
    dv(False, lambda: v.tensor_scalar(out=m1, in0=selm, scalar1=top1, scalar2=None, op0=ALU.is_ge))
    dv(False, lambda: v.scalar_tensor_tensor(out=selm2, in0=m1, scalar=-1e30, in1=selm, op0=ALU.mult, op1=ALU.add))
    dv(False, lambda: v.tensor_reduce(out=top2, in_=selm2, axis=AX.X, op=ALU.max))
    dv(False, lambda: v.tensor_scalar(out=m2, in0=selm2, scalar1=top2, scalar2=None, op0=ALU.is_ge))
    dv(False, lambda: v.tensor_tensor(out=m2, in0=m2, in1=m1, op=ALU.add))
    dv(False, lambda: v.tensor_tensor(out=wu, in0=probs, in1=m2, op=ALU.mult))
    dv(False, lambda: v.tensor_reduce(out=den, in_=wu, axis=AX.X, op=ALU.add))
    dv(False, lambda: v.reciprocal(out=den, in_=den))
    dv(False, lambda: v.tensor_scalar(out=comb, in0=wu, scalar1=den, scalar2=None, op0=ALU.mult))
    k.dma("sp", W["comb"], W["comb"][i * 128:(i + 1) * 128, :], sm, comb)


def stage_experts(k, cx, W, l, tiles, out_dst):
    nc = k.nc
    m0 = k.mark()
    nt = len(tiles)
    ntok = nt * 128
    t0 = tiles[0] * 128
    assert tiles == list(range(tiles[0], tiles[0] + nt))
    if ntok % 512 == 0:
        tgs = [(a, 512) for a in range(0, ntok, 512)]
    else:
        tgs = [(a, 384) for a in range(0, ntok, 384)]
        assert ntok % 384 == 0
    fT = k.sbuf("efT", [128, 16, ntok], BF16)
    for q in range(4):
        k.dma("sp", fT, fT[:, 4 * q:4 * q + 4, :], W["fT"], W["fT"][:, 4 * q:4 * q + 4, t0:t0 + ntok])
    comb = k.sbuf("ecomb", [128, nt, NE], F32)
    k.dma("sp", comb, comb[:, :, :], W["comb"], W["comb"][t0:t0 + ntok, :].rearrange("(i p) e -> p i e", p=128))
    yacc = [k.sbuf("yacc%d" % i, [128, D], F32) for i in range(nt)]
    he = [k.sbuf("he%d" % j, [128, ntok], BF16) for j in range(8)]
    wg = [k.sbuf("wg%d" % i, [128, 16, 256], BF16) for i in range(2)]
    wu = [k.sbuf("wu%d" % i, [128, 16, 256], BF16) for i in range(2)]
    wd = [k.sbuf("wd%d" % i, [128, 8, 512], BF16) for i in range(2)]
    sil = [k.sbuf("sil%d" % i, [128, 512], BF16) for i in range(2)]
    psg = [k.psum("psg%d" % i, [128, 512], F32) for i in range(2)]
    psu = [k.psum("psu%d" % i, [128, 512], F32) for i in range(2)]
    psd = [k.psum("psd%d" % i, [128, 512], F32) for i in range(3)]
    ng = 0
    nd = 0
    nq = 0
    ndq = 0
    for e in range(NE):
        for q in range(4):
            g_t = wg[nq % 2]
            u_t = wu[nq % 2]
            nq += 1
            k.dma("pool", g_t, g_t[:, :, :], W["expert_w_gate"],
                  W["expert_w_gate"][l, e, :, q * 256:(q + 1) * 256].rearrange("(k p) n -> p k n", p=128))
            k.dma("pool", u_t, u_t[:, :, :], W["expert_w_up"],
                  W["expert_w_up"][l, e, :, q * 256:(q + 1) * 256].rearrange("(k p) n -> p k n", p=128))
            for c2 in range(2):
                jc = q * 2 + c2
                for (a, n) in tgs:
                    pg = psg[ng % 2]
                    pu = psu[ng % 2]
                    sl = sil[ng % 2]
                    ng += 1
                    for kk in range(16):
                        _mm(k, pg, pg[:, 0:n], g_t, g_t[:, kk, c2 * 128:(c2 + 1) * 128], fT, fT[:, kk, a:a + n], kk == 0, kk == 15)
                    for kk in range(16):
                        _mm(k, pu, pu[:, 0:n], u_t, u_t[:, kk, c2 * 128:(c2 + 1) * 128], fT, fT[:, kk, a:a + n], kk == 0, kk == 15)
                    k.op("act", [pg], [sl], lambda: nc.scalar.activation(out=sl[:, 0:n], in_=pg[:, 0:n], func=AF.Silu))
                    k.op("dve", [sl, pu], [he[jc]], lambda: nc.vector.tensor_tensor(out=he[jc][:, a:a + n], in0=sl[:, 0:n],
                                                                                    in1=pu[:, 0:n], op=ALU.mult))
        for g in range(4):
            d_t = wd[ndq % 2]
            ndq += 1
            k.dma("pool", d_t, d_t[:, :, :], W["expert_w_down"],
                  W["expert_w_down"][l, e, :, g * 512:(g + 1) * 512].rearrange("(k p) n -> p k n", p=128))
            gs = slice(g * 512, (g + 1) * 512)
            for it in range(nt):
                pd = psd[nd % 3]
                nd += 1
                for jc in range(8):
                    _mm(k, pd, pd[:, :], he[jc], he[jc][:, it * 128:(it + 1) * 128], d_t, d_t[:, jc, :], jc == 0, jc == 7)
                ya = yacc[it]
                if e == 0:
                    k.op("dve", [pd, comb], [ya], lambda: nc.vector.tensor_scalar(out=ya[:, gs], in0=pd[:, :],
                                                                                  scalar1=comb[:, it, e:e + 1], scalar2=None,
                                                                                  op0=ALU.mult))
                else:
                    k.op("dve", [pd, comb, ya], [ya], lambda: nc.vector.scalar_tensor_tensor(
                        out=ya[:, gs], in0=pd[:, :], scalar=comb[:, it, e:e + 1], in1=ya[:, gs], op0=ALU.mult, op1=ALU.add))
    rows = sorted(set(1 if i < 2 else 0 for i in tiles))
    gf = {}
    for r in rows:
        gf[r] = _bc_load(k, "gf%d" % r, W["mod"], W["mod"][l, r:r + 1, 10240:12288], D)
    xo = [k.sbuf("exo%d" % i, [128, D], F32) for i in range(1)]
    for it, i in enumerate(tiles):
        r = 1 if i < 2 else 0
        xb = xo[0]
        ya = yacc[it]
        k.dma("sp", xb, xb[:, :], W["xres"], W["xres"][i * 128:(i + 1) * 128, :])
        k.op("pool", [ya, gf[r]], [ya], lambda: nc.gpsimd.tensor_tensor(out=ya[:, :], in0=ya[:, :], in1=gf[r][:, :], op=ALU.mult))
        k.op("dve", [xb, ya], [xb], lambda: nc.vector.tensor_tensor(out=xb[:, :], in0=xb[:, :], in1=ya[:, :], op=ALU.add))
        db, dap = out_dst(i)
        k.dma("sp", db, dap, xb, xb[:, :])
    k.release(m0)


DRAM_SPECS = {
    "x": ([S, D], F32, "in"), "ctx": ([LC, D], F32, "in"), "cvec": ([2, D], F32, "in"),
    "norm1_w": ([DEPTH, D], F32, "in"), "norm2_w": ([DEPTH, D], F32, "in"),
    "ada_w": ([DEPTH, D, 6 * D], F32, "in"), "ada_b": ([DEPTH, 6 * D], F32, "in"),
    "w_in": ([DEPTH, D, DIN], F32, "in"), "w_out": ([DEPTH, D, D], F32, "in"),
    "gla_gate_w": ([DEPTH, 2, 16, 256], F32, "in"), "gla_gate_b": ([DEPTH, 512], F32, "in"),
    "gla_norm_w": ([DEPTH, 64], F32, "in"),
    "swa_q_norm_w": ([DEPTH, 64], F32, "in"), "swa_k_norm_w": ([DEPTH, 64], F32, "in"), "swa_sink": ([DEPTH, 8], F32, "in"),
    "hyena_conv_w": ([DEPTH, 3, 1536], F32, "in"), "hyena_conv_b": ([DEPTH, 1536], F32, "in"),
    "hyena_ffn_w1": ([DEPTH, 33, 64], F32, "in"), "hyena_ffn_b1": ([DEPTH, 64], F32, "in"),
    "hyena_ffn_w2": ([DEPTH, 64, 64], F32, "in"), "hyena_ffn_b2": ([DEPTH, 64], F32, "in"),
    "hyena_ffn_w3": ([DEPTH, 64, 2048], F32, "in"), "hyena_ffn_freq": ([DEPTH, 2, 64], F32, "in"),
    "hyena_bias": ([DEPTH, 2, 512], F32, "in"),
    "diff_q_norm_w": ([DEPTH, 64], F32, "in"), "diff_k_norm_w": ([DEPTH, 64], F32, "in"),
    "diff_lambda": ([DEPTH, 256], F32, "in"), "diff_subln_w": ([DEPTH, 128], F32, "in"),
    "router_w": ([D, NE], F32, "in"), "router_bias": ([1, NE], F32, "in"),
    "expert_w_gate": ([DEPTH, NE, D, DFF], F32, "in"), "expert_w_up": ([DEPTH, NE, D, DFF], F32, "in"),
    "expert_w_down": ([DEPTH, NE, DFF, D], F32, "in"),
    "rope_cos": ([S, 64], F32, "in"), "rope_sin": ([S, 64], F32, "in"),
    "dftc": ([17, 128, 17, 128], BF16, "in"), "dfts": ([17, 128, 17, 128], BF16, "in"),
    "dftc_s": ([3, 128, 3, 128], BF16, "in"), "dfts_s": ([3, 128, 3, 128], BF16, "in"),
    "hy_zTh": ([33, S], BF16, "in"), "hy_zTh_s": ([33, LC], BF16, "in"),
    "hy_zTl": ([33, S], BF16, "in"), "hy_zTl_s": ([33, LC], BF16, "in"),
    "hy_decay": ([S, 512], F32, "in"), "hy_decay_s": ([LC, 512], F32, "in"),
    "hy_wf": ([128, 17], F32, "in"), "hy_wf_s": ([128, 3], F32, "in"),
    "mod": ([DEPTH, 2, 6 * D], F32, "tmp"), "u": ([T, DIN], BF16, "tmp"), "mix": ([T, D], BF16, "tmp"),
    "xres": ([T, D], F32, "tmp"), "fT": ([128, 16, T], BF16, "tmp"), "comb": ([T, NE], F32, "tmp"),
    "out": ([S, D], F32, "out"),
}


class DramSet:
    def __init__(self, k, ext_in=(), ext_out=()):
        self.k = k
        self.ext_in = set(ext_in)
        self.ext_out = set(ext_out)
        self.bufs = {}
        self.inputs = []
        self.outputs = []

    def __getitem__(self, name):
        if name not in self.bufs:
            shape, dt, kind = DRAM_SPECS[name]
            if name in self.ext_in:
                kind = "in"
            elif name in self.ext_out:
                kind = "out"
            kk = {"in": "ExternalInput", "out": "ExternalOutput", "tmp": "Internal"}[kind]
            self.bufs[name] = self.k.dram(name, shape, dt, kind=kk)
            if kind == "in":
                self.inputs.append(name)
            if kind == "out":
                self.outputs.append(name)
        return self.bufs[name]


def new_program(ext_in=(), ext_out=()):
    nc = bass.Bass("TRN2", target_bir_lowering=False)
    k = K(nc)
    cx = Ctx()
    W = DramSet(k, ext_in, ext_out)
    make_consts(k, cx)
    return nc, k, cx, W


def end_program(k, W):
    k.finish([W.bufs[n] for n in W.outputs])
    k.close()


def host_consts():
    hc = {}
    t = np.arange(S)
    row = (t // 64).astype(np.float32)
    col = (t % 64).astype(np.float32)
    inv = (np.float32(10000.0) ** (-np.arange(16, dtype=np.float32) / np.float32(16))).astype(np.float32)
    ar = row[:, None] * inv
    ac = col[:, None] * inv
    hc["rope_cos"] = np.concatenate([np.cos(ar), np.cos(ar), np.cos(ac), np.cos(ac)], 1).astype(np.float32)
    hc["rope_sin"] = np.concatenate([-np.sin(ar), np.sin(ar), -np.sin(ac), np.sin(ac)], 1).astype(np.float32)
    for (L, sfx) in ((S, ""), (LC, "_s")):
        nb = L // 128 + 1
        n = nb * 128
        idx = np.arange(n, dtype=np.int64)
        ang = (2.0 * np.pi / (2 * L)) * ((idx[:, None] * idx[None, :]) % (2 * L)).astype(np.float64)
        valid = (idx[:, None] <= L) & (idx[None, :] <= L)
        for nm, tab in (("dftc", np.cos(ang)), ("dfts", np.sin(ang))):
            tab = np.where(valid, tab, 0.0)
            blk = tab.reshape(nb, 128, nb, 128).transpose(2, 1, 0, 3)
            hc[nm + sfx] = np.ascontiguousarray(blk).astype(ml_dtypes.bfloat16)
        tt = np.linspace(0.0, 1.0, L, dtype=np.float32)[:, None]
        w = (np.float32(2.0 * math.pi / L) * np.arange(L, dtype=np.float32))[:, None]
        bands = np.linspace(1e-4, 15, 16, dtype=np.float32)
        z = np.concatenate([tt, np.cos(w * bands), -np.sin(w * bands)], axis=-1).astype(np.float32)
        zT = np.ascontiguousarray(z.T)
        zh = zT.astype(ml_dtypes.bfloat16)
        hc["hy_zTh" + sfx] = zh
        hc["hy_zTl" + sfx] = (zT - zh.astype(np.float32)).astype(ml_dtypes.bfloat16)
        dmin = math.log(100.0) / 1.5
        dmax = math.log(100.0) / 0.3
        deltas = np.linspace(dmin, dmax, 512, dtype=np.float32)
        hc["hy_decay" + sfx] = np.exp(-tt * deltas).astype(np.float32)
        wf = np.zeros((128, nb), np.float32)
        f = np.arange(n)
        wv = np.where((f == 0) | (f == L), 1.0 / (2 * L), np.where(f < L, 1.0 / L, 0.0))
        hc["hy_wf" + sfx] = np.ascontiguousarray(wv.reshape(nb, 128).T).astype(np.float32)
    return hc


def _qk_prep(k, cx, W, pz, i, c0, G, out_t):
    nc = k.nc
    n = 64 * G
    raw, sq, xn, t2, ss, rs, wbc, cs, sn = pz["raw"], pz["sq"], pz["xn"], pz["t2"], pz["ss"], pz["rs"], pz["wbc"], pz["cs"], pz["sn"]
    k.dma("sp", raw, raw[:, 0:n], W["u"], W["u"][i * 128:(i + 1) * 128, c0:c0 + n])
    k.op("act", [raw], [sq], lambda: nc.scalar.activation(out=sq[:, 0:n], in_=raw[:, 0:n], func=AF.Square))
    k.op("dve", [sq], [ss], lambda: nc.vector.tensor_reduce(out=ss[:, 0:G], in_=sq[:, 0:n].rearrange("p (g d) -> p g d", d=64),
                                                            axis=AX.X, op=ALU.add))
    _rsqrt(k, rs, rs[:, 0:G], ss, ss[:, 0:G], EPS, scale=1.0 / 64)
    k.op("dve", [raw, rs], [xn], lambda: nc.vector.tensor_tensor(
        out=xn[:, 0:n].rearrange("p (g d) -> p g d", d=64), in0=raw[:, 0:n].rearrange("p (g d) -> p g d", d=64),
        in1=rs[:, 0:G].unsqueeze(2).broadcast_to([128, G, 64]), op=ALU.mult))
    if i < 2:
        k.op("pool", [xn, wbc], [out_t], lambda: nc.gpsimd.tensor_tensor(out=out_t[:, 0:n], in0=xn[:, 0:n], in1=wbc[:, 0:n],
                                                                         op=ALU.mult))
        return
    k.op("pool", [xn, wbc], [xn], lambda: nc.gpsimd.tensor_tensor(out=xn[:, 0:n], in0=xn[:, 0:n], in1=wbc[:, 0:n], op=ALU.mult))
    t = i - 2
    k.dma("sp", cs, cs[:, :], W["rope_cos"], W["rope_cos"][t * 128:(t + 1) * 128, :])
    k.dma("sp", sn, sn[:, :], W["rope_sin"], W["rope_sin"][t * 128:(t + 1) * 128, :])
    xv = xn[:, 0:n].rearrange("p (g a b d) -> p g a b d", a=2, b=2, d=16)
    tv = t2[:, 0:n].rearrange("p (g a b d) -> p g a b d", a=2, b=2, d=16)
    sv = sn[:, :].rearrange("p (a b d) -> p a b d", a=2, b=2)
    for b in range(2):
        k.op("dve", [xn, sn], [t2], lambda: nc.vector.tensor_tensor(
            out=tv[:, :, :, b, :], in0=xv[:, :, :, 1 - b, :],
            in1=sv[:, :, b, :].unsqueeze(1).broadcast_to([128, G, 2, 16]), op=ALU.mult))
    k.op("pool", [xn, cs], [xn], lambda: nc.gpsimd.tensor_tensor(
        out=xn[:, 0:n].rearrange("p (g d) -> p g d", d=64), in0=xn[:, 0:n].rearrange("p (g d) -> p g d", d=64),
        in1=cs[:, :].unsqueeze(1).broadcast_to([128, G, 64]), op=ALU.mult))
    k.op("dve", [xn, t2], [out_t], lambda: nc.vector.tensor_tensor(out=out_t[:, 0:n], in0=xn[:, 0:n], in1=t2[:, 0:n], op=ALU.add))


def _prep_alloc(k, nmax, wbc=None):
    G = nmax // 64
    return {"raw": k.sbuf("pz_raw", [128, nmax], BF16), "sq": k.sbuf("pz_sq", [128, nmax], F32),
            "xn": k.sbuf("pz_xn", [128, nmax], F32), "t2": k.sbuf("pz_t2", [128, nmax], F32),
            "ss": k.sbuf("pz_ss", [128, G], F32), "rs": k.sbuf("pz_rs", [128, G], F32),
            "wbc": wbc if wbc is not None else k.sbuf("pz_wbc", [128, nmax], F32), "cs": k.sbuf("pz_cs", [128, 64], F32),
            "sn": k.sbuf("pz_sn", [128, 64], F32)}


def stage_diff(k, cx, W, l, update_ctx):
    nc = k.nc
    lam_init = 0.8 - 0.6 * math.exp(-0.3 * l)
    m0 = k.mark()
    qT = k.sbuf("d_qT", [128, 4, T], BF16)
    kT = k.sbuf("d_kT", [128, 4, T], BF16)
    vext = k.sbuf("d_vext", [128, NT, 4, 129], BF16)
    k.op("pool", [], [vext], lambda: nc.gpsimd.memset(vext[:, :, :, :], 1.0))
    pz = _prep_alloc(k, 1024)
    pzs = [pz, _prep_alloc(k, 1024, wbc=pz["wbc"])]
    wv = pz["wbc"][:, :].rearrange("p (g d) -> p g d", d=64)
    k.dma("sp", pz["wbc"], wv[:, 0:8, :], W["diff_q_norm_w"], W["diff_q_norm_w"][l:l + 1, :].unsqueeze(1).broadcast_to([128, 8, 64]))
    k.dma("sp", pz["wbc"], wv[:, 8:16, :], W["diff_k_norm_w"], W["diff_k_norm_w"][l:l + 1, :].unsqueeze(1).broadcast_to([128, 8, 64]))
    qk = [k.sbuf("d_qk%d" % i, [128, 1024], BF16) for i in range(2)]
    tps = [k.psum("d_tp%d" % i, [128, 4, 128], BF16) for i in range(2)]
    dl = _bc_load(k, "d_dl", W["diff_lambda"], W["diff_lambda"][l:l + 1, :], 256)
    sm = k.sbuf("d_sm", [128, 16], F32)
    pr = k.sbuf("d_pr", [128, 128], F32)
    dlv = dl[:, :].rearrange("p (a b d) -> p a b d", a=2, b=2)
    k.op("dve", [dl], [pr], lambda: nc.vector.tensor_tensor(out=pr[:, :].rearrange("p (a d) -> p a d", a=2), in0=dlv[:, :, 0, :],
                                                            in1=dlv[:, :, 1, :], op=ALU.mult))
    k.op("dve", [pr], [sm], lambda: nc.vector.tensor_reduce(out=sm[:, 0:2], in_=pr[:, :].rearrange("p (a d) -> p a d", a=2),
                                                            axis=AX.X, op=ALU.add))
    k.op("act", [sm], [sm], lambda: nc.scalar.activation(out=sm[:, 2:4], in_=sm[:, 0:2], func=AF.Exp))
    k.op("dve", [sm], [sm], lambda: nc.vector.tensor_tensor(out=sm[:, 4:5], in0=sm[:, 2:3], in1=sm[:, 3:4], op=ALU.subtract))
    k.op("dve", [sm], [sm], lambda: nc.vector.tensor_scalar(out=sm[:, 5:6], in0=sm[:, 4:5], scalar1=lam_init, scalar2=None, op0=ALU.add))
    lam = sm[:, 5:6]
    wsub = _bc_load(k, "d_wsub", W["diff_subln_w"], W["diff_subln_w"][l:l + 1, :], 128)
    k.op("dve", [wsub], [wsub], lambda: nc.vector.tensor_scalar(out=wsub[:, :], in0=wsub[:, :], scalar1=1.0 - lam_init, scalar2=None,
                                                                op0=ALU.mult))
    for i in range(NT):
        o = qk[i % 2]
        _qk_prep(k, cx, W, pzs[i % 2], i, C_DQ, 16, o)
        k.dma("sp", vext, vext[:, i, :, 0:128], W["u"], W["u"][i * 128:(i + 1) * 128, C_DV:C_DV + 512].rearrange("p (h d) -> p h d", d=128))
        for half, dst in ((0, qT), (1, kT)):
            if half == 0 and i < 2 and not update_ctx:
                continue
            tp = tps[half]
            for h in range(4):
                _tr(k, tp, tp[:, h, :], o, o[:, half * 512 + h * 128: half * 512 + (h + 1) * 128], cx.ident, cx.ident[:, :])
            _copy(k, "act" if half else "dve", dst, dst[:, :, i * 128:(i + 1) * 128], tp, tp[:, :, :])
    pss = [k.psum("d_ps%d" % i, [128, 512], F32) for i in range(2)]
    accb = [k.psum("d_acc%d" % i, [128, 3, 129], F32) for i in range(3)]
    Et = [k.sbuf("d_E%d" % i, [128, 512], BF16) for i in range(3)]
    tt = k.sbuf("d_tt", [128, 128], F32)
    oo = k.sbuf("d_oo", [128, 128], F32)
    junk = k.sbuf("d_junk", [128, 128], F32)
    ob = [k.sbuf("d_ob%d" % i, [128, 128], BF16) for i in range(2)]
    fs = k.sbuf("d_fs", [128, 8], F32)

    def acc_ap(m, qt):
        a = m * 4 + qt
        return accb[a // 3], a // 3, a % 3

    groups = []
    if update_ctx:
        groups.append((0, 256, [0, 1]))
    for g in range(4):
        groups.append((256 + g * 512, 512, list(range(NT))))
    ne = 0
    nf = 0
    ngrp = 0
    accs = [[k.sbuf("d_accs%d_%d" % (a_, b_), [128, 3, 129], F32) for b_ in range(3)] for a_ in range(2)]
    for h in range(4):
        for (q0, qn, kts) in groups:
            nqt = qn // 128
            started = set()
            iters = [(ki, kt, m) for ki, kt in enumerate(kts) for m in range(2)]
            slots = {}

            def emit_s(n_):
                ki, kt, m = iters[n_]
                ps = pss[(ne + n_) % 2]
                E = Et[(ne + n_) % 3]
                slots[n_] = E
                _mm(k, ps, ps[:, 0:qn], kT, kT[m * 64:(m + 1) * 64, h, kt * 128:(kt + 1) * 128],
                    qT, qT[m * 64:(m + 1) * 64, h, q0:q0 + qn], True, True)
                k.op("act", [ps], [E], lambda: nc.scalar.activation(out=E[:, 0:qn], in_=ps[:, 0:qn], func=AF.Exp, scale=0.125))

            def emit_pv(n_):
                ki, kt, m = iters[n_]
                E = slots.pop(n_)
                for qt in range(nqt):
                    ab, bk, sl = acc_ap(m, qt)
                    st = bk not in started
                    started.add(bk)
                    _mm(k, ab, ab[:, sl, :], E, E[:, qt * 128:(qt + 1) * 128], vext, vext[:, kt, h, :], st, ki == len(kts) - 1)

            for n_ in range(len(iters) + 1):
                if n_ < len(iters):
                    emit_s(n_)
                if n_ >= 1:
                    emit_pv(n_ - 1)
            ne += len(iters)
            aset = accs[ngrp % 2]
            ngrp += 1
            for bk_ in sorted(started):
                _copy(k, "act" if bk_ % 2 else "dve", aset[bk_], aset[bk_][:, :, :], accb[bk_], accb[bk_][:, :, :])
            for qt in range(nqt):
                a0, b0_, s0 = acc_ap(0, qt)
                a1, b1_, s1 = acc_ap(1, qt)
                a0, a1 = aset[b0_], aset[b1_]
                v = nc.vector
                k.op("dve", [a0], [fs], lambda: v.reciprocal(out=fs[:, 0:1], in_=a0[:, s0, 128:129]))
                k.op("dve", [a1], [fs], lambda: v.reciprocal(out=fs[:, 1:2], in_=a1[:, s1, 128:129]))
                k.op("dve", [fs, sm], [fs], lambda: v.tensor_tensor(out=fs[:, 2:3], in0=fs[:, 1:2], in1=lam, op=ALU.mult))
                k.op("dve", [a1, fs], [tt], lambda: v.tensor_scalar(out=tt[:, :], in0=a1[:, s1, 0:128], scalar1=fs[:, 2:3], scalar2=None,
                                                                    op0=ALU.mult))
                k.op("dve", [a0, fs, tt], [oo], lambda: v.scalar_tensor_tensor(out=oo[:, :], in0=a0[:, s0, 0:128], scalar=fs[:, 0:1],
                                                                               in1=tt[:, :], op0=ALU.mult, op1=ALU.subtract))
                k.op("dve", [], [fs], lambda: v.memset(fs[:, 3:4], 0.0))
                k.op("act", [oo, fs], [junk, fs], lambda: nc.scalar.activation(out=junk[:, :], in_=oo[:, :], func=AF.Square,
                                                                              accum_out=fs[:, 3:4]))
                _rsqrt(k, fs, fs[:, 4:5], fs, fs[:, 3:4], EPS, scale=1.0 / 128)
                o_b = ob[nf % 2]
                nf += 1
                k.op("dve", [oo, fs, wsub], [o_b], lambda: v.scalar_tensor_tensor(out=o_b[:, :], in0=oo[:, :], scalar=fs[:, 4:5],
                                                                                  in1=wsub[:, :], op0=ALU.mult, op1=ALU.mult))
                r0 = q0 + qt * 128
                k.dma("sp", W["mix"], W["mix"][r0:r0 + 128, 1536 + h * 128:1536 + (h + 1) * 128], o_b, o_b[:, :])
    k.release(m0)


def stage_swa(k, cx, W, l, update_ctx):
    nc = k.nc
    m0 = k.mark()
    qT = k.sbuf("s_qT", [128, 4, T], BF16)
    kTd = k.sbuf("s_kTd", [128, 2, T], BF16)
    vext = k.sbuf("s_vext", [128, NT, 2, 65], BF16)
    k.op("pool", [], [vext], lambda: nc.gpsimd.memset(vext[:, :, :, :], 1.0))
    pz = _prep_alloc(k, 640)
    pzs = [pz, _prep_alloc(k, 640, wbc=pz["wbc"])]
    wv = pz["wbc"][:, :].rearrange("p (g d) -> p g d", d=64)
    k.dma("sp", pz["wbc"], wv[:, 0:8, :], W["swa_q_norm_w"], W["swa_q_norm_w"][l:l + 1, :].unsqueeze(1).broadcast_to([128, 8, 64]))
    k.dma("sp", pz["wbc"], wv[:, 8:10, :], W["swa_k_norm_w"], W["swa_k_norm_w"][l:l + 1, :].unsqueeze(1).broadcast_to([128, 2, 64]))
    qk = [k.sbuf("s_qk%d" % i, [128, 640], BF16) for i in range(2)]
    kdup = [k.sbuf("s_kdup%d" % i, [128, 256], BF16) for i in range(2)]
    tps = [k.psum("s_tp%d" % i, [128, 4, 128], BF16) for i in range(2)]
    mprev = k.sbuf("s_mprev", [128, 128], BF16)
    mnext = k.sbuf("s_mnext", [128, 128], BF16)
    for t_, cm, pat in ((mprev, 1, -1), (mnext, -1, 1)):
        k.op("pool", [], [t_], lambda: nc.gpsimd.memset(t_[:, :], 1.0))
        k.op("pool", [t_], [t_], lambda: nc.gpsimd.affine_select(out=t_[:, :], in_=t_[:, :], pattern=[[pat, 128]],
                                                                 compare_op=ALU.is_ge, fill=0.0, base=0, channel_multiplier=cm))
    esink = _bc_load(k, "s_esink", W["swa_sink"], W["swa_sink"][l:l + 1, :], 8)
    k.op("act", [esink], [esink], lambda: nc.scalar.activation(out=esink[:, :], in_=esink[:, :], func=AF.Exp))
    for i in range(NT):
        o = qk[i % 2]
        kd = kdup[i % 2]
        _qk_prep(k, cx, W, pzs[i % 2], i, C_SQ, 10, o)
        k.dma("sp", vext, vext[:, i, :, 0:64], W["u"], W["u"][i * 128:(i + 1) * 128, C_SV:C_SV + 128].rearrange("p (h d) -> p h d", d=64))
        k.op("pool", [o], [kd], lambda: nc.gpsimd.tensor_copy(
            out=kd[:, :].rearrange("p (j c d) -> p j c d", j=2, c=2),
            in_=o[:, 512:640].rearrange("p (j d) -> p j d", j=2).unsqueeze(2).broadcast_to([128, 2, 2, 64])))
        if not (i < 2 and not update_ctx):
            tp = tps[0]
            for c in range(4):
                _tr(k, tp, tp[:, c, :], o, o[:, c * 128:(c + 1) * 128], cx.ident, cx.ident[:, :])
            _copy(k, "dve", qT, qT[:, :, i * 128:(i + 1) * 128], tp, tp[:, :, :])
        tp = tps[1]
        for j in range(2):
            _tr(k, tp, tp[:, j, :], kd, kd[:, j * 128:(j + 1) * 128], cx.ident, cx.ident[:, :])
        _copy(k, "act", kTd, kTd[:, :, i * 128:(i + 1) * 128], tp, tp[:, 0:2, :])
    pss = [k.psum("s_ps%d" % i, [128, 512], F32) for i in range(2)]
    accb = [k.psum("s_acc%d" % i, [128, 4, 65], F32) for i in range(2)]
    Et = [k.sbuf("s_E%d" % i, [128, 256], BF16) for i in range(3)]
    den = k.sbuf("s_den", [128, 8], F32)
    ob = [k.sbuf("s_ob%d" % i, [128, 512], BF16) for i in range(2)]
    blocks = []
    if update_ctx:
        for n in range(2):
            blocks.append((n, [(0, None), (1, None)]))
    for n in range(16):
        kts = [(0, None), (1, None)]
        for d_, mk in ((-1, mprev), (0, None), (1, mnext)):
            if 0 <= n + d_ < 16:
                kts.append((2 + n + d_, mk))
        blocks.append((2 + n, kts))
    ne = 0
    for bi, (qi, kts) in enumerate(blocks):
        q0 = qi * 128
        started = set()
        iters = [(j, par, ki, kt, mk) for j in range(2) for par in range(2) for ki, (kt, mk) in enumerate(kts)]
        slots = {}

        def emit_s(n_):
            j, par, ki, kt, mk = iters[n_]
            ps = pss[(ne + n_) % 2]
            E = Et[(ne + n_) % 3]
            slots[n_] = E
            _mm(k, ps, ps[:, 0:256].rearrange("p (c q) -> p c q", c=2), kTd, kTd[par * 64:(par + 1) * 64, j, kt * 128:(kt + 1) * 128],
                qT, qT[par * 64:(par + 1) * 64, 2 * j:2 * j + 2, q0:q0 + 128], True, True)
            k.op("act", [ps], [E], lambda: nc.scalar.activation(out=E[:, :], in_=ps[:, 0:256], func=AF.Exp, scale=0.125))
            if mk is not None:
                k.op("dve", [E, mk], [E], lambda: nc.vector.tensor_tensor(
                    out=E[:, :].rearrange("p (c q) -> p c q", c=2), in0=E[:, :].rearrange("p (c q) -> p c q", c=2),
                    in1=mk[:, :].unsqueeze(1).broadcast_to([128, 2, 128]), op=ALU.mult))

        def emit_pv(n_):
            j, par, ki, kt, mk = iters[n_]
            E = slots.pop(n_)
            for c2 in range(2):
                h = 4 * j + 2 * c2 + par
                ab = accb[h // 4]
                st = (h // 4) not in started
                started.add(h // 4)
                _mm(k, ab, ab[:, h % 4, :], E, E[:, c2 * 128:(c2 + 1) * 128], vext, vext[:, kt, j, :], st, ki == len(kts) - 1)

        for n_ in range(len(iters) + 1):
            if n_ < len(iters):
                emit_s(n_)
            if n_ >= 1:
                emit_pv(n_ - 1)
        ne += len(iters)
        o_b = ob[bi % 2]
        for hb in range(2):
            ab = accb[hb]
            k.op("dve", [ab, esink], [den], lambda: nc.vector.tensor_tensor(out=den[:, hb * 4:(hb + 1) * 4], in0=ab[:, :, 64],
                                                                           in1=esink[:, hb * 4:(hb + 1) * 4], op=ALU.add))
            k.op("dve", [den], [den], lambda: nc.vector.reciprocal(out=den[:, hb * 4:(hb + 1) * 4], in_=den[:, hb * 4:(hb + 1) * 4]))
            k.op("dve", [ab, den], [o_b], lambda: nc.vector.tensor_tensor(
                out=o_b[:, hb * 256:(hb + 1) * 256].rearrange("p (h d) -> p h d", d=64), in0=ab[:, :, 0:64],
                in1=den[:, hb * 4:(hb + 1) * 4].unsqueeze(2).broadcast_to([128, 4, 64]), op=ALU.mult))
        k.dma("sp", W["mix"], W["mix"][q0:q0 + 128, 512:1024], o_b, o_b[:, :])
    k.release(m0)


def stage_gla(k, cx, W, l, update_ctx):
    nc = k.nc
    v_ = nc.vector
    m0 = k.mark()
    QS = math.log(32.0 ** -0.5)
    wbd32 = k.sbuf("g_wbd32", [32, 512], F32)
    wbd = k.sbuf("g_wbd", [32, 512], BF16)
    k.op("dve", [], [wbd32], lambda: v_.memset(wbd32[:, :], 0.0))
    k.dma("sp", wbd32, wbd32[0:16, 0:256], W["gla_gate_w"], W["gla_gate_w"][l, 0, :, :])
    k.dma("sp", wbd32, wbd32[16:32, 256:512], W["gla_gate_w"], W["gla_gate_w"][l, 1, :, :])
    k.op("dve", [wbd32], [wbd], lambda: v_.tensor_copy(out=wbd[:, :], in_=wbd32[:, :]))
    gb = _bc_load(k, "g_gb", W["gla_gate_b"], W["gla_gate_b"][l:l + 1, :], 512)
    tri = [k.sbuf("g_tri%d" % d, [128, 128], BF16) for d in range(2)]
    cmask = [k.sbuf("g_cm%d" % d, [128, 128], BF16) for d in range(2)]
    for d in range(2):
        pat, cm = ((1, -1), (-1, 1))[d]
        for t_, val in ((tri[d], -1.0 / 16), (cmask[d], 1.0)):
            k.op("pool", [], [t_], lambda: nc.gpsimd.memset(t_[:, :], val))
            k.op("pool", [t_], [t_], lambda: nc.gpsimd.affine_select(out=t_[:, :], in_=t_[:, :], pattern=[[pat, 128]],
                                                                     compare_op=ALU.is_ge, fill=0.0, base=0, channel_multiplier=cm))
    negcol = k.sbuf("g_negcol", [128, 1], BF16)
    k.op("pool", [], [negcol], lambda: nc.gpsimd.memset(negcol[:, :], -1.0 / 16))
    mbd = k.sbuf("g_mbd", [128, 4, 128], BF16)
    k.op("pool", [], [mbd], lambda: nc.gpsimd.memset(mbd[:, :, :], 1.0))
    k.op("pool", [mbd], [mbd], lambda: nc.gpsimd.affine_select(out=mbd[:, :, :], in_=mbd[:, :, :], pattern=[[-32, 4], [0, 128]],
                                                               compare_op=ALU.is_ge, fill=0.0, base=0, channel_multiplier=1))
    k.op("pool", [mbd], [mbd], lambda: nc.gpsimd.affine_select(out=mbd[:, :, :], in_=mbd[:, :, :], pattern=[[32, 4], [0, 128]],
                                                               compare_op=ALU.is_ge, fill=0.0, base=31, channel_multiplier=-1))
    out_tiles = list(range(NT)) if update_ctx else list(range(2, NT))
    ostore = [{i: k.sbuf("g_o%d_%d" % (d, i), [128, 512], F32) for i in out_tiles} for d in range(2)]
    B = []
    for d in range(2):
        b = {}
        b["qk"] = k.sbuf("g_qk%d" % d, [128, 512], BF16)
        b["v"] = k.sbuf("g_v%d" % d, [128, 512], BF16)
        b["a"] = k.sbuf("g_a%d" % d, [128, 32], BF16)
        b["aT"] = k.sbuf("g_aT%d" % d, [32, 128], BF16)
        b["zb"] = k.sbuf("g_zb%d" % d, [128, 256], F32)
        b["sp"] = k.sbuf("g_sp%d" % d, [128, 256], F32)
        b["sph"] = k.sbuf("g_sph%d" % d, [128, 256], BF16)
        b["spl"] = k.sbuf("g_spl%d" % d, [128, 256], BF16)
        b["eb"] = k.sbuf("g_eb%d" % d, [128, 256], F32)
        b["enb"] = k.sbuf("g_enb%d" % d, [128, 256], F32)
        b["qkt"] = k.sbuf("g_qkt%d" % d, [128, 512], BF16)
        b["qkT"] = k.sbuf("g_qkT%d" % d, [128, 4, 128], BF16)
        b["ebl"] = k.sbuf("g_ebl%d" % d, [128, 2], F32)
        b["Qbd"] = [k.sbuf("g_Qbd%d_%d" % (d, h), [128, 4, 128], BF16) for h in range(2)]
        b["Em"] = [k.sbuf("g_Em%d_%d" % (d, h), [128, 4, 128], BF16) for h in range(2)]
        b["tmp"] = k.sbuf("g_tmp%d" % d, [128, 256], F32)
        b["S32"] = [k.sbuf("g_S32_%d_%d" % (d, h), [128, 256], F32) for h in range(2)]
        b["Sbf"] = [k.sbuf("g_Sbf_%d_%d" % (d, h), [128, 256], BF16) for h in range(2)]
        b["pT"] = k.psum("g_pT%d" % d, [128, 8, 128], BF16)
        b["pZ"] = k.psum("g_pZ%d" % d, [128, 512], F32)
        b["pA"] = k.psum("g_pA%d" % d, [128, 512], F32)
        b["pO"] = k.psum("g_pO%d" % d, [128, 512], F32)
        for h in range(2):
            k.op("dve", [], [b["S32"][h]], lambda: v_.memset(b["S32"][h][:, :], 0.0))
            k.op("dve", [], [b["Sbf"][h]], lambda: v_.memset(b["Sbf"][h][:, :], 0.0))
        B.append(b)
    order = [list(range(NT)), [1, 0] + list(range(NT - 1, 1, -1))]
    for d in range(2):
        B[d]["pre"] = [{"qk": B[d]["qk"], "v": B[d]["v"], "qkt": B[d]["qkt"], "qkT": B[d]["qkT"], "ebl": B[d]["ebl"]},
                       {"qk": k.sbuf("g_qkB%d" % d, [128, 512], BF16), "v": k.sbuf("g_vB%d" % d, [128, 512], BF16),
                        "qkt": k.sbuf("g_qktB%d" % d, [128, 512], BF16), "qkT": k.sbuf("g_qkTB%d" % d, [128, 4, 128], BF16),
                        "ebl": k.sbuf("g_eblB%d" % d, [128, 2], F32)}]

    def pre(step, d):
        i = order[d][step]
        b = B[d]
        pb = b["pre"][step % 2]
        dc = slice(d * 256, (d + 1) * 256)
        rows = slice(i * 128, (i + 1) * 128)
        k.dma("sp", pb["qk"], pb["qk"][:, :], W["u"], W["u"][rows, C_GQ:C_GQ + 512])
        k.dma("sp", pb["v"], pb["v"][:, :], W["u"], W["u"][rows, C_GV:C_GV + 512])
        k.dma("sp", b["a"], b["a"][:, :], W["u"], W["u"][rows, C_GA:C_GA + 32])
        pT, pZ = b["pT"], b["pZ"]
        _tr(k, pT, pT[0:32, 4, :], b["a"], b["a"][:, :], cx.ident, cx.ident[:, :])
        _copy(k, "dve", b["aT"], b["aT"][:, :], pT, pT[0:32, 4, :])
        _mm(k, pZ, pZ[:, 0:256], b["aT"], b["aT"][:, :], wbd, wbd[:, dc], True, True)
        k.op("dve", [pZ, gb], [b["zb"]], lambda: v_.tensor_tensor(out=b["zb"][:, :], in0=pZ[:, 0:256], in1=gb[:, dc], op=ALU.add))
        k.op("act", [b["zb"]], [b["sp"]], lambda: nc.scalar.activation(out=b["sp"][:, :], in_=b["zb"][:, :], func=AF.Exp, scale=-1.0))
        k.op("act", [b["sp"]], [b["sp"]], lambda: nc.scalar.activation(out=b["sp"][:, :], in_=b["sp"][:, :], func=AF.Ln, bias=1.0))
        k.op("act", [b["sp"]], [b["sph"]], lambda: nc.scalar.copy(out=b["sph"][:, :], in_=b["sp"][:, :]))
        k.op("dve", [b["sp"], b["sph"]], [b["spl"]], lambda: v_.tensor_tensor(out=b["spl"][:, :], in0=b["sp"][:, :], in1=b["sph"][:, :],
                                                                              op=ALU.subtract))
        _mm(k, pZ, pZ[:, 0:256], tri[d], tri[d][:, :], b["sph"], b["sph"][:, :], True, False)
        _mm(k, pZ, pZ[:, 0:256], tri[d], tri[d][:, :], b["spl"], b["spl"][:, :], False, True)
        for hg in range(2):
            _mm(k, pZ, pZ[:, 256 + hg:257 + hg], b["sph"], b["sph"][:, hg * 128:(hg + 1) * 128], negcol, negcol[:, :], False, False)
            _mm(k, pZ, pZ[:, 256 + hg:257 + hg], b["spl"], b["spl"][:, hg * 128:(hg + 1) * 128], negcol, negcol[:, :], False, True)
        k.op("act", [pZ], [b["eb"]], lambda: nc.scalar.activation(out=b["eb"][:, :], in_=pZ[:, 0:256], func=AF.Exp, bias=QS))
        k.op("act", [pZ], [b["enb"]], lambda: nc.scalar.activation(out=b["enb"][:, :], in_=pZ[:, 0:256], func=AF.Exp, scale=-1.0))
        k.op("act", [pZ], [pb["ebl"]], lambda: nc.scalar.activation(out=pb["ebl"][:, :], in_=pZ[:, 256:258], func=AF.Exp))
        k.op("dve", [pb["qk"], b["eb"]], [pb["qkt"]], lambda: v_.tensor_tensor(out=pb["qkt"][:, 0:256], in0=pb["qk"][:, 0:256],
                                                                               in1=b["eb"][:, :], op=ALU.mult))
        k.op("pool", [pb["qk"], b["enb"]], [pb["qkt"]], lambda: nc.gpsimd.tensor_tensor(out=pb["qkt"][:, 256:512], in0=pb["qk"][:, 256:512],
                                                                                        in1=b["enb"][:, :], op=ALU.mult))
        for c in range(4):
            _tr(k, pT, pT[:, c, :], pb["qkt"], pb["qkt"][:, c * 128:(c + 1) * 128], cx.ident, cx.ident[:, :])
        _copy(k, "act", pb["qkT"], pb["qkT"][:, :, :], pT, pT[:, 0:4, :])

    def main(step, d):
        i = order[d][step]
        b = B[d]
        pb = b["pre"][step % 2]
        pA, pO = b["pA"], b["pO"]
        qkT, qkt, vv, ebl = pb["qkT"], pb["qkt"], pb["v"], pb["ebl"]
        need_out = i in ostore[d]
        for hg in range(2):
            Qbd = b["Qbd"][hg]
            Em = b["Em"][hg]
            S32 = b["S32"][hg]
            Sbf = b["Sbf"][hg]
            if need_out:
                k.op("pool", [qkT, mbd], [Qbd], lambda: nc.gpsimd.tensor_tensor(
                    out=Qbd[:, :, :], in0=qkT[:, hg, :].unsqueeze(1).broadcast_to([128, 4, 128]), in1=mbd[:, :, :], op=ALU.mult))
                _mm(k, pA, pA[:, :], qkT, qkT[:, 2 + hg, :], Qbd, Qbd[:, :, :].rearrange("p h t -> p (h t)"), True, True)
                k.op("dve", [pA, cmask[d]], [Em], lambda: v_.tensor_tensor(
                    out=Em[:, :, :], in0=pA[:, :].rearrange("p (h t) -> p h t", h=4),
                    in1=cmask[d][:, :].unsqueeze(1).broadcast_to([128, 4, 128]), op=ALU.mult))
                _mm(k, pO, pO[:, 0:256], qkT, qkT[:, hg, :], Sbf, Sbf[:, :], True, False)
                for h4 in range(4):
                    hh = hg * 4 + h4
                    _mm(k, pO, pO[:, h4 * 64:(h4 + 1) * 64], Em, Em[:, h4, :], vv, vv[:, hh * 64:(hh + 1) * 64], False, h4 == 3)
                os_ = ostore[d][i]
                _copy(k, "act", os_, os_[:, hg * 256:(hg + 1) * 256], pO, pO[:, 0:256])
            _mm(k, pO, pO[:, 256:512], qkt, qkt[:, 256 + hg * 128:256 + (hg + 1) * 128], vv, vv[:, hg * 256:(hg + 1) * 256],
                not need_out, True)
            k.op("dve", [pO, mbd], [b["tmp"]], lambda: v_.tensor_tensor(
                out=b["tmp"][:, :].rearrange("p (h v) -> p h v", h=4), in0=pO[:, 256:512].rearrange("p (h v) -> p h v", h=4),
                in1=mbd[:, :, 0:64], op=ALU.mult))
            k.op("dve", [b["tmp"], S32], [S32], lambda: v_.tensor_tensor(out=S32[:, :], in0=b["tmp"][:, :], in1=S32[:, :], op=ALU.add))
            k.op("dve", [S32, ebl], [S32], lambda: v_.tensor_scalar(out=S32[:, :], in0=S32[:, :], scalar1=ebl[:, hg:hg + 1],
                                                                    scalar2=None, op0=ALU.mult))
            k.op("act", [S32], [Sbf], lambda: nc.scalar.copy(out=Sbf[:, :], in_=S32[:, :]))

    for step in range(NT + 1):
        for d in range(2):
            if step < NT:
                pre(step, d)
        for d in range(2):
            if step >= 1:
                main(step - 1, d)
    gw = k.sbuf("g_gw", [128, 8, 64], F32)
    k.dma("sp", gw, gw[:, :, :], W["gla_norm_w"], W["gla_norm_w"][l:l + 1, :].unsqueeze(1).broadcast_to([128, 8, 64]))
    gt = [k.sbuf("g_gt%d" % i, [128, 512], BF16) for i in range(2)]
    sg = [k.sbuf("g_sg%d" % i, [128, 512], F32) for i in range(2)]
    sq = k.sbuf("g_sq", [128, 512], F32)
    ss = k.sbuf("g_ss", [128, 8], F32)
    rs = k.sbuf("g_rs", [128, 8], F32)
    ob = [k.sbuf("g_ob%d" % i, [128, 512], BF16) for i in range(2)]
    for n, i in enumerate(out_tiles):
        g_ = gt[n % 2]
        s_ = sg[n % 2]
        o_ = ob[n % 2]
        of, obk = ostore[0][i], ostore[1][i]
        k.dma("sp", g_, g_[:, :], W["u"], W["u"][i * 128:(i + 1) * 128, C_GG:C_GG + 512])
        k.op("act", [g_], [s_], lambda: nc.scalar.activation(out=s_[:, :], in_=g_[:, :], func=AF.Silu))
        k.op("dve", [of, obk], [of], lambda: v_.tensor_tensor(out=of[:, :], in0=of[:, :], in1=obk[:, :], op=ALU.add))
        k.op("act", [of], [sq], lambda: nc.scalar.activation(out=sq[:, :], in_=of[:, :], func=AF.Square))
        k.op("dve", [sq], [ss], lambda: v_.tensor_reduce(out=ss[:, :], in_=sq[:, :].rearrange("p (h d) -> p h d", d=64), axis=AX.X, op=ALU.add))
        _rsqrt(k, rs, rs[:, :], ss, ss[:, :], EPS, scale=1.0 / 64)
        k.op("dve", [of, rs], [of], lambda: v_.tensor_tensor(out=of[:, :].rearrange("p (h d) -> p h d", d=64),
                                                             in0=of[:, :].rearrange("p (h d) -> p h d", d=64),
                                                             in1=rs[:, :].unsqueeze(2).broadcast_to([128, 8, 64]), op=ALU.mult))
        k.op("pool", [of, gw], [of], lambda: nc.gpsimd.tensor_tensor(out=of[:, :], in0=of[:, :], in1=gw[:, :, :].rearrange("p h d -> p (h d)"),
                                                                     op=ALU.mult))
        k.op("dve", [of, s_], [o_], lambda: v_.tensor_tensor(out=o_[:, :], in0=of[:, :], in1=s_[:, :], op=ALU.mult))
        k.dma("sp", W["mix"], W["mix"][i * 128:(i + 1) * 128, 0:512], o_, o_[:, :])
    k.release(m0)


def _split_hl(k, src, shape, name):
    nc = k.nc
    hi = k.sbuf(name + "h", shape, BF16)
    lo = k.sbuf(name + "l", shape, BF16)
    k.op("dve", [src], [hi], lambda: nc.vector.tensor_copy(out=hi[:, :], in_=src[:, :]))
    k.op("dve", [src, hi], [lo], lambda: nc.vector.tensor_tensor(out=lo[:, :], in0=src[:, :], in1=hi[:, :], op=ALU.subtract))
    return hi, lo


def _sin_reduced(k, out_b, out_ap, arg_b, arg_ap, kf_b, kf_ap):
    nc = k.nc
    MAGIC = 12582912.0
    k.op("dve", [arg_b], [kf_b], lambda: nc.vector.tensor_scalar(out=kf_ap, in0=arg_ap, scalar1=1.0 / (2 * math.pi), scalar2=MAGIC,
                                                                 op0=ALU.mult, op1=ALU.add))
    k.op("dve", [kf_b], [kf_b], lambda: nc.vector.tensor_scalar(out=kf_ap, in0=kf_ap, scalar1=-MAGIC, scalar2=None, op0=ALU.add))
    k.op("dve", [kf_b, arg_b], [arg_b], lambda: nc.vector.scalar_tensor_tensor(out=arg_ap, in0=kf_ap, scalar=-2 * math.pi, in1=arg_ap,
                                                                              op0=ALU.mult, op1=ALU.add))
    k.op("act", [arg_b], [out_b], lambda: nc.scalar.activation(out=out_ap, in_=arg_ap, func=AF.Sin, scale=0.999999))


def _hyena_seq(k, cx, W, l, L, tile0, sfx):
    nc = k.nc
    v_ = nc.vector
    nt = L // 128
    nb = nt + 1
    PI = math.pi
    tabC, tabS = W["dftc" + sfx], W["dfts" + sfx]
    m0 = k.mark()
    G = k.sbuf("h_G", [128, nb, 1024], BF16)
    Bt = k.sbuf("h_B", [128, nb, 1024], BF16)
    cb_ = [k.sbuf("h_cblk%d" % i, [128, nb, 128], BF16) for i in range(2)]
    sb_ = [k.sbuf("h_sblk%d" % i, [128, nb, 128], BF16) for i in range(2)]
    nld = [0]

    def load_blk(bc):
        c_t = cb_[nld[0] % 2]
        s_t = sb_[nld[0] % 2]
        nld[0] += 1
        k.dma("sp", c_t, c_t[:, :, :], tabC, tabC[bc, :, :, :])
        k.dma("sp", s_t, s_t[:, :, :], tabS, tabS[bc, :, :, :])
        return c_t, s_t

    m1 = k.mark()
    col = k.sbuf("h_col", [64, 8], F32)
    for a_ in range(2):
        k.dma("sp", col, col[:, a_:a_ + 1], W["hyena_ffn_freq"], W["hyena_ffn_freq"][l, a_, :].rearrange("(m o) -> m o", o=1))
    k.dma("sp", col, col[:, 2:3], W["hyena_ffn_b1"], W["hyena_ffn_b1"][l, :].rearrange("(m o) -> m o", o=1))
    k.dma("sp", col, col[:, 3:4], W["hyena_ffn_b2"], W["hyena_ffn_b2"][l, :].rearrange("(m o) -> m o", o=1))
    k.op("dve", [col], [col], lambda: v_.tensor_tensor(out=col[:, 4:6], in0=col[:, 0:2], in1=col[:, 2:4], op=ALU.mult))
    w1 = k.sbuf("h_w1", [64, 64], F32)
    k.op("dve", [], [w1], lambda: v_.memset(w1[:, :], 0.0))
    k.dma("sp", w1, w1[0:33, :], W["hyena_ffn_w1"], W["hyena_ffn_w1"][l, :, :])
    w1h, w1l = _split_hl(k, w1, [64, 64], "h_w1")
    w2 = k.sbuf("h_w2", [64, 64], F32)
    k.dma("sp", w2, w2[:, :], W["hyena_ffn_w2"], W["hyena_ffn_w2"][l, :, :])
    w2h, w2l = _split_hl(k, w2, [64, 64], "h_w2")
    w3b = k.sbuf("h_w3b", [64, 2048], BF16)
    k.dma("pool", w3b, w3b[:, :], W["hyena_ffn_w3"], W["hyena_ffn_w3"][l, :, :])
    h2b = k.sbuf("h_h2b", [64, L], BF16)
    psA = [k.psum("h_psA%d" % i, [128, 512], F32) for i in range(2)]
    m1a = k.mark()
    zh = k.sbuf("h_zh", [64, L], BF16)
    zl = k.sbuf("h_zl", [64, L], BF16)
    k.op("pool", [], [zh], lambda: nc.gpsimd.memset(zh[:, :], 0.0))
    k.op("pool", [], [zl], lambda: nc.gpsimd.memset(zl[:, :], 0.0))
    k.dma("sp", zh, zh[0:33, :], W["hy_zTh" + sfx], W["hy_zTh" + sfx][:, :])
    k.dma("sp", zl, zl[0:33, :], W["hy_zTl" + sfx], W["hy_zTl" + sfx][:, :])
    h1 = k.sbuf("h_h1", [64, L], F32)
    h2 = k.sbuf("h_h2", [64, L], F32)
    arg = k.sbuf("h_arg", [64, 512], F32)
    kf = k.sbuf("h_kf", [64, 512], F32)
    nblk = max(1, L // 512)
    bw = min(512, L)
    for blk in range(nblk):
        cs = slice(blk * bw, (blk + 1) * bw)
        ps = psA[blk % 2]
        for n_, (a_, b_) in enumerate(((w1h, zh), (w1h, zl), (w1l, zh))):
            _mm(k, ps, ps[0:64, 0:bw], a_, a_[:, :], b_, b_[:, cs], n_ == 0, n_ == 2)
        k.op("dve", [ps, col], [arg], lambda: v_.tensor_scalar(out=arg[:, 0:bw], in0=ps[0:64, 0:bw], scalar1=col[:, 0:1], scalar2=col[:, 4:5],
                                                               op0=ALU.mult, op1=ALU.add))
        _sin_reduced(k, h1, h1[:, cs], arg, arg[:, 0:bw], kf, kf[:, 0:bw])
    h1h, h1l = _split_hl(k, h1, [64, L], "h_h1")
    for blk in range(nblk):
        cs = slice(blk * bw, (blk + 1) * bw)
        ps = psA[blk % 2]
        for n_, (a_, b_) in enumerate(((w2h, h1h), (w2h, h1l), (w2l, h1h))):
            _mm(k, ps, ps[0:64, 0:bw], a_, a_[:, :], b_, b_[:, cs], n_ == 0, n_ == 2)
        k.op("dve", [ps, col], [arg], lambda: v_.tensor_scalar(out=arg[:, 0:bw], in0=ps[0:64, 0:bw], scalar1=col[:, 1:2], scalar2=col[:, 5:6],
                                                               op0=ALU.mult, op1=ALU.add))
        _sin_reduced(k, h2, h2[:, cs], arg, arg[:, 0:bw], kf, kf[:, 0:bw])
    k.op("dve", [h2], [h2b], lambda: v_.tensor_copy(out=h2b[:, :], in_=h2[:, :]))
    k.release(m1a)
    hp = k.sbuf("h_hp", [128, nt, 1024], BF16)
    hm = k.sbuf("h_hm", [128, nt, 1024], BF16)
    dec = [k.sbuf("h_dec%d" % i, [128, 512], F32) for i in range(2)]
    hd = [k.sbuf("h_hd%d" % i, [128, 512], F32) for i in range(4)]
    hab = [k.sbuf("h_hab%d" % i, [128, 512], BF16) for i in range(2)]
    ones_c = k.sbuf("h_ones_c", [128, 128], BF16)
    rowm = k.sbuf("h_rowm", [128, 1], F32)
    k.op("pool", [], [ones_c], lambda: nc.gpsimd.memset(ones_c[:, :], 1.0))
    k.op("pool", [], [rowm], lambda: nc.gpsimd.memset(rowm[:, :], 1.0))
    k.op("pool", [rowm], [rowm], lambda: nc.gpsimd.affine_select(out=rowm[:, :], in_=rowm[:, :], pattern=[[0, 1]], compare_op=ALU.is_ge,
                                                                 fill=0.0, base=-1, channel_multiplier=1))
    psN = [k.psum("h_psN%d" % i, [128, 512], F32) for i in range(4)]
    na = 0
    for tc in range(nt):
        dt_ = dec[tc % 2]
        k.dma("sp", dt_, dt_[:, :], W["hy_decay" + sfx], W["hy_decay" + sfx][tc * 128:(tc + 1) * 128, :])
        for cg in range(4):
            ps = psA[cg % 2]
            _mm(k, ps, ps[:, :], h2b, h2b[:, tc * 128:(tc + 1) * 128], w3b, w3b[:, cg * 512:(cg + 1) * 512], True, True)
            k.op("dve", [ps, dt_], [hd[cg]], lambda: v_.tensor_tensor(out=hd[cg][:, :], in0=ps[:, :], in1=dt_[:, :], op=ALU.mult))
            ab = hab[na % 2]
            na += 1
            k.op("act", [hd[cg]], [ab], lambda: nc.scalar.activation(out=ab[:, :], in_=hd[cg][:, :], func=AF.Abs))
            _mm(k, psN[cg], psN[cg][:, :], ones_c, ones_c[:, :], ab, ab[:, :], tc == 0, tc == nt - 1)
            if tc == 0 and cg % 2 == 1:
                k.op("dve", [hd[cg], rowm], [hd[cg]], lambda: v_.tensor_scalar(out=hd[cg][:, :], in0=hd[cg][:, :], scalar1=rowm[:, 0:1],
                                                                               scalar2=None, op0=ALU.mult))
        for o in range(2):
            k.op("pool", [hd[2 * o], hd[2 * o + 1]], [hp], lambda: nc.gpsimd.tensor_tensor(
                out=hp[:, tc, o * 512:(o + 1) * 512], in0=hd[2 * o][:, :], in1=hd[2 * o + 1][:, :], op=ALU.add))
            k.op("pool", [hd[2 * o], hd[2 * o + 1]], [hm], lambda: nc.gpsimd.tensor_tensor(
                out=hm[:, tc, o * 512:(o + 1) * 512], in0=hd[2 * o][:, :], in1=hd[2 * o + 1][:, :], op=ALU.subtract))
    nr = k.sbuf("h_nr", [128, 2048], F32)
    for cg in range(4):
        _copy(k, "dve", nr, nr[:, cg * 512:(cg + 1) * 512], psN[cg], psN[cg][:, :])
    rnb = k.sbuf("h_rnb", [128, 1024], F32)
    nrv = nr[:, :].rearrange("p (o d c) -> p o d c", o=2, d=2)
    k.op("dve", [nr], [rnb], lambda: v_.tensor_tensor(out=rnb[:, :].rearrange("p (o c) -> p o c", o=2), in0=nrv[:, :, 0, :], in1=nrv[:, :, 1, :],
                                                      op=ALU.add))
    k.op("dve", [rnb], [rnb], lambda: v_.reciprocal(out=rnb[:, :], in_=rnb[:, :]))
    wf = k.sbuf("h_wf", [128, nb], F32)
    k.dma("sp", wf, wf[:, :], W["hy_wf" + sfx], W["hy_wf" + sfx][:, :])
    npz = 0
    for fb in range(nb):
        c_t, s_t = load_blk(fb)
        for o in range(2):
            os_ = slice(o * 512, (o + 1) * 512)
            for (tab, src, dst) in ((c_t, hp, G), (s_t, hm, Bt)):
                ps = psN[npz % 4]
                npz += 1
                for tc in range(nt):
                    _mm(k, ps, ps[:, :], tab, tab[:, tc, :], src, src[:, tc, os_], tc == 0, tc == nt - 1)
                k.op("dve", [ps, wf, rnb], [dst], lambda: v_.scalar_tensor_tensor(out=dst[:, fb, os_], in0=ps[:, :], scalar=wf[:, fb:fb + 1],
                                                                                in1=rnb[:, os_], op0=ALU.mult, op1=ALU.mult))
    k.release(m1)
    import os as _os
    if _os.environ.get("HY_STOP") == "1":
        k.release(m0)
        return
    y = k.sbuf("h_y", [128, nt, 512], BF16)
    x1 = k.sbuf("h_x1", [128, nt, 512], BF16)
    x2 = k.sbuf("h_x2", [128, nt, 512], BF16)
    m2 = k.mark()
    cw = k.sbuf("h_cw", [128, 3, 1536], F32)
    for j in range(3):
        k.dma("sp", cw, cw[:, j, :], W["hyena_conv_w"], W["hyena_conv_w"][l, j:j + 1, :].broadcast_to([128, 1536]))
    cbias = _bc_load(k, "h_cbias", W["hyena_conv_b"], W["hyena_conv_b"][l:l + 1, :], 1536)
    ut = [[k.sbuf("h_u%d_%d" % (a, i), [128, 1536], BF16) for a in range(3)] for i in range(2)]
    za = k.sbuf("h_za", [128, 1536], F32)
    zb = k.sbuf("h_zb", [128, 1536], F32)
    for j in range(nt):
        um, uc, up = ut[j % 2]
        r0 = (tile0 + j) * 128
        hy = slice(C_HY, C_HY + 1536)
        if j == 0:
            k.op("pool", [], [um], lambda: nc.gpsimd.memset(um[:, :], 0.0))
            k.dma("sp", um, um[1:128, :], W["u"], W["u"][r0:r0 + 127, hy])
        else:
            k.dma("sp", um, um[:, :], W["u"], W["u"][r0 - 1:r0 + 127, hy])
        k.dma("sp", uc, uc[:, :], W["u"], W["u"][r0:r0 + 128, hy])
        if j == nt - 1:
            k.op("pool", [], [up], lambda: nc.gpsimd.memset(up[:, :], 0.0))
            k.dma("sp", up, up[0:127, :], W["u"], W["u"][r0 + 1:r0 + 128, hy])
        else:
            k.dma("sp", up, up[:, :], W["u"], W["u"][r0 + 1:r0 + 129, hy])
        k.op("dve", [um, cw], [za], lambda: v_.tensor_tensor(out=za[:, :], in0=um[:, :], in1=cw[:, 0, :], op=ALU.mult))
        k.op("pool", [uc, cw], [zb], lambda: nc.gpsimd.tensor_tensor(out=zb[:, :], in0=uc[:, :], in1=cw[:, 1, :], op=ALU.mult))
        k.op("dve", [za, zb], [za], lambda: v_.tensor_tensor(out=za[:, :], in0=za[:, :], in1=zb[:, :], op=ALU.add))
        k.op("pool", [up, cw], [zb], lambda: nc.gpsimd.tensor_tensor(out=zb[:, :], in0=up[:, :], in1=cw[:, 2, :], op=ALU.mult))
        k.op("pool", [zb, cbias], [zb], lambda: nc.gpsimd.tensor_tensor(out=zb[:, :], in0=zb[:, :], in1=cbias[:, :], op=ALU.add))
        for (dst, c0, eng) in ((x1, 0, "dve"), (x2, 512, "pool"), (y, 1024, "dve")):
            e_ = v_ if eng == "dve" else nc.gpsimd
            k.op(eng, [za, zb], [dst], lambda: e_.tensor_tensor(out=dst[:, j, :], in0=za[:, c0:c0 + 512], in1=zb[:, c0:c0 + 512], op=ALU.add))
    k.release(m2)
    ReY = k.sbuf("h_ReY", [128, nb, 512], BF16)
    ImY = k.sbuf("h_ImY", [128, nb, 512], BF16)
    hbias = k.sbuf("h_hbias", [128, 2, 512], F32)
    for o in range(2):
        k.dma("sp", hbias, hbias[:, o, :], W["hyena_bias"], W["hyena_bias"][l, o:o + 1, :].broadcast_to([128, 512]))
    tq = [k.sbuf("h_tq%d" % i, [128, 512], F32) for i in range(4)]
    ob = [k.sbuf("h_ob%d" % i, [128, 512], BF16) for i in range(2)]
    psP = [k.psum("h_psP%d" % i, [128, 512], F32) for i in range(2)]
    psQ = [k.psum("h_psQ%d" % i, [128, 512], F32) for i in range(2)]
    psO = [k.psum("h_psO%d" % i, [128, 512], F32) for i in range(2)]
    for o in range(2):
        os_ = slice(o * 512, (o + 1) * 512)
        xg = x1 if o == 0 else x2
        for fb in range(nb):
            c_t, s_t = load_blk(fb)
            pP, pQ = psP[fb % 2], psQ[fb % 2]
            for tc in range(nt):
                _mm(k, pP, pP[:, :], c_t, c_t[:, tc, :], y, y[:, tc, :], tc == 0, tc == nt - 1)
            for tc in range(nt):
                _mm(k, pQ, pQ[:, :], s_t, s_t[:, tc, :], y, y[:, tc, :], tc == 0, tc == nt - 1)
            k.op("dve", [pP, G], [tq[0]], lambda: v_.tensor_tensor(out=tq[0][:, :], in0=pP[:, :], in1=G[:, fb, os_], op=ALU.mult))
            k.op("dve", [pQ, Bt], [tq[1]], lambda: v_.tensor_tensor(out=tq[1][:, :], in0=pQ[:, :], in1=Bt[:, fb, os_], op=ALU.mult))
            k.op("pool", [tq[0], tq[1]], [ReY], lambda: nc.gpsimd.tensor_tensor(out=ReY[:, fb, :], in0=tq[0][:, :], in1=tq[1][:, :], op=ALU.subtract))
            k.op("dve", [pP, Bt], [tq[2]], lambda: v_.tensor_tensor(out=tq[2][:, :], in0=pP[:, :], in1=Bt[:, fb, os_], op=ALU.mult))
            k.op("dve", [pQ, G], [tq[3]], lambda: v_.tensor_tensor(out=tq[3][:, :], in0=pQ[:, :], in1=G[:, fb, os_], op=ALU.mult))
            k.op("pool", [tq[2], tq[3]], [ImY], lambda: nc.gpsimd.tensor_tensor(out=ImY[:, fb, :], in0=tq[2][:, :], in1=tq[3][:, :], op=ALU.add))
        for tc in range(nt):
            c_t, s_t = load_blk(tc)
            pO = psO[tc % 2]
            n_ = 0
            for fb in range(nb):
                for (tab, src) in ((c_t, ReY), (s_t, ImY)):
                    _mm(k, pO, pO[:, :], tab, tab[:, fb, :], src, src[:, fb, :], n_ == 0, n_ == 2 * nb - 1)
                    n_ += 1
            t0_, t1_ = tq[0], tq[1]
            k.op("pool", [y, hbias], [t0_], lambda: nc.gpsimd.tensor_tensor(out=t0_[:, :], in0=y[:, tc, :], in1=hbias[:, o, :], op=ALU.mult))
            k.op("dve", [pO, t0_], [t1_], lambda: v_.tensor_tensor(out=t1_[:, :], in0=pO[:, :], in1=t0_[:, :], op=ALU.add))
            if o == 0:
                k.op("pool", [xg, t1_], [y], lambda: nc.gpsimd.tensor_tensor(out=y[:, tc, :], in0=xg[:, tc, :], in1=t1_[:, :], op=ALU.mult))
            else:
                o_b = ob[tc % 2]
                k.op("pool", [xg, t1_], [o_b], lambda: nc.gpsimd.tensor_tensor(out=o_b[:, :], in0=xg[:, tc, :], in1=t1_[:, :], op=ALU.mult))
                r0 = (tile0 + tc) * 128
                k.dma("sp", W["mix"], W["mix"][r0:r0 + 128, 1024:1536], o_b, o_b[:, :])
    k.release(m0)


def stage_hyena(k, cx, W, l, update_ctx):
    if update_ctx:
        _hyena_seq(k, cx, W, l, LC, 0, "_s")
    _hyena_seq(k, cx, W, l, S, 2, "")


def build_full():
    nc, k, cx, W = new_program()
    stage_mod(k, cx, W)
    for l in range(DEPTH):
        upd = l < DEPTH - 1
        if l == 0:
            def xsrc(i):
                if i < 2:
                    return W["ctx"], W["ctx"][i * 128:(i + 1) * 128, :]
                return W["x"], W["x"][(i - 2) * 128:(i - 1) * 128, :]
        else:
            def xsrc(i):
                return W["xres"], W["xres"][i * 128:(i + 1) * 128, :]
        stage_inproj(k, cx, W, l, xsrc)
        stage_gla(k, cx, W, l, upd)
        stage_swa(k, cx, W, l, upd)
        stage_hyena(k, cx, W, l, upd)
        stage_diff(k, cx, W, l, upd)
        if upd:
            tiles = list(range(NT))
            passes = [list(range(0, 9)), list(range(9, 18))]
            dst = lambda i: (W["xres"], W["xres"][i * 128:(i + 1) * 128, :])
        else:
            tiles = list(range(2, NT))
            passes = [list(range(2, 10)), list(range(10, 18))]
            dst = lambda i: (W["out"], W["out"][(i - 2) * 128:(i - 1) * 128, :])
        stage_outproj_norm_router(k, cx, W, l, xsrc, tiles)
        for tl in passes:
            stage_experts(k, cx, W, l, tl, dst)
    end_program(k, W)
    return nc, k, W


_CACHE = {}


def kernel(**inputs):
    if "prog" not in _CACHE:
        _CACHE["prog"] = build_full()
        _CACHE["hc"] = host_consts()
    nc, k, W = _CACHE["prog"]
    hc = _CACHE["hc"]
    f32 = lambda a: np.ascontiguousarray(np.asarray(a, dtype=np.float32))
    shared = {}
    for n in W.inputs:
        if n in ("x", "ctx", "cvec"):
            continue
        if n in hc:
            shared[n] = hc[n]
        elif n == "router_bias":
            shared[n] = f32(inputs[n]).reshape(1, NE)
        elif n == "gla_gate_b":
            shared[n] = f32(inputs[n]).reshape(DEPTH, 512)
        elif n == "diff_lambda":
            shared[n] = f32(inputs[n]).reshape(DEPTH, 256)
        else:
            shared[n] = f32(inputs[n])
    x = f32(inputs["x"])
    ctx = f32(inputs["ctx"])
    c = f32(inputs["c"])
    c_ctx = f32(inputs["c_ctx"])
    nb = x.shape[0]
    in_maps = []
    for b in range(nb):
        m = dict(shared)
        m["x"] = x[b]
        m["ctx"] = ctx[b]
        m["cvec"] = np.ascontiguousarray(np.stack([c[b], c_ctx]))
        in_maps.append(m)
    res = run_bass_kernel_spmd(nc, in_maps, core_ids=list(range(nb)))
    return np.stack([np.asarray(r["out"], dtype=np.float32) for r in res.results])
```

```python
import math
import numpy as np
import ml_dtypes
import concourse.bass as bass
import concourse.mybir as mybir
from concourse.bass_utils import run_bass_kernel_spmd

F32 = mybir.dt.float32
BF16 = mybir.dt.bfloat16
ALU = mybir.AluOpType
AF = mybir.ActivationFunctionType
AX = mybir.AxisListType

D = 2048
S = 2048
LC = 256
T = S + LC
NT = T // 128
DIN = 5408
DEPTH = 2
EPS = 1e-6
NE = 16
DFF = 1024
C_GQ, C_GK, C_GV, C_GG, C_GA = 0, 256, 512, 1024, 1536
C_SQ, C_SK, C_SV = 1568, 2080, 2208
C_HY = 2336
C_DQ, C_DK, C_DV = 3872, 4384, 4896


class Buf:
    __slots__ = ("t", "name", "w", "r")

    def __init__(self, t, name=""):
        self.t = t
        self.name = name
        self.w = {}
        self.r = {}

    def __getitem__(self, idx):
        return self.t[idx]


class K:
    def __init__(self, nc, n_dma_sems=48):
        self.nc = nc
        self.engs = {"pe": nc.tensor, "act": nc.scalar, "dve": nc.vector, "pool": nc.gpsimd, "sp": nc.sync}
        self._ctx = []
        self.sems = {}
        self.count = {}
        self.semobj = {}
        for n in self.engs:
            self.sems[n] = self._sem("e_" + n)
            self.count[n] = 0
            self.semobj[("e", n)] = self.sems[n]
        self.dma_sems = []
        for i in range(n_dma_sems):
            s = self._sem("d%d" % i)
            self.dma_sems.append([s, 0])
            self.semobj[("d", i)] = s
        self.dma_rr = 0
        self.waited = {n: {} for n in self.engs}
        self.pending = {}
        self.epoch = {}
        self.n_inst = 0
        self.n_wait = 0

    def _sem(self, name):
        cm = self.nc.semaphore(name)
        s = cm.__enter__()
        self._ctx.append((cm, None))
        return s

    def _newbuf(self, t, name):
        b = Buf(t, name)
        b.r = dict(self.pending)
        return b

    def sbuf(self, name, shape, dtype):
        self.n_alloc = getattr(self, "n_alloc", 0) + 1
        name = "%s_%d" % (name, self.n_alloc)
        cm = self.nc.sbuf_tensor(name, list(shape), dtype)
        t = cm.__enter__()
        b = self._newbuf(t, name)
        self._ctx.append((cm, b))
        return b

    def psum(self, name, shape, dtype=F32):
        self.n_alloc = getattr(self, "n_alloc", 0) + 1
        name = "%s_%d" % (name, self.n_alloc)
        cm = self.nc.psum_tensor(name, list(shape), dtype)
        t = cm.__enter__()
        b = self._newbuf(t, name)
        self._ctx.append((cm, b))
        return b

    def dram(self, name, shape, dtype, kind="Internal"):
        t = self.nc.dram_tensor(name, list(shape), dtype, kind=kind)
        return Buf(t.ap(), name)

    def mark(self):
        return len(self._ctx)

    def release(self, mark):
        while len(self._ctx) > mark:
            cm, b = self._ctx.pop()
            if b is not None:
                for evs in (b.w, b.r):
                    for key, (val, en) in evs.items():
                        if self.pending.get(key, (0, ""))[0] < val:
                            self.pending[key] = (val, "rel")
            cm.__exit__(None, None, None)

    def _need(self, eng, reads, writes):
        need = {}
        is_dma = eng.startswith("dma:")

        def add(evs, same_ok, skip_dma=False):
            for key, (val, en) in evs.items():
                if en == eng and same_ok:
                    continue
                if skip_dma and en == "dma":
                    continue
                if need.get(key, 0) < val:
                    need[key] = val

        for b in reads:
            add(b.w, False)
        for b in writes:
            add(b.w, True, skip_dma=is_dma)
            add(b.r, True)
        return need

    def _do_waits(self, eng, need):
        e = self.engs[eng]
        wd = self.waited[eng]
        for key, val in need.items():
            if wd.get(key, 0) >= val:
                continue
            e.wait_ge(self.semobj[key], val)
            wd[key] = val
            self.n_wait += 1

    def op(self, eng, reads, writes, fn):
        need = self._need(eng, reads, writes)
        self._do_waits(eng, need)
        if self.count[eng] >= 30000:
            self.epoch[eng] = self.epoch.get(eng, 0) + 1
            self.sems[eng] = self._sem("e_%s_%d" % (eng, self.epoch[eng]))
            self.count[eng] = 0
            self.semobj[("e", eng, self.epoch[eng])] = self.sems[eng]
        ins = fn()
        self.count[eng] += 1
        ins.then_inc(self.sems[eng], 1)
        ep = self.epoch.get(eng, 0)
        key = ("e", eng) if ep == 0 else ("e", eng, ep)
        ev = (self.count[eng], eng)
        for b in reads:
            b.r[key] = ev
        for b in writes:
            b.w = {key: ev}
            b.r = {}
        self.n_inst += 1
        return ins

    def dma(self, queue, out_buf, out_ap, in_buf, in_ap, **kw):
        i = self.dma_rr
        self.dma_rr = (self.dma_rr + 1) % len(self.dma_sems)
        d = self.dma_sems[i]
        key = ("d", i)
        need = self._need("dma:" + queue, [in_buf], [out_buf])
        if d[1] > 0 and need.get(key, 0) < d[1]:
            need[key] = d[1]
        self._do_waits(queue, need)
        ins = self.engs[queue].dma_start(out=out_ap, in_=in_ap, **kw)
        d[1] += 16
        ins.then_inc(d[0], 16)
        ev = (d[1], "dma")
        in_buf.r[key] = ev
        if out_buf.r or any(en != "dma" for (_, en) in out_buf.w.values()):
            out_buf.w = {}
        out_buf.w[key] = ev
        out_buf.r = {}
        self.n_inst += 1
        return ins

    def finish(self, out_bufs):
        need = {}
        for b in out_bufs:
            for key, (val, _) in b.w.items():
                if need.get(key, 0) < val:
                    need[key] = val
        self._do_waits("sp", need)
        need2 = {}
        for n in self.engs:
            if n != "sp" and self.count[n] > 0:
                ep = self.epoch.get(n, 0)
                need2[("e", n) if ep == 0 else ("e", n, ep)] = self.count[n]
        self._do_waits("sp", need2)
        for i, d in enumerate(self.dma_sems):
            if d[1] > 0:
                self._do_waits("sp", {("d", i): d[1]})

    def close(self):
        self.release(0)


class Ctx:
    pass


def _mm(k, out_b, out_ap, l_b, l_ap, r_b, r_ap, start, stop):
    nc = k.nc
    return k.op("pe", [l_b, r_b], [out_b], lambda: nc.tensor.matmul(out_ap, lhsT=l_ap, rhs=r_ap, start=start, stop=stop))


def _tr(k, out_b, out_ap, in_b, in_ap, ident_b, ident_ap):
    nc = k.nc
    return k.op("pe", [in_b, ident_b], [out_b], lambda: nc.tensor.transpose(out_ap, in_ap, ident_ap))


def _copy(k, eng, out_b, out_ap, in_b, in_ap):
    nc = k.nc
    if eng == "act":
        return k.op("act", [in_b], [out_b], lambda: nc.scalar.copy(out=out_ap, in_=in_ap))
    e = nc.vector if eng == "dve" else nc.gpsimd
    return k.op(eng, [in_b], [out_b], lambda: e.tensor_copy(out=out_ap, in_=in_ap))


def _rsqrt(k, out_b, out_ap, in_b, in_ap, eps, scale=1.0):
    nc = k.nc
    k.op("act", [in_b], [out_b], lambda: nc.scalar.activation(out=out_ap, in_=in_ap, func=AF.Sqrt, bias=eps, scale=scale))
    k.op("dve", [out_b], [out_b], lambda: nc.vector.reciprocal(out=out_ap, in_=out_ap))


def _rsqrt_ln(k, out_b, out_ap, in_b, in_ap, eps, scale=1.0):
    nc = k.nc
    k.op("act", [in_b], [out_b], lambda: nc.scalar.activation(out=out_ap, in_=in_ap, func=AF.Ln, bias=eps, scale=scale))
    k.op("act", [out_b], [out_b], lambda: nc.scalar.activation(out=out_ap, in_=out_ap, func=AF.Exp, scale=-0.5))


def make_consts(k, cx):
    nc = k.nc
    cx.ident = k.sbuf("ident", [128, 128], BF16)
    cx.ident32 = k.sbuf("ident32", [128, 128], F32)
    for t in (cx.ident, cx.ident32):
        k.op("pool", [], [t], lambda: nc.gpsimd.memset(t[:, :], 1.0))
        k.op("pool", [t], [t], lambda: nc.gpsimd.affine_select(out=t[:, :], in_=t[:, :], pattern=[[-1, 128]],
                                                               compare_op=ALU.is_equal, fill=0.0, base=0,
                                                               channel_multiplier=1))


def stage_mod(k, cx, W):
    nc = k.nc
    m0 = k.mark()
    cv = k.sbuf("cv", [128, 2, 16], F32)
    csT = k.sbuf("csT", [128, 16, 2], BF16)
    for r in range(2):
        k.dma("sp", cv, cv[:, r, :], W["cvec"], W["cvec"][r, :].rearrange("(p k) -> p k", k=16))
    k.op("act", [cv], [csT], lambda: nc.scalar.activation(out=csT[:, :, :].rearrange("p k r -> p r k"), in_=cv[:, :, :],
                                                           func=AF.Silu))
    wb = [k.sbuf("adaw%d" % i, [128, 16, 512], BF16) for i in range(2)]
    bb = [k.sbuf("adab%d" % i, [2, 512], F32) for i in range(2)]
    ob = [k.sbuf("modo%d" % i, [2, 512], F32) for i in range(2)]
    ps = [k.psum("modps%d" % i, [128, 512], F32) for i in range(2)]
    it = 0
    for l in range(DEPTH):
        for n in range(24):
            s = it % 2
            it += 1
            wt = wb[s]
            k.dma("pool", wt, wt[:, :, :], W["ada_w"],
                  W["ada_w"][l, :, n * 512:(n + 1) * 512].rearrange("(p k) n -> p k n", k=16))
            k.dma("sp", bb[s], bb[s][:, :], W["ada_b"], W["ada_b"][l:l + 1, n * 512:(n + 1) * 512].broadcast_to([2, 512]))
            for kk in range(16):
                _mm(k, ps[s], ps[s][0:2, :], csT, csT[:, kk, :], wt, wt[:, kk, :], kk == 0, kk == 15)
            k.op("dve", [ps[s], bb[s]], [ob[s]], lambda: nc.vector.tensor_tensor(out=ob[s][:, :], in0=ps[s][0:2, :],
                                                                                  in1=bb[s][:, :], op=ALU.add))
            k.dma("sp", W["mod"], W["mod"][l, :, n * 512:(n + 1) * 512], ob[s], ob[s][:, :])
    k.release(m0)


def _bc_load(k, name, src_b, src_ap_row, n, dtype=F32, parts=128, queue="sp"):
    t = k.sbuf(name, [parts, n], dtype)
    k.dma(queue, t, t[:, :], src_b, src_ap_row.broadcast_to([parts, n]))
    return t


def stage_inproj(k, cx, W, l, xsrc):
    nc = k.nc
    m0 = k.mark()
    wsc = []
    shb = []
    for r in range(2):
        a = _bc_load(k, "wsc%d" % r, W["mod"], W["mod"][l, r:r + 1, 2048:4096], D)
        b = _bc_load(k, "shb%d" % r, W["mod"], W["mod"][l, r:r + 1, 0:2048], D)
        wsc.append(a)
        shb.append(b)
    m1 = k.mark()
    nw = _bc_load(k, "nw", W["norm1_w"], W["norm1_w"][l:l + 1, :], D)
    for r in range(2):
        a = wsc[r]
        k.op("dve", [a, nw], [a], lambda: nc.vector.scalar_tensor_tensor(out=a[:, :], in0=a[:, :], scalar=1.0, in1=nw[:, :],
                                                                         op0=ALU.add, op1=ALU.mult))
        k.op("act", [a], [a], lambda: nc.scalar.mul(out=a[:, :], in_=a[:, :], mul=math.sqrt(D)))
    k.release(m1)
    hT = [k.sbuf("hT%d" % i, [128, 16, 128], BF16) for i in range(NT)]
    xt = [k.sbuf("xt%d" % i, [128, D], F32) for i in range(2)]
    hb = [k.sbuf("hb%d" % i, [128, D], BF16) for i in range(2)]
    junk = k.sbuf("junk", [128, D], BF16)
    ss = k.sbuf("ss", [128, NT], F32)
    rs = k.sbuf("rs", [128, NT], F32)
    tps = [k.psum("tp%d" % i, [128, 4, 128], BF16) for i in range(2)]
    pss = [k.psum("pps%d" % i, [128, 512], F32) for i in range(4)]
    wbuf = [k.sbuf("winb%d" % i, [128, 16, 512], BF16) for i in range(2)]
    uos = [k.sbuf("uo%d" % i, [128, 512], BF16) for i in range(4)]
    k.op("dve", [], [ss], lambda: nc.vector.memset(ss[:, :], 0.0))
    def load_w(g):
        c0 = g * 512
        cw = min(512, DIN - c0)
        wt = wbuf[g % 2]
        k.dma("pool", wt, wt[:, :, 0:cw], W["w_in"], W["w_in"][l, :, c0:c0 + cw].rearrange("(k p) n -> p k n", p=128))
    load_w(0)
    for i in range(NT):
        xb = xt[i % 2]
        h = hb[i % 2]
        sb, sap = xsrc(i)
        k.dma("sp", xb, xb[:, :], sb, sap)
        r = 1 if i < 2 else 0
        k.op("act", [xb], [junk, ss], lambda: nc.scalar.activation(out=junk[:, :], in_=xb[:, :], func=AF.Square,
                                                                    accum_out=ss[:, i:i + 1]))
        _rsqrt(k, rs, rs[:, i:i + 1], ss, ss[:, i:i + 1], EPS * D)
        k.op("dve", [xb, rs, wsc[r]], [xb], lambda: nc.vector.scalar_tensor_tensor(out=xb[:, :], in0=xb[:, :],
                                                                                   scalar=rs[:, i:i + 1], in1=wsc[r][:, :],
                                                                                   op0=ALU.mult, op1=ALU.mult))
        k.op("pool", [xb, shb[r]], [h], lambda: nc.gpsimd.tensor_tensor(out=h[:, :], in0=xb[:, :], in1=shb[r][:, :],
                                                                         op=ALU.add))
        for kq in range(4):
            tp = tps[(4 * i + kq) % 2]
            for j in range(4):
                kk = 4 * kq + j
                _tr(k, tp, tp[:, j, :], h, h[:, kk * 128:(kk + 1) * 128], cx.ident, cx.ident[:, :])
            _copy(k, "act" if kq % 2 else "dve", hT[i], hT[i][:, 4 * kq:4 * kq + 4, :], tp, tp[:, :, :])
    n = 0
    for g in range(11):
        c0 = g * 512
        cw = min(512, DIN - c0)
        wt = wbuf[g % 2]
        if g + 1 < 11:
            load_w(g + 1)
        for i in range(NT):
            ps = pss[n % 4]
            uo = uos[n % 4]
            for kk in range(16):
                _mm(k, ps, ps[:, 0:cw], hT[i], hT[i][:, kk, :], wt, wt[:, kk, 0:cw], kk == 0, kk == 15)
            _copy(k, "act" if n % 2 else "dve", uo, uo[:, 0:cw], ps, ps[:, 0:cw])
            k.dma("sp", W["u"], W["u"][i * 128:(i + 1) * 128, c0:c0 + cw], uo, uo[:, 0:cw])
            n += 1
    k.release(m0)


def stage_outproj_norm_router(k, cx, W, l, xsrc, tiles):
    nc = k.nc
    m0 = k.mark()
    wo = k.sbuf("wo", [128, 16, D], BF16)
    for q in range(4):
        k.dma("pool", wo, wo[:, 4 * q:4 * q + 4, :], W["w_out"],
              W["w_out"][l, q * 512:(q + 1) * 512, :].rearrange("(k p) n -> p k n", p=128))
    rows = sorted(set(1 if i < 2 else 0 for i in tiles))
    ga = {}
    wsc = {}
    shf = {}
    for r in rows:
        ga[r] = _bc_load(k, "ga%d" % r, W["mod"], W["mod"][l, r:r + 1, 4096:6144], D)
        wsc[r] = _bc_load(k, "wscf%d" % r, W["mod"], W["mod"][l, r:r + 1, 8192:10240], D)
        shf[r] = _bc_load(k, "shf%d" % r, W["mod"], W["mod"][l, r:r + 1, 6144:8192], D)
    xt = [k.sbuf("oxt%d" % i, [128, D], F32) for i in range(2)]
    nw = xt[1]
    k.dma("sp", nw, nw[:, :], W["norm2_w"], W["norm2_w"][l:l + 1, :].broadcast_to([128, D]))
    for r in rows:
        a = wsc[r]
        k.op("dve", [a, nw], [a], lambda: nc.vector.scalar_tensor_tensor(out=a[:, :], in0=a[:, :], scalar=1.0, in1=nw[:, :],
                                                                         op0=ALU.add, op1=ALU.mult))
        k.op("act", [a], [a], lambda: nc.scalar.mul(out=a[:, :], in_=a[:, :], mul=math.sqrt(D)))
    rw = k.sbuf("rw", [128, 16, NE], F32)
    k.dma("sp", rw, rw[:, :, :], W["router_w"], W["router_w"].t.rearrange("(k p) e -> p k e", p=128))
    rwh = k.sbuf("rwh", [128, 16, NE], BF16)
    rwl = k.sbuf("rwl", [128, 16, NE], BF16)
    k.op("dve", [rw], [rwh], lambda: nc.vector.tensor_copy(out=rwh[:, :, :], in_=rw[:, :, :]))
    k.op("dve", [rw, rwh], [rwl], lambda: nc.vector.tensor_tensor(out=rwl[:, :, :], in0=rw[:, :, :], in1=rwh[:, :, :],
                                                                  op=ALU.subtract))
    rbias = _bc_load(k, "rbias", W["router_bias"], W["router_bias"][0:1, :], NE)
    mt = [k.sbuf("mt%d" % i, [128, D], BF16) for i in range(2)]
    mT = [k.sbuf("mT%d" % i, [128, 16, 128], BF16) for i in range(2)]
    ft_ = [k.sbuf("ft%d" % i, [128, D], F32) for i in range(2)]
    fh_ = [k.sbuf("fh%d" % i, [128, D], BF16) for i in range(2)]
    fl_ = [k.sbuf("fl%d" % i, [128, D], BF16) for i in range(2)]
    fTl_ = [k.sbuf("fTl%d" % i, [128, 16, 128], BF16) for i in range(2)]
    fTb = [k.sbuf("fTb%d" % i, [128, 16, 128], BF16) for i in range(2)]
    junk = k.sbuf("ojunk", [128, D], BF16)
    ss = k.sbuf("oss", [128, NT], F32)
    rs = k.sbuf("ors", [128, NT], F32)
    sm = k.sbuf("rsm", [128, 256], F32)
    tps = [k.psum("otp%d" % i, [128, 4, 128], BF16) for i in range(2)]
    pss = [k.psum("ops%d" % i, [128, 512], F32) for i in range(3)]
    psr = k.psum("opsr", [128, 512], F32)
    k.op("dve", [], [ss], lambda: nc.vector.memset(ss[:, :], 0.0))
    n = 0
    for ii, i in enumerate(tiles):
        r = 1 if i < 2 else 0
        xb = xt[ii % 2]
        m = mt[ii % 2]
        mTt = mT[ii % 2]
        ft, fh, fl, fTl = ft_[ii % 2], fh_[ii % 2], fl_[ii % 2], fTl_[ii % 2]
        sb, sap = xsrc(i)
        k.dma("sp", xb, xb[:, :], sb, sap)
        k.dma("sp", m, m[:, :], W["mix"], W["mix"][i * 128:(i + 1) * 128, :])
        for kq in range(4):
            tp = tps[kq % 2]
            for j in range(4):
                kk = 4 * kq + j
                _tr(k, tp, tp[:, j, :], m, m[:, kk * 128:(kk + 1) * 128], cx.ident, cx.ident[:, :])
            _copy(k, "act" if kq % 2 else "dve", mTt, mTt[:, 4 * kq:4 * kq + 4, :], tp, tp[:, :, :])
        for g in range(4):
            ps = pss[n % 3]
            n += 1
            for kk in range(16):
                _mm(k, ps, ps[:, :], mTt, mTt[:, kk, :], wo, wo[:, kk, g * 512:(g + 1) * 512], kk == 0, kk == 15)
            gs = slice(g * 512, (g + 1) * 512)
            k.op("dve", [ps, ga[r]], [ft], lambda: nc.vector.tensor_tensor(out=ft[:, gs], in0=ps[:, :], in1=ga[r][:, gs],
                                                                          op=ALU.mult))
            k.op("pool", [xb, ft], [xb], lambda: nc.gpsimd.tensor_tensor(out=xb[:, gs], in0=xb[:, gs], in1=ft[:, gs],
                                                                         op=ALU.add))
        k.dma("sp", W["xres"], W["xres"][i * 128:(i + 1) * 128, :], xb, xb[:, :])
        k.op("act", [xb], [junk, ss], lambda: nc.scalar.activation(out=junk[:, :], in_=xb[:, :], func=AF.Square,
                                                                    accum_out=ss[:, i:i + 1]))
        _rsqrt(k, rs, rs[:, i:i + 1], ss, ss[:, i:i + 1], EPS * D)
        k.op("dve", [xb, rs, wsc[r]], [ft], lambda: nc.vector.scalar_tensor_tensor(out=ft[:, :], in0=xb[:, :],
                                                                                   scalar=rs[:, i:i + 1], in1=wsc[r][:, :],
                                                                                   op0=ALU.mult, op1=ALU.mult))
        k.op("pool", [ft, shf[r]], [ft], lambda: nc.gpsimd.tensor_tensor(out=ft[:, :], in0=ft[:, :], in1=shf[r][:, :],
                                                                          op=ALU.add))
        k.op("act", [ft], [fh], lambda: nc.scalar.copy(out=fh[:, :], in_=ft[:, :]))
        k.op("dve", [ft, fh], [fl], lambda: nc.vector.tensor_tensor(out=fl[:, :], in0=ft[:, :], in1=fh[:, :], op=ALU.subtract))
        fb = fTb[ii % 2]
        nq = 0
        for (src, dst) in ((fh, fb), (fl, fTl)):
            for kq in range(4):
                tp = tps[nq % 2]
                nq += 1
                for j in range(4):
                    kk = 4 * kq + j
                    _tr(k, tp, tp[:, j, :], src, src[:, kk * 128:(kk + 1) * 128], cx.ident, cx.ident[:, :])
                _copy(k, "act" if kq % 2 else "dve", dst, dst[:, 4 * kq:4 * kq + 4, :], tp, tp[:, :, :])
        k.dma("sp", W["fT"], W["fT"][:, :, i * 128:(i + 1) * 128], fb, fb[:, :, :])
        terms = [(fb, rwh), (fTl, rwh), (fb, rwl)]
        nmm = 0
        for (fx_, w_) in terms:
            for kk in range(16):
                _mm(k, psr, psr[:, 0:NE], fx_, fx_[:, kk, :], w_, w_[:, kk, :], nmm == 0, nmm == 47)
                nmm += 1
        _router(k, cx, W, psr, sm, rbias, i)
    k.release(m0)


def _router(k, cx, W, psr, sm, rbias, i):
    nc = k.nc
    v = nc.vector
    lg = psr[:, 0:NE]
    mx, nmx, se, rse = sm[:, 0:1], sm[:, 1:2], sm[:, 2:3], sm[:, 3:4]
    best, top1, top2, den = sm[:, 4:5], sm[:, 5:6], sm[:, 6:7], sm[:, 7:8]
    e = sm[:, 16:32]
    probs = sm[:, 32:48]
    sel = sm[:, 48:64]
    gmax = sm[:, 64:68]
    gsel = sm[:, 68:72]
    pen = sm[:, 72:76]
    selm = sm[:, 80:96]
    m1 = sm[:, 96:112]
    selm2 = sm[:, 112:128]
    m2 = sm[:, 128:144]
    wu = sm[:, 144:160]
    comb = sm[:, 160:176]

    def dv(reads_ps, fn):
        return k.op("dve", [sm] + ([psr] if reads_ps else []) + [rbias], [sm], fn)

    dv(True, lambda: v.tensor_reduce(out=mx, in_=lg, axis=AX.X, op=ALU.max))
    dv(False, lambda: v.tensor_scalar(out=nmx, in0=mx, scalar1=-1.0, scalar2=None, op0=ALU.mult))
    dv(False, lambda: v.memset(se, 0.0))
    k.op("act", [psr, sm], [sm], lambda: nc.scalar.activation(out=e, in_=lg, func=AF.Exp, bias=nmx, scale=1.0, accum_out=se))
    dv(False, lambda: v.reciprocal(out=rse, in_=se))
    dv(False, lambda: v.tensor_scalar(out=probs, in0=e, scalar1=rse, scalar2=None, op0=ALU.mult))
    dv(False, lambda: v.tensor_tensor(out=sel, in0=probs, in1=rbias[:, :], op=ALU.add))
    dv(False, lambda: v.tensor_reduce(out=gmax, in_=sel.rearrange("p (g e) -> p g e", e=4), axis=AX.X, op=ALU.max))
    dv(False, lambda: v.tensor_reduce(out=best, in_=gmax, axis=AX.X, op=ALU.max))
    dv(False, lambda: v.tensor_scalar(out=gsel, in0=gmax, scalar1=best, scalar2=None, op0=ALU.is_ge))
    dv(False, lambda: v.tensor_scalar(out=pen, in0=gsel, scalar1=-1.0, scalar2=1e30, op0=ALU.add, op1=ALU.mult))
    dv(False, lambda: v.tensor_tensor(out=selm.rearrange("p (g e) -> p g e", e=4), in0=sel.rearrange("p (g e) -> p g e", e=4),
                                      in1=pen.unsqueeze(2).broadcast_to([128, 4, 4]), op=ALU.add))
    dv(False, lambda: v.tensor_reduce(out=top1, in_=selm, axis=AX.X, op=ALU.max))
    dv(False, lambda: v.tensor_scalar(out=m1, in0=selm, scalar1=top1, scalar2=None, op0=ALU.is_ge))
    dv(False, lambda: v.scalar_tensor_tensor(out=selm2, in0=m1, scalar=-1e30, in1=selm, op0=ALU.mult, op1=ALU.add))
    dv(False, lambda: v.tensor_reduce(out=top2, in_=selm2, axis=AX.X, op=ALU.max))
    dv(False, lambda: v.tensor_scalar(out=m2, in0=selm2, scalar1=top2, scalar2=None, op0=ALU.is_ge))
    dv(False, lambda: v.tensor_tensor(out=m2, in0=m2, in1=m1, op=ALU.add))
    dv(False, lambda: v.tensor_tensor(out=wu, in0=probs, in1=m2, op=ALU.mult))
    dv(False, lambda: v.tensor_reduce(out=den, in_=wu, axis=AX.X, op=ALU.add))
    dv(False, lambda: v.reciprocal(out=den, in_=den))
    dv(False, lambda: v.tensor_scalar(out=comb, in0=wu, scalar1=den, scalar2=None, op0=ALU.mult))
    k.dma("sp", W["comb"], W["comb"][i * 128:(i + 1) * 128, :], sm, comb)


def stage_experts(k, cx, W, l, tiles, out_dst):
    nc = k.nc
    m0 = k.mark()
    nt = len(tiles)
    ntok = nt * 128
    t0 = tiles[0] * 128
    assert tiles == list(range(tiles[0], tiles[0] + nt))
    if ntok % 512 == 0:
        tgs = [(a, 512) for a in range(0, ntok, 512)]
    else:
        tgs = [(a, 384) for a in range(0, ntok, 384)]
        assert ntok % 384 == 0
    fT = k.sbuf("efT", [128, 16, ntok], BF16)
    for q in range(4):
        k.dma("sp", fT, fT[:, 4 * q:4 * q + 4, :], W["fT"], W["fT"][:, 4 * q:4 * q + 4, t0:t0 + ntok])
    comb = k.sbuf("ecomb", [128, nt, NE], F32)
    k.dma("sp", comb, comb[:, :, :], W["comb"], W["comb"][t0:t0 + ntok, :].rearrange("(i p) e -> p i e", p=128))
    yacc = [k.sbuf("yacc%d" % i, [128, D], F32) for i in range(nt)]
    he = [k.sbuf("he%d" % j, [128, ntok], BF16) for j in range(8)]
    wg = [k.sbuf("wg%d" % i, [128, 16, 256], BF16) for i in range(2)]
    wu = [k.sbuf("wu%d" % i, [128, 16, 256], BF16) for i in range(2)]
    wd = [k.sbuf("wd%d" % i, [128, 8, 512], BF16) for i in range(2)]
    sil = [k.sbuf("sil%d" % i, [128, 512], BF16) for i in range(2)]
    psg = [k.psum("psg%d" % i, [128, 512], F32) for i in range(2)]
    psu = [k.psum("psu%d" % i, [128, 512], F32) for i in range(2)]
    psd = [k.psum("psd%d" % i, [128, 512], F32) for i in range(3)]
    ng = 0
    nd = 0
    nq = 0
    ndq = 0
    for e in range(NE):
        for q in range(4):
            g_t = wg[nq % 2]
            u_t = wu[nq % 2]
            nq += 1
            k.dma("pool", g_t, g_t[:, :, :], W["expert_w_gate"],
                  W["expert_w_gate"][l, e, :, q * 256:(q + 1) * 256].rearrange("(k p) n -> p k n", p=128))
            k.dma("pool", u_t, u_t[:, :, :], W["expert_w_up"],
                  W["expert_w_up"][l, e, :, q * 256:(q + 1) * 256].rearrange("(k p) n -> p k n", p=128))
            for c2 in range(2):
                jc = q * 2 + c2
                for (a, n) in tgs:
                    pg = psg[ng % 2]
                    pu = psu[ng % 2]
                    sl = sil[ng % 2]
                    ng += 1
                    for kk in range(16):
                        _mm(k, pg, pg[:, 0:n], g_t, g_t[:, kk, c2 * 128:(c2 + 1) * 128], fT, fT[:, kk, a:a + n], kk == 0, kk == 15)
                    for kk in range(16):
                        _mm(k, pu, pu[:, 0:n], u_t, u_t[:, kk, c2 * 128:(c2 + 1) * 128], fT, fT[:, kk, a:a + n], kk == 0, kk == 15)
                    k.op("act", [pg], [sl], lambda: nc.scalar.activation(out=sl[:, 0:n], in_=pg[:, 0:n], func=AF.Silu))
                    k.op("dve", [sl, pu], [he[jc]], lambda: nc.vector.tensor_tensor(out=he[jc][:, a:a + n], in0=sl[:, 0:n],
                                                                                    in1=pu[:, 0:n], op=ALU.mult))
        for g in range(4):
            d_t = wd[ndq % 2]
            ndq += 1
            k.dma("pool", d_t, d_t[:, :, :], W["expert_w_down"],
                  W["expert_w_down"][l, e, :, g * 512:(g + 1) * 512].rearrange("(k p) n -> p k n", p=128))
            gs = slice(g * 512, (g + 1) * 512)
            for it in range(nt):
                pd = psd[nd % 3]
                nd += 1
                for jc in range(8):
                    _mm(k, pd, pd[:, :], he[jc], he[jc][:, it * 128:(it + 1) * 128], d_t, d_t[:, jc, :], jc == 0, jc == 7)
                ya = yacc[it]
                if e == 0:
                    k.op("dve", [pd, comb], [ya], lambda: nc.vector.tensor_scalar(out=ya[:, gs], in0=pd[:, :],
                                                                                  scalar1=comb[:, it, e:e + 1], scalar2=None,
                                                                                  op0=ALU.mult))
                else:
                    k.op("dve", [pd, comb, ya], [ya], lambda: nc.vector.scalar_tensor_tensor(
                        out=ya[:, gs], in0=pd[:, :], scalar=comb[:, it, e:e + 1], in1=ya[:, gs], op0=ALU.mult, op1=ALU.add))
    rows = sorted(set(1 if i < 2 else 0 for i in tiles))
    gf = {}
    for r in rows:
        gf[r] = _bc_load(k, "gf%d" % r, W["mod"], W["mod"][l, r:r + 1, 10240:12288], D)
    xo = [k.sbuf("exo%d" % i, [128, D], F32) for i in range(1)]
    for it, i in enumerate(tiles):
        r = 1 if i < 2 else 0
        xb = xo[0]
        ya = yacc[it]
        k.dma("sp", xb, xb[:, :], W["xres"], W["xres"][i * 128:(i + 1) * 128, :])
        k.op("pool", [ya, gf[r]], [ya], lambda: nc.gpsimd.tensor_tensor(out=ya[:, :], in0=ya[:, :], in1=gf[r][:, :], op=ALU.mult))
        k.op("dve", [xb, ya], [xb], lambda: nc.vector.tensor_tensor(out=xb[:, :], in0=xb[:, :], in1=ya[:, :], op=ALU.add))
        db, dap = out_dst(i)
        k.dma("sp", db, dap, xb, xb[:, :])
    k.release(m0)


DRAM_SPECS = {
    "x": ([S, D], F32, "in"), "ctx": ([LC, D], F32, "in"), "cvec": ([2, D], F32, "in"),
    "norm1_w": ([DEPTH, D], F32, "in"), "norm2_w": ([DEPTH, D], F32, "in"),
    "ada_w": ([DEPTH, D, 6 * D], F32, "in"), "ada_b": ([DEPTH, 6 * D], F32, "in"),
    "w_in": ([DEPTH, D, DIN], F32, "in"), "w_out": ([DEPTH, D, D], F32, "in"),
    "gla_gate_w": ([DEPTH, 2, 16, 256], F32, "in"), "gla_gate_b": ([DEPTH, 512], F32, "in"),
    "gla_norm_w": ([DEPTH, 64], F32, "in"),
    "swa_q_norm_w": ([DEPTH, 64], F32, "in"), "swa_k_norm_w": ([DEPTH, 64], F32, "in"), "swa_sink": ([DEPTH, 8], F32, "in"),
    "hyena_conv_w": ([DEPTH, 3, 1536], F32, "in"), "hyena_conv_b": ([DEPTH, 1536], F32, "in"),
    "hyena_ffn_w1": ([DEPTH, 33, 64], F32, "in"), "hyena_ffn_b1": ([DEPTH, 64], F32, "in"),
    "hyena_ffn_w2": ([DEPTH, 64, 64], F32, "in"), "hyena_ffn_b2": ([DEPTH, 64], F32, "in"),
    "hyena_ffn_w3": ([DEPTH, 64, 2048], F32, "in"), "hyena_ffn_freq": ([DEPTH, 2, 64], F32, "in"),
    "hyena_bias": ([DEPTH, 2, 512], F32, "in"),
    "diff_q_norm_w": ([DEPTH, 64], F32, "in"), "diff_k_norm_w": ([DEPTH, 64], F32, "in"),
    "diff_lambda": ([DEPTH, 256], F32, "in"), "diff_subln_w": ([DEPTH, 128], F32, "in"),
    "router_w": ([D, NE], F32, "in"), "router_bias": ([1, NE], F32, "in"),
    "expert_w_gate": ([DEPTH, NE, D, DFF], F32, "in"), "expert_w_up": ([DEPTH, NE, D, DFF], F32, "in"),
    "expert_w_down": ([DEPTH, NE, DFF, D], F32, "in"),
    "rope_cos": ([S, 64], F32, "in"), "rope_sin": ([S, 64], F32, "in"),
    "dftc": ([17, 128, 17, 128], BF16, "in"), "dfts": ([17, 128, 17, 128], BF16, "in"),
    "dftc_s": ([3, 128, 3, 128], BF16, "in"), "dfts_s": ([3, 128, 3, 128], BF16, "in"),
    "hy_zTh": ([33, S], BF16, "in"), "hy_zTh_s": ([33, LC], BF16, "in"),
    "hy_zTl": ([33, S], BF16, "in"), "hy_zTl_s": ([33, LC], BF16, "in"),
    "hy_decay": ([S, 512], F32, "in"), "hy_decay_s": ([LC, 512], F32, "in"),
    "hy_wf": ([128, 17], F32, "in"), "hy_wf_s": ([128, 3], F32, "in"),
    "mod": ([DEPTH, 2, 6 * D], F32, "tmp"), "u": ([T, DIN], BF16, "tmp"), "mix": ([T, D], BF16, "tmp"),
    "xres": ([T, D], F32, "tmp"), "fT": ([128, 16, T], BF16, "tmp"), "comb": ([T, NE], F32, "tmp"),
    "out": ([S, D], F32, "out"),
}


class DramSet:
    def __init__(self, k, ext_in=(), ext_out=()):
        self.k = k
        self.ext_in = set(ext_in)
        self.ext_out = set(ext_out)
        self.bufs = {}
        self.inputs = []
        self.outputs = []

    def __getitem__(self, name):
        if name not in self.bufs:
            shape, dt, kind = DRAM_SPECS[name]
            if name in self.ext_in:
                kind = "in"
            elif name in self.ext_out:
                kind = "out"
            kk = {"in": "ExternalInput", "out": "ExternalOutput", "tmp": "Internal"}[kind]
            self.bufs[name] = self.k.dram(name, shape, dt, kind=kk)
            if kind == "in":
                self.inputs.append(name)
            if kind == "out":
                self.outputs.append(name)
        return self.bufs[name]


def new_program(ext_in=(), ext_out=()):
    nc = bass.Bass("TRN2", target_bir_lowering=False)
    k = K(nc)
    cx = Ctx()
    W = DramSet(k, ext_in, ext_out)
    make_consts(k, cx)
    return nc, k, cx, W


def end_program(k, W):
    k.finish([W.bufs[n] for n in W.outputs])
    k.close()


def host_consts():
    hc = {}
    t = np.arange(S)
    row = (t // 64).astype(np.float32)
    col = (t % 64).astype(np.float32)
    inv = (np.float32(10000.0) ** (-np.arange(16, dtype=np.float32) / np.float32(16))).astype(np.float32)
    ar = row[:, None] * inv
    ac = col[:, None] * inv
    hc["rope_cos"] = np.concatenate([np.cos(ar), np.cos(ar), np.cos(ac), np.cos(ac)], 1).astype(np.float32)
    hc["rope_sin"] = np.concatenate([-np.sin(ar), np.sin(ar), -np.sin(ac), np.sin(ac)], 1).astype(np.float32)
    for (L, sfx) in ((S, ""), (LC, "_s")):
        nb = L // 128 + 1
        n = nb * 128
        idx = np.arange(n, dtype=np.int64)
        ang = (2.0 * np.pi / (2 * L)) * ((idx[:, None] * idx[None, :]) % (2 * L)).astype(np.float64)
        valid = (idx[:, None] <= L) & (idx[None, :] <= L)
        for nm, tab in (("dftc", np.cos(ang)), ("dfts", np.sin(ang))):
            tab = np.where(valid, tab, 0.0)
            blk = tab.reshape(nb, 128, nb, 128).transpose(2, 1, 0, 3)
            hc[nm + sfx] = np.ascontiguousarray(blk).astype(ml_dtypes.bfloat16)
        tt = np.linspace(0.0, 1.0, L, dtype=np.float32)[:, None]
        w = (np.float32(2.0 * math.pi / L) * np.arange(L, dtype=np.float32))[:, None]
        bands = np.linspace(1e-4, 15, 16, dtype=np.float32)
        z = np.concatenate([tt, np.cos(w * bands), -np.sin(w * bands)], axis=-1).astype(np.float32)
        zT = np.ascontiguousarray(z.T)
        zh = zT.astype(ml_dtypes.bfloat16)
        hc["hy_zTh" + sfx] = zh
        hc["hy_zTl" + sfx] = (zT - zh.astype(np.float32)).astype(ml_dtypes.bfloat16)
        dmin = math.log(100.0) / 1.5
        dmax = math.log(100.0) / 0.3
        deltas = np.linspace(dmin, dmax, 512, dtype=np.float32)
        hc["hy_decay" + sfx] = np.exp(-tt * deltas).astype(np.float32)
        wf = np.zeros((128, nb), np.float32)
        f = np.arange(n)
        wv = np.where((f == 0) | (f == L), 1.0 / (2 * L), np.where(f < L, 1.0 / L, 0.0))
        hc["hy_wf" + sfx] = np.ascontiguousarray(wv.reshape(nb, 128).T).astype(np.float32)
    return hc


def _qk_prep(k, cx, W, pz, i, c0, G, out_t):
    nc = k.nc
    n = 64 * G
    raw, sq, xn, t2, ss, rs, wbc, cs, sn = pz["raw"], pz["sq"], pz["xn"], pz["t2"], pz["ss"], pz["rs"], pz["wbc"], pz["cs"], pz["sn"]
    k.dma("sp", raw, raw[:, 0:n], W["u"], W["u"][i * 128:(i + 1) * 128, c0:c0 + n])
    k.op("dve", [raw], [sq], lambda: nc.vector.tensor_tensor(out=sq[:, 0:n], in0=raw[:, 0:n], in1=raw[:, 0:n], op=ALU.mult))
    k.op("dve", [sq], [ss], lambda: nc.vector.tensor_reduce(out=ss[:, 0:G], in_=sq[:, 0:n].rearrange("p (g d) -> p g d", d=64),
                                                            axis=AX.X, op=ALU.add))
    _rsqrt_ln(k, rs, rs[:, 0:G], ss, ss[:, 0:G], EPS, scale=1.0 / 64)
    k.op("dve", [raw, rs], [xn], lambda: nc.vector.tensor_tensor(
        out=xn[:, 0:n].rearrange("p (g d) -> p g d", d=64), in0=raw[:, 0:n].rearrange("p (g d) -> p g d", d=64),
        in1=rs[:, 0:G].unsqueeze(2).broadcast_to([128, G, 64]), op=ALU.mult))
    if i < 2:
        k.op("pool", [xn, wbc], [out_t], lambda: nc.gpsimd.tensor_tensor(out=out_t[:, 0:n], in0=xn[:, 0:n], in1=wbc[:, 0:n],
                                                                         op=ALU.mult))
        return
    k.op("pool", [xn, wbc], [xn], lambda: nc.gpsimd.tensor_tensor(out=xn[:, 0:n], in0=xn[:, 0:n], in1=wbc[:, 0:n], op=ALU.mult))
    t = i - 2
    k.dma("sp", cs, cs[:, :], W["rope_cos"], W["rope_cos"][t * 128:(t + 1) * 128, :])
    k.dma("sp", sn, sn[:, :], W["rope_sin"], W["rope_sin"][t * 128:(t + 1) * 128, :])
    xv = xn[:, 0:n].rearrange("p (g a b d) -> p g a b d", a=2, b=2, d=16)
    tv = t2[:, 0:n].rearrange("p (g a b d) -> p g a b d", a=2, b=2, d=16)
    sv = sn[:, :].rearrange("p (a b d) -> p a b d", a=2, b=2)
    for b in range(2):
        k.op("dve", [xn, sn], [t2], lambda: nc.vector.tensor_tensor(
            out=tv[:, :, :, b, :], in0=xv[:, :, :, 1 - b, :],
            in1=sv[:, :, b, :].unsqueeze(1).broadcast_to([128, G, 2, 16]), op=ALU.mult))
    k.op("pool", [xn, cs], [xn], lambda: nc.gpsimd.tensor_tensor(
        out=xn[:, 0:n].rearrange("p (g d) -> p g d", d=64), in0=xn[:, 0:n].rearrange("p (g d) -> p g d", d=64),
        in1=cs[:, :].unsqueeze(1).broadcast_to([128, G, 64]), op=ALU.mult))
    k.op("dve", [xn, t2], [out_t], lambda: nc.vector.tensor_tensor(out=out_t[:, 0:n], in0=xn[:, 0:n], in1=t2[:, 0:n], op=ALU.add))


def _prep_alloc(k, nmax, wbc=None):
    G = nmax // 64
    return {"raw": k.sbuf("pz_raw", [128, nmax], BF16), "sq": k.sbuf("pz_sq", [128, nmax], F32),
            "xn": k.sbuf("pz_xn", [128, nmax], F32), "t2": k.sbuf("pz_t2", [128, nmax], F32),
            "ss": k.sbuf("pz_ss", [128, G], F32), "rs": k.sbuf("pz_rs", [128, G], F32),
            "wbc": wbc if wbc is not None else k.sbuf("pz_wbc", [128, nmax], F32), "cs": k.sbuf("pz_cs", [128, 64], F32),
            "sn": k.sbuf("pz_sn", [128, 64], F32)}


def stage_diff(k, cx, W, l, update_ctx):
    nc = k.nc
    lam_init = 0.8 - 0.6 * math.exp(-0.3 * l)
    m0 = k.mark()
    qT = k.sbuf("d_qT", [128, 4, T], BF16)
    kT = k.sbuf("d_kT", [128, 4, T], BF16)
    vext = k.sbuf("d_vext", [128, NT, 4, 129], BF16)
    k.op("pool", [], [vext], lambda: nc.gpsimd.memset(vext[:, :, :, :], 1.0))
    pz = _prep_alloc(k, 1024)
    pzs = [pz, _prep_alloc(k, 1024, wbc=pz["wbc"])]
    wv = pz["wbc"][:, :].rearrange("p (g d) -> p g d", d=64)
    k.dma("sp", pz["wbc"], wv[:, 0:8, :], W["diff_q_norm_w"], W["diff_q_norm_w"][l:l + 1, :].unsqueeze(1).broadcast_to([128, 8, 64]))
    k.dma("sp", pz["wbc"], wv[:, 8:16, :], W["diff_k_norm_w"], W["diff_k_norm_w"][l:l + 1, :].unsqueeze(1).broadcast_to([128, 8, 64]))
    qk = [k.sbuf("d_qk%d" % i, [128, 1024], BF16) for i in range(2)]
    tps = [k.psum("d_tp%d" % i, [128, 4, 128], BF16) for i in range(2)]
    dl = _bc_load(k, "d_dl", W["diff_lambda"], W["diff_lambda"][l:l + 1, :], 256)
    sm = k.sbuf("d_sm", [128, 16], F32)
    pr = k.sbuf("d_pr", [128, 128], F32)
    dlv = dl[:, :].rearrange("p (a b d) -> p a b d", a=2, b=2)
    k.op("dve", [dl], [pr], lambda: nc.vector.tensor_tensor(out=pr[:, :].rearrange("p (a d) -> p a d", a=2), in0=dlv[:, :, 0, :],
                                                            in1=dlv[:, :, 1, :], op=ALU.mult))
    k.op("dve", [pr], [sm], lambda: nc.vector.tensor_reduce(out=sm[:, 0:2], in_=pr[:, :].rearrange("p (a d) -> p a d", a=2),
                                                            axis=AX.X, op=ALU.add))
    k.op("act", [sm], [sm], lambda: nc.scalar.activation(out=sm[:, 2:4], in_=sm[:, 0:2], func=AF.Exp))
    k.op("dve", [sm], [sm], lambda: nc.vector.tensor_tensor(out=sm[:, 4:5], in0=sm[:, 2:3], in1=sm[:, 3:4], op=ALU.subtract))
    k.op("dve", [sm], [sm], lambda: nc.vector.tensor_scalar(out=sm[:, 5:6], in0=sm[:, 4:5], scalar1=lam_init, scalar2=None, op0=ALU.add))
    lam = sm[:, 5:6]
    wsub = _bc_load(k, "d_wsub", W["diff_subln_w"], W["diff_subln_w"][l:l + 1, :], 128)
    k.op("dve", [wsub], [wsub], lambda: nc.vector.tensor_scalar(out=wsub[:, :], in0=wsub[:, :], scalar1=1.0 - lam_init, scalar2=None,
                                                                op0=ALU.mult))
    for i in range(NT):
        o = qk[i % 2]
        _qk_prep(k, cx, W, pzs[i % 2], i, C_DQ, 16, o)
        k.dma("sp", vext, vext[:, i, :, 0:128], W["u"], W["u"][i * 128:(i + 1) * 128, C_DV:C_DV + 512].rearrange("p (h d) -> p h d", d=128))
        for half, dst in ((0, qT), (1, kT)):
            if half == 0 and i < 2 and not update_ctx:
                continue
            tp = tps[half]
            for h in range(4):
                _tr(k, tp, tp[:, h, :], o, o[:, half * 512 + h * 128: half * 512 + (h + 1) * 128], cx.ident, cx.ident[:, :])
            _copy(k, "act" if half else "dve", dst, dst[:, :, i * 128:(i + 1) * 128], tp, tp[:, :, :])
    pss = [k.psum("d_ps%d" % i, [128, 512], F32) for i in range(2)]
    accb = [k.psum("d_acc%d" % i, [128, 3, 129], F32) for i in range(3)]
    Et = [k.sbuf("d_E%d" % i, [128, 512], BF16) for i in range(3)]
    tt = k.sbuf("d_tt", [128, 128], F32)
    oo = k.sbuf("d_oo", [128, 128], F32)
    junk = k.sbuf("d_junk", [128, 128], F32)
    ob = [k.sbuf("d_ob%d" % i, [128, 128], BF16) for i in range(2)]
    fs = k.sbuf("d_fs", [128, 8], F32)

    def acc_ap(m, qt):
        a = m * 4 + qt
        return accb[a // 3], a // 3, a % 3

    groups = []
    if update_ctx:
        groups.append((0, 256, [0, 1]))
    for g in range(4):
        groups.append((256 + g * 512, 512, list(range(NT))))
    ne = 0
    nf = 0
    ngrp = 0
    accs = [[k.sbuf("d_accs%d_%d" % (a_, b_), [128, 3, 129], F32) for b_ in range(3)] for a_ in range(2)]
    for h in range(4):
        for (q0, qn, kts) in groups:
            nqt = qn // 128
            started = set()
            iters = [(ki, kt, m) for ki, kt in enumerate(kts) for m in range(2)]
            slots = {}

            def emit_s(n_):
                ki, kt, m = iters[n_]
                ps = pss[(ne + n_) % 2]
                E = Et[(ne + n_) % 3]
                slots[n_] = E
                _mm(k, ps, ps[:, 0:qn], kT, kT[m * 64:(m + 1) * 64, h, kt * 128:(kt + 1) * 128],
                    qT, qT[m * 64:(m + 1) * 64, h, q0:q0 + qn], True, True)
                k.op("act", [ps], [E], lambda: nc.scalar.activation(out=E[:, 0:qn], in_=ps[:, 0:qn], func=AF.Exp, scale=0.125))

            def emit_pv(n_):
                ki, kt, m = iters[n_]
                E = slots.pop(n_)
                for qt in range(nqt):
                    ab, bk, sl = acc_ap(m, qt)
                    st = bk not in started
                    started.add(bk)
                    _mm(k, ab, ab[:, sl, :], E, E[:, qt * 128:(qt + 1) * 128], vext, vext[:, kt, h, :], st, ki == len(kts) - 1)

            for n_ in range(len(iters) + 1):
                if n_ < len(iters):
                    emit_s(n_)
                if n_ >= 1:
                    emit_pv(n_ - 1)
            ne += len(iters)
            aset = accs[ngrp % 2]
            ngrp += 1
            for bk_ in sorted(started):
                _copy(k, "act" if bk_ % 2 else "dve", aset[bk_], aset[bk_][:, :, :], accb[bk_], accb[bk_][:, :, :])
            for qt in range(nqt):
                a0, b0_, s0 = acc_ap(0, qt)
                a1, b1_, s1 = acc_ap(1, qt)
                a0, a1 = aset[b0_], aset[b1_]
                v = nc.vector
                k.op("dve", [a0], [fs], lambda: v.reciprocal(out=fs[:, 0:1], in_=a0[:, s0, 128:129]))
                k.op("dve", [a1], [fs], lambda: v.reciprocal(out=fs[:, 1:2], in_=a1[:, s1, 128:129]))
                k.op("dve", [fs, sm], [fs], lambda: v.tensor_tensor(out=fs[:, 2:3], in0=fs[:, 1:2], in1=lam, op=ALU.mult))
                k.op("dve", [a1, fs], [tt], lambda: v.tensor_scalar(out=tt[:, :], in0=a1[:, s1, 0:128], scalar1=fs[:, 2:3], scalar2=None,
                                                                    op0=ALU.mult))
                k.op("dve", [a0, fs, tt], [oo], lambda: v.scalar_tensor_tensor(out=oo[:, :], in0=a0[:, s0, 0:128], scalar=fs[:, 0:1],
                                                                               in1=tt[:, :], op0=ALU.mult, op1=ALU.subtract))
                k.op("dve", [oo], [junk], lambda: v.tensor_tensor(out=junk[:, :], in0=oo[:, :], in1=oo[:, :], op=ALU.mult))
                k.op("dve", [junk], [fs], lambda: v.tensor_reduce(out=fs[:, 3:4], in_=junk[:, :], axis=AX.X, op=ALU.add))
                _rsqrt_ln(k, fs, fs[:, 4:5], fs, fs[:, 3:4], EPS, scale=1.0 / 128)
                o_b = ob[nf % 2]
                nf += 1
                k.op("dve", [oo, fs, wsub], [o_b], lambda: v.scalar_tensor_tensor(out=o_b[:, :], in0=oo[:, :], scalar=fs[:, 4:5],
                                                                                  in1=wsub[:, :], op0=ALU.mult, op1=ALU.mult))
                r0 = q0 + qt * 128
                k.dma("sp", W["mix"], W["mix"][r0:r0 + 128, 1536 + h * 128:1536 + (h + 1) * 128], o_b, o_b[:, :])
    k.release(m0)


def stage_swa(k, cx, W, l, update_ctx):
    nc = k.nc
    m0 = k.mark()
    qT = k.sbuf("s_qT", [128, 4, T], BF16)
    kTd = k.sbuf("s_kTd", [128, 2, T], BF16)
    vext = k.sbuf("s_vext", [128, NT, 2, 65], BF16)
    k.op("pool", [], [vext], lambda: nc.gpsimd.memset(vext[:, :, :, :], 1.0))
    pz = _prep_alloc(k, 640)
    pzs = [pz, _prep_alloc(k, 640, wbc=pz["wbc"])]
    wv = pz["wbc"][:, :].rearrange("p (g d) -> p g d", d=64)
    k.dma("sp", pz["wbc"], wv[:, 0:8, :], W["swa_q_norm_w"], W["swa_q_norm_w"][l:l + 1, :].unsqueeze(1).broadcast_to([128, 8, 64]))
    k.dma("sp", pz["wbc"], wv[:, 8:10, :], W["swa_k_norm_w"], W["swa_k_norm_w"][l:l + 1, :].unsqueeze(1).broadcast_to([128, 2, 64]))
    qk = [k.sbuf("s_qk%d" % i, [128, 640], BF16) for i in range(2)]
    kdup = [k.sbuf("s_kdup%d" % i, [128, 256], BF16) for i in range(2)]
    tps = [k.psum("s_tp%d" % i, [128, 4, 128], BF16) for i in range(2)]
    mprev = k.sbuf("s_mprev", [128, 128], BF16)
    mnext = k.sbuf("s_mnext", [128, 128], BF16)
    for t_, cm, pat in ((mprev, 1, -1), (mnext, -1, 1)):
        k.op("pool", [], [t_], lambda: nc.gpsimd.memset(t_[:, :], 1.0))
        k.op("pool", [t_], [t_], lambda: nc.gpsimd.affine_select(out=t_[:, :], in_=t_[:, :], pattern=[[pat, 128]],
                                                                 compare_op=ALU.is_ge, fill=0.0, base=0, channel_multiplier=cm))
    esink = _bc_load(k, "s_esink", W["swa_sink"], W["swa_sink"][l:l + 1, :], 8)
    k.op("act", [esink], [esink], lambda: nc.scalar.activation(out=esink[:, :], in_=esink[:, :], func=AF.Exp))
    for i in range(NT):
        o = qk[i % 2]
        kd = kdup[i % 2]
        _qk_prep(k, cx, W, pzs[i % 2], i, C_SQ, 10, o)
        k.dma("sp", vext, vext[:, i, :, 0:64], W["u"], W["u"][i * 128:(i + 1) * 128, C_SV:C_SV + 128].rearrange("p (h d) -> p h d", d=64))
        k.op("pool", [o], [kd], lambda: nc.gpsimd.tensor_copy(
            out=kd[:, :].rearrange("p (j c d) -> p j c d", j=2, c=2),
            in_=o[:, 512:640].rearrange("p (j d) -> p j d", j=2).unsqueeze(2).broadcast_to([128, 2, 2, 64])))
        if not (i < 2 and not update_ctx):
            tp = tps[0]
            for c in range(4):
                _tr(k, tp, tp[:, c, :], o, o[:, c * 128:(c + 1) * 128], cx.ident, cx.ident[:, :])
            _copy(k, "dve", qT, qT[:, :, i * 128:(i + 1) * 128], tp, tp[:, :, :])
        tp = tps[1]
        for j in range(2):
            _tr(k, tp, tp[:, j, :], kd, kd[:, j * 128:(j + 1) * 128], cx.ident, cx.ident[:, :])
        _copy(k, "act", kTd, kTd[:, :, i * 128:(i + 1) * 128], tp, tp[:, 0:2, :])
    pss = [k.psum("s_ps%d" % i, [128, 512], F32) for i in range(2)]
    accb = [k.psum("s_acc%d" % i, [128, 4, 65], F32) for i in range(2)]
    Et = [k.sbuf("s_E%d" % i, [128, 256], BF16) for i in range(3)]
    den = k.sbuf("s_den", [128, 8], F32)
    ob = [k.sbuf("s_ob%d" % i, [128, 512], BF16) for i in range(2)]
    blocks = []
    if update_ctx:
        for n in range(2):
            blocks.append((n, [(0, None), (1, None)]))
    for n in range(16):
        kts = [(0, None), (1, None)]
        for d_, mk in ((-1, mprev), (0, None), (1, mnext)):
            if 0 <= n + d_ < 16:
                kts.append((2 + n + d_, mk))
        blocks.append((2 + n, kts))
    ne = 0
    for bi, (qi, kts) in enumerate(blocks):
        q0 = qi * 128
        started = set()
        iters = [(j, par, ki, kt, mk) for j in range(2) for par in range(2) for ki, (kt, mk) in enumerate(kts)]
        slots = {}

        def emit_s(n_):
            j, par, ki, kt, mk = iters[n_]
            ps = pss[(ne + n_) % 2]
            E = Et[(ne + n_) % 3]
            slots[n_] = E
            _mm(k, ps, ps[:, 0:256].rearrange("p (c q) -> p c q", c=2), kTd, kTd[par * 64:(par + 1) * 64, j, kt * 128:(kt + 1) * 128],
                qT, qT[par * 64:(par + 1) * 64, 2 * j:2 * j + 2, q0:q0 + 128], True, True)
            k.op("act", [ps], [E], lambda: nc.scalar.activation(out=E[:, :], in_=ps[:, 0:256], func=AF.Exp, scale=0.125))
            if mk is not None:
                k.op("dve", [E, mk], [E], lambda: nc.vector.tensor_tensor(
                    out=E[:, :].rearrange("p (c q) -> p c q", c=2), in0=E[:, :].rearrange("p (c q) -> p c q", c=2),
                    in1=mk[:, :].unsqueeze(1).broadcast_to([128, 2, 128]), op=ALU.mult))

        def emit_pv(n_):
            j, par, ki, kt, mk = iters[n_]
            E = slots.pop(n_)
            for c2 in range(2):
                h = 4 * j + 2 * c2 + par
                ab = accb[h // 4]
                st = (h // 4) not in started
                started.add(h // 4)
                _mm(k, ab, ab[:, h % 4, :], E, E[:, c2 * 128:(c2 + 1) * 128], vext, vext[:, kt, j, :], st, ki == len(kts) - 1)

        for n_ in range(len(iters) + 1):
            if n_ < len(iters):
                emit_s(n_)
            if n_ >= 1:
                emit_pv(n_ - 1)
        ne += len(iters)
        o_b = ob[bi % 2]
        for hb in range(2):
            ab = accb[hb]
            k.op("dve", [ab, esink], [den], lambda: nc.vector.tensor_tensor(out=den[:, hb * 4:(hb + 1) * 4], in0=ab[:, :, 64],
                                                                           in1=esink[:, hb * 4:(hb + 1) * 4], op=ALU.add))
            k.op("dve", [den], [den], lambda: nc.vector.reciprocal(out=den[:, hb * 4:(hb + 1) * 4], in_=den[:, hb * 4:(hb + 1) * 4]))
            k.op("dve", [ab, den], [o_b], lambda: nc.vector.tensor_tensor(
                out=o_b[:, hb * 256:(hb + 1) * 256].rearrange("p (h d) -> p h d", d=64), in0=ab[:, :, 0:64],
                in1=den[:, hb * 4:(hb + 1) * 4].unsqueeze(2).broadcast_to([128, 4, 64]), op=ALU.mult))
        k.dma("sp", W["mix"], W["mix"][q0:q0 + 128, 512:1024], o_b, o_b[:, :])
    k.release(m0)


def stage_gla(k, cx, W, l, update_ctx):
    nc = k.nc
    v_ = nc.vector
    m0 = k.mark()
    QS = math.log(32.0 ** -0.5)
    wbd32 = k.sbuf("g_wbd32", [32, 512], F32)
    wbd = k.sbuf("g_wbd", [32, 512], BF16)
    k.op("dve", [], [wbd32], lambda: v_.memset(wbd32[:, :], 0.0))
    k.dma("sp", wbd32, wbd32[0:16, 0:256], W["gla_gate_w"], W["gla_gate_w"][l, 0, :, :])
    k.dma("sp", wbd32, wbd32[16:32, 256:512], W["gla_gate_w"], W["gla_gate_w"][l, 1, :, :])
    k.op("dve", [wbd32], [wbd], lambda: v_.tensor_copy(out=wbd[:, :], in_=wbd32[:, :]))
    gb = _bc_load(k, "g_gb", W["gla_gate_b"], W["gla_gate_b"][l:l + 1, :], 512)
    tri = [k.sbuf("g_tri%d" % d, [128, 128], BF16) for d in range(2)]
    cmask = [k.sbuf("g_cm%d" % d, [128, 128], BF16) for d in range(2)]
    for d in range(2):
        pat, cm = ((1, -1), (-1, 1))[d]
        for t_, val in ((tri[d], -1.0 / 16), (cmask[d], 1.0)):
            k.op("pool", [], [t_], lambda: nc.gpsimd.memset(t_[:, :], val))
            k.op("pool", [t_], [t_], lambda: nc.gpsimd.affine_select(out=t_[:, :], in_=t_[:, :], pattern=[[pat, 128]],
                                                                     compare_op=ALU.is_ge, fill=0.0, base=0, channel_multiplier=cm))
    negcol = k.sbuf("g_negcol", [128, 1], BF16)
    k.op("pool", [], [negcol], lambda: nc.gpsimd.memset(negcol[:, :], -1.0 / 16))
    mbd = k.sbuf("g_mbd", [128, 4, 128], BF16)
    k.op("pool", [], [mbd], lambda: nc.gpsimd.memset(mbd[:, :, :], 1.0))
    k.op("pool", [mbd], [mbd], lambda: nc.gpsimd.affine_select(out=mbd[:, :, :], in_=mbd[:, :, :], pattern=[[-32, 4], [0, 128]],
                                                               compare_op=ALU.is_ge, fill=0.0, base=0, channel_multiplier=1))
    k.op("pool", [mbd], [mbd], lambda: nc.gpsimd.affine_select(out=mbd[:, :, :], in_=mbd[:, :, :], pattern=[[32, 4], [0, 128]],
                                                               compare_op=ALU.is_ge, fill=0.0, base=31, channel_multiplier=-1))
    out_tiles = list(range(NT)) if update_ctx else list(range(2, NT))
    ostore = [{i: k.sbuf("g_o%d_%d" % (d, i), [128, 512], F32) for i in out_tiles} for d in range(2)]
    B = []
    for d in range(2):
        b = {}
        b["qk"] = k.sbuf("g_qk%d" % d, [128, 512], BF16)
        b["v"] = k.sbuf("g_v%d" % d, [128, 512], BF16)
        b["a"] = k.sbuf("g_a%d" % d, [128, 32], BF16)
        b["aT"] = k.sbuf("g_aT%d" % d, [32, 128], BF16)
        b["zb"] = k.sbuf("g_zb%d" % d, [128, 256], F32)
        b["sp"] = k.sbuf("g_sp%d" % d, [128, 256], F32)
        b["sph"] = k.sbuf("g_sph%d" % d, [128, 256], BF16)
        b["spl"] = k.sbuf("g_spl%d" % d, [128, 256], BF16)
        b["eb"] = k.sbuf("g_eb%d" % d, [128, 256], F32)
        b["enb"] = k.sbuf("g_enb%d" % d, [128, 256], F32)
        b["qkt"] = k.sbuf("g_qkt%d" % d, [128, 512], BF16)
        b["qkT"] = k.sbuf("g_qkT%d" % d, [128, 4, 128], BF16)
        b["ebl"] = k.sbuf("g_ebl%d" % d, [128, 2], F32)
        b["Qbd"] = [k.sbuf("g_Qbd%d_%d" % (d, h), [128, 4, 128], BF16) for h in range(2)]
        b["Em"] = [k.sbuf("g_Em%d_%d" % (d, h), [128, 4, 128], BF16) for h in range(2)]
        b["tmp"] = k.sbuf("g_tmp%d" % d, [128, 256], F32)
        b["S32"] = [k.sbuf("g_S32_%d_%d" % (d, h), [128, 256], F32) for h in range(2)]
        b["Sbf"] = [k.sbuf("g_Sbf_%d_%d" % (d, h), [128, 256], BF16) for h in range(2)]
        b["pT"] = k.psum("g_pT%d" % d, [128, 8, 128], BF16)
        b["pZ"] = k.psum("g_pZ%d" % d, [128, 512], F32)
        b["pA"] = k.psum("g_pA%d" % d, [128, 512], F32)
        b["pO"] = k.psum("g_pO%d" % d, [128, 512], F32)
        for h in range(2):
            k.op("dve", [], [b["S32"][h]], lambda: v_.memset(b["S32"][h][:, :], 0.0))
            k.op("dve", [], [b["Sbf"][h]], lambda: v_.memset(b["Sbf"][h][:, :], 0.0))
        B.append(b)
    order = [list(range(NT)), [1, 0] + list(range(NT - 1, 1, -1))]
    for d in range(2):
        B[d]["pre"] = [{"qk": B[d]["qk"], "v": B[d]["v"], "qkt": B[d]["qkt"], "qkT": B[d]["qkT"], "ebl": B[d]["ebl"]},
                       {"qk": k.sbuf("g_qkB%d" % d, [128, 512], BF16), "v": k.sbuf("g_vB%d" % d, [128, 512], BF16),
                        "qkt": k.sbuf("g_qktB%d" % d, [128, 512], BF16), "qkT": k.sbuf("g_qkTB%d" % d, [128, 4, 128], BF16),
                        "ebl": k.sbuf("g_eblB%d" % d, [128, 2], F32)}]

    def pre(step, d):
        i = order[d][step]
        b = B[d]
        pb = b["pre"][step % 2]
        dc = slice(d * 256, (d + 1) * 256)
        rows = slice(i * 128, (i + 1) * 128)
        k.dma("sp", pb["qk"], pb["qk"][:, :], W["u"], W["u"][rows, C_GQ:C_GQ + 512])
        k.dma("sp", pb["v"], pb["v"][:, :], W["u"], W["u"][rows, C_GV:C_GV + 512])
        k.dma("sp", b["a"], b["a"][:, :], W["u"], W["u"][rows, C_GA:C_GA + 32])
        pT, pZ = b["pT"], b["pZ"]
        _tr(k, pT, pT[0:32, 4, :], b["a"], b["a"][:, :], cx.ident, cx.ident[:, :])
        _copy(k, "dve", b["aT"], b["aT"][:, :], pT, pT[0:32, 4, :])
        _mm(k, pZ, pZ[:, 0:256], b["aT"], b["aT"][:, :], wbd, wbd[:, dc], True, True)
        k.op("dve", [pZ, gb], [b["zb"]], lambda: v_.tensor_tensor(out=b["zb"][:, :], in0=pZ[:, 0:256], in1=gb[:, dc], op=ALU.add))
        k.op("act", [b["zb"]], [b["sp"]], lambda: nc.scalar.activation(out=b["sp"][:, :], in_=b["zb"][:, :], func=AF.Exp, scale=-1.0))
        k.op("act", [b["sp"]], [b["sp"]], lambda: nc.scalar.activation(out=b["sp"][:, :], in_=b["sp"][:, :], func=AF.Ln, bias=1.0))
        k.op("act", [b["sp"]], [b["sph"]], lambda: nc.scalar.copy(out=b["sph"][:, :], in_=b["sp"][:, :]))
        k.op("dve", [b["sp"], b["sph"]], [b["spl"]], lambda: v_.tensor_tensor(out=b["spl"][:, :], in0=b["sp"][:, :], in1=b["sph"][:, :],
                                                                              op=ALU.subtract))
        _mm(k, pZ, pZ[:, 0:256], tri[d], tri[d][:, :], b["sph"], b["sph"][:, :], True, False)
        _mm(k, pZ, pZ[:, 0:256], tri[d], tri[d][:, :], b["spl"], b["spl"][:, :], False, True)
        for hg in range(2):
            _mm(k, pZ, pZ[:, 256 + hg:257 + hg], b["sph"], b["sph"][:, hg * 128:(hg + 1) * 128], negcol, negcol[:, :], False, False)
            _mm(k, pZ, pZ[:, 256 + hg:257 + hg], b["spl"], b["spl"][:, hg * 128:(hg + 1) * 128], negcol, negcol[:, :], False, True)
        k.op("act", [pZ], [b["eb"]], lambda: nc.scalar.activation(out=b["eb"][:, :], in_=pZ[:, 0:256], func=AF.Exp, bias=QS))
        k.op("act", [pZ], [b["enb"]], lambda: nc.scalar.activation(out=b["enb"][:, :], in_=pZ[:, 0:256], func=AF.Exp, scale=-1.0))
        k.op("act", [pZ], [pb["ebl"]], lambda: nc.scalar.activation(out=pb["ebl"][:, :], in_=pZ[:, 256:258], func=AF.Exp))
        k.op("dve", [pb["qk"], b["eb"]], [pb["qkt"]], lambda: v_.tensor_tensor(out=pb["qkt"][:, 0:256], in0=pb["qk"][:, 0:256],
                                                                               in1=b["eb"][:, :], op=ALU.mult))
        k.op("pool", [pb["qk"], b["enb"]], [pb["qkt"]], lambda: nc.gpsimd.tensor_tensor(out=pb["qkt"][:, 256:512], in0=pb["qk"][:, 256:512],
                                                                                        in1=b["enb"][:, :], op=ALU.mult))
        for c in range(4):
            _tr(k, pT, pT[:, c, :], pb["qkt"], pb["qkt"][:, c * 128:(c + 1) * 128], cx.ident, cx.ident[:, :])
        _copy(k, "act", pb["qkT"], pb["qkT"][:, :, :], pT, pT[:, 0:4, :])

    def main(step, d):
        i = order[d][step]
        b = B[d]
        pb = b["pre"][step % 2]
        pA, pO = b["pA"], b["pO"]
        qkT, qkt, vv, ebl = pb["qkT"], pb["qkt"], pb["v"], pb["ebl"]
        need_out = i in ostore[d]
        for hg in range(2):
            Qbd = b["Qbd"][hg]
            Em = b["Em"][hg]
            S32 = b["S32"][hg]
            Sbf = b["Sbf"][hg]
            if need_out:
                k.op("pool", [qkT, mbd], [Qbd], lambda: nc.gpsimd.tensor_tensor(
                    out=Qbd[:, :, :], in0=qkT[:, hg, :].unsqueeze(1).broadcast_to([128, 4, 128]), in1=mbd[:, :, :], op=ALU.mult))
                _mm(k, pA, pA[:, :], qkT, qkT[:, 2 + hg, :], Qbd, Qbd[:, :, :].rearrange("p h t -> p (h t)"), True, True)
                k.op("dve", [pA, cmask[d]], [Em], lambda: v_.tensor_tensor(
                    out=Em[:, :, :], in0=pA[:, :].rearrange("p (h t) -> p h t", h=4),
                    in1=cmask[d][:, :].unsqueeze(1).broadcast_to([128, 4, 128]), op=ALU.mult))
                _mm(k, pO, pO[:, 0:256], qkT, qkT[:, hg, :], Sbf, Sbf[:, :], True, False)
                for h4 in range(4):
                    hh = hg * 4 + h4
                    _mm(k, pO, pO[:, h4 * 64:(h4 + 1) * 64], Em, Em[:, h4, :], vv, vv[:, hh * 64:(hh + 1) * 64], False, h4 == 3)
                os_ = ostore[d][i]
                _copy(k, "act", os_, os_[:, hg * 256:(hg + 1) * 256], pO, pO[:, 0:256])
            _mm(k, pO, pO[:, 256:512], qkt, qkt[:, 256 + hg * 128:256 + (hg + 1) * 128], vv, vv[:, hg * 256:(hg + 1) * 256],
                not need_out, True)
            k.op("dve", [pO, mbd], [b["tmp"]], lambda: v_.tensor_tensor(
                out=b["tmp"][:, :].rearrange("p (h v) -> p h v", h=4), in0=pO[:, 256:512].rearrange("p (h v) -> p h v", h=4),
                in1=mbd[:, :, 0:64], op=ALU.mult))
            k.op("dve", [b["tmp"], S32], [S32], lambda: v_.tensor_tensor(out=S32[:, :], in0=b["tmp"][:, :], in1=S32[:, :], op=ALU.add))
            k.op("dve", [S32, ebl], [S32], lambda: v_.tensor_scalar(out=S32[:, :], in0=S32[:, :], scalar1=ebl[:, hg:hg + 1],
                                                                    scalar2=None, op0=ALU.mult))
            k.op("act", [S32], [Sbf], lambda: nc.scalar.copy(out=Sbf[:, :], in_=S32[:, :]))

    for step in range(NT + 1):
        for d in range(2):
            if step < NT:
                pre(step, d)
        for d in range(2):
            if step >= 1:
                main(step - 1, d)
    gw = k.sbuf("g_gw", [128, 8, 64], F32)
    k.dma("sp", gw, gw[:, :, :], W["gla_norm_w"], W["gla_norm_w"][l:l + 1, :].unsqueeze(1).broadcast_to([128, 8, 64]))
    gt = [k.sbuf("g_gt%d" % i, [128, 512], BF16) for i in range(2)]
    sg = [k.sbuf("g_sg%d" % i, [128, 512], F32) for i in range(2)]
    sq = k.sbuf("g_sq", [128, 512], F32)
    ss = k.sbuf("g_ss", [128, 8], F32)
    rs = k.sbuf("g_rs", [128, 8], F32)
    ob = [k.sbuf("g_ob%d" % i, [128, 512], BF16) for i in range(2)]
    for n, i in enumerate(out_tiles):
        g_ = gt[n % 2]
        s_ = sg[n % 2]
        o_ = ob[n % 2]
        of, obk = ostore[0][i], ostore[1][i]
        k.dma("sp", g_, g_[:, :], W["u"], W["u"][i * 128:(i + 1) * 128, C_GG:C_GG + 512])
        k.op("act", [g_], [s_], lambda: nc.scalar.activation(out=s_[:, :], in_=g_[:, :], func=AF.Silu))
        k.op("dve", [of, obk], [of], lambda: v_.tensor_tensor(out=of[:, :], in0=of[:, :], in1=obk[:, :], op=ALU.add))
        k.op("act", [of], [sq], lambda: nc.scalar.activation(out=sq[:, :], in_=of[:, :], func=AF.Square))
        k.op("dve", [sq], [ss], lambda: v_.tensor_reduce(out=ss[:, :], in_=sq[:, :].rearrange("p (h d) -> p h d", d=64), axis=AX.X, op=ALU.add))
        _rsqrt(k, rs, rs[:, :], ss, ss[:, :], EPS, scale=1.0 / 64)
        k.op("dve", [of, rs], [of], lambda: v_.tensor_tensor(out=of[:, :].rearrange("p (h d) -> p h d", d=64),
                                                             in0=of[:, :].rearrange("p (h d) -> p h d", d=64),
                                                             in1=rs[:, :].unsqueeze(2).broadcast_to([128, 8, 64]), op=ALU.mult))
        k.op("pool", [of, gw], [of], lambda: nc.gpsimd.tensor_tensor(out=of[:, :], in0=of[:, :], in1=gw[:, :, :].rearrange("p h d -> p (h d)"),
                                                                     op=ALU.mult))
        k.op("dve", [of, s_], [o_], lambda: v_.tensor_tensor(out=o_[:, :], in0=of[:, :], in1=s_[:, :], op=ALU.mult))
        k.dma("sp", W["mix"], W["mix"][i * 128:(i + 1) * 128, 0:512], o_, o_[:, :])
    k.release(m0)


def _split_hl(k, src, shape, name):
    nc = k.nc
    hi = k.sbuf(name + "h", shape, BF16)
    lo = k.sbuf(name + "l", shape, BF16)
    k.op("dve", [src], [hi], lambda: nc.vector.tensor_copy(out=hi[:, :], in_=src[:, :]))
    k.op("dve", [src, hi], [lo], lambda: nc.vector.tensor_tensor(out=lo[:, :], in0=src[:, :], in1=hi[:, :], op=ALU.subtract))
    return hi, lo


def _sin_reduced(k, out_b, out_ap, arg_b, arg_ap, kf_b, kf_ap):
    nc = k.nc
    MAGIC = 12582912.0
    k.op("dve", [arg_b], [kf_b], lambda: nc.vector.tensor_scalar(out=kf_ap, in0=arg_ap, scalar1=1.0 / (2 * math.pi), scalar2=MAGIC,
                                                                 op0=ALU.mult, op1=ALU.add))
    k.op("dve", [kf_b], [kf_b], lambda: nc.vector.tensor_scalar(out=kf_ap, in0=kf_ap, scalar1=-MAGIC, scalar2=None, op0=ALU.add))
    k.op("dve", [kf_b, arg_b], [arg_b], lambda: nc.vector.scalar_tensor_tensor(out=arg_ap, in0=kf_ap, scalar=-2 * math.pi, in1=arg_ap,
                                                                              op0=ALU.mult, op1=ALU.add))
    k.op("act", [arg_b], [out_b], lambda: nc.scalar.activation(out=out_ap, in_=arg_ap, func=AF.Sin, scale=0.999999))


def _hyena_seq(k, cx, W, l, L, tile0, sfx):
    nc = k.nc
    v_ = nc.vector
    nt = L // 128
    nb = nt + 1
    PI = math.pi
    tabC, tabS = W["dftc" + sfx], W["dfts" + sfx]
    m0 = k.mark()
    G = k.sbuf("h_G", [128, nb, 1024], BF16)
    Bt = k.sbuf("h_B", [128, nb, 1024], BF16)
    cb_ = [k.sbuf("h_cblk%d" % i, [128, nb, 128], BF16) for i in range(2)]
    sb_ = [k.sbuf("h_sblk%d" % i, [128, nb, 128], BF16) for i in range(2)]
    nld = [0]

    def load_blk(bc):
        c_t = cb_[nld[0] % 2]
        s_t = sb_[nld[0] % 2]
        nld[0] += 1
        k.dma("sp", c_t, c_t[:, :, :], tabC, tabC[bc, :, :, :])
        k.dma("sp", s_t, s_t[:, :, :], tabS, tabS[bc, :, :, :])
        return c_t, s_t

    m1 = k.mark()
    col = k.sbuf("h_col", [64, 8], F32)
    for a_ in range(2):
        k.dma("sp", col, col[:, a_:a_ + 1], W["hyena_ffn_freq"], W["hyena_ffn_freq"][l, a_, :].rearrange("(m o) -> m o", o=1))
    k.dma("sp", col, col[:, 2:3], W["hyena_ffn_b1"], W["hyena_ffn_b1"][l, :].rearrange("(m o) -> m o", o=1))
    k.dma("sp", col, col[:, 3:4], W["hyena_ffn_b2"], W["hyena_ffn_b2"][l, :].rearrange("(m o) -> m o", o=1))
    k.op("dve", [col], [col], lambda: v_.tensor_tensor(out=col[:, 4:6], in0=col[:, 0:2], in1=col[:, 2:4], op=ALU.mult))
    w1 = k.sbuf("h_w1", [64, 64], F32)
    k.op("dve", [], [w1], lambda: v_.memset(w1[:, :], 0.0))
    k.dma("sp", w1, w1[0:33, :], W["hyena_ffn_w1"], W["hyena_ffn_w1"][l, :, :])
    w1h, w1l = _split_hl(k, w1, [64, 64], "h_w1")
    w2 = k.sbuf("h_w2", [64, 64], F32)
    k.dma("sp", w2, w2[:, :], W["hyena_ffn_w2"], W["hyena_ffn_w2"][l, :, :])
    w2h, w2l = _split_hl(k, w2, [64, 64], "h_w2")
    w3b = k.sbuf("h_w3b", [64, 2048], BF16)
    k.dma("pool", w3b, w3b[:, :], W["hyena_ffn_w3"], W["hyena_ffn_w3"][l, :, :])
    h2b = k.sbuf("h_h2b", [64, L], BF16)
    psA = [k.psum("h_psA%d" % i, [128, 512], F32) for i in range(2)]
    m1a = k.mark()
    zh = k.sbuf("h_zh", [64, L], BF16)
    zl = k.sbuf("h_zl", [64, L], BF16)
    k.op("pool", [], [zh], lambda: nc.gpsimd.memset(zh[:, :], 0.0))
    k.op("pool", [], [zl], lambda: nc.gpsimd.memset(zl[:, :], 0.0))
    k.dma("sp", zh, zh[0:33, :], W["hy_zTh" + sfx], W["hy_zTh" + sfx][:, :])
    k.dma("sp", zl, zl[0:33, :], W["hy_zTl" + sfx], W["hy_zTl" + sfx][:, :])
    h1 = k.sbuf("h_h1", [64, L], F32)
    h2 = k.sbuf("h_h2", [64, L], F32)
    arg = k.sbuf("h_arg", [64, 512], F32)
    kf = k.sbuf("h_kf", [64, 512], F32)
    nblk = max(1, L // 512)
    bw = min(512, L)
    for blk in range(nblk):
        cs = slice(blk * bw, (blk + 1) * bw)
        ps = psA[blk % 2]
        for n_, (a_, b_) in enumerate(((w1h, zh), (w1h, zl), (w1l, zh))):
            _mm(k, ps, ps[0:64, 0:bw], a_, a_[:, :], b_, b_[:, cs], n_ == 0, n_ == 2)
        k.op("dve", [ps, col], [arg], lambda: v_.tensor_scalar(out=arg[:, 0:bw], in0=ps[0:64, 0:bw], scalar1=col[:, 0:1], scalar2=col[:, 4:5],
                                                               op0=ALU.mult, op1=ALU.add))
        _sin_reduced(k, h1, h1[:, cs], arg, arg[:, 0:bw], kf, kf[:, 0:bw])
    h1h, h1l = _split_hl(k, h1, [64, L], "h_h1")
    for blk in range(nblk):
        cs = slice(blk * bw, (blk + 1) * bw)
        ps = psA[blk % 2]
        for n_, (a_, b_) in enumerate(((w2h, h1h), (w2h, h1l), (w2l, h1h))):
            _mm(k, ps, ps[0:64, 0:bw], a_, a_[:, :], b_, b_[:, cs], n_ == 0, n_ == 2)
        k.op("dve", [ps, col], [arg], lambda: v_.tensor_scalar(out=arg[:, 0:bw], in0=ps[0:64, 0:bw], scalar1=col[:, 1:2], scalar2=col[:, 5:6],
                                                               op0=ALU.mult, op1=ALU.add))
        _sin_reduced(k, h2, h2[:, cs], arg, arg[:, 0:bw], kf, kf[:, 0:bw])
    k.op("dve", [h2], [h2b], lambda: v_.tensor_copy(out=h2b[:, :], in_=h2[:, :]))
    k.release(m1a)
    hp = k.sbuf("h_hp", [128, nt, 1024], BF16)
    hm = k.sbuf("h_hm", [128, nt, 1024], BF16)
    dec = [k.sbuf("h_dec%d" % i, [128, 512], F32) for i in range(2)]
    hd = [k.sbuf("h_hd%d" % i, [128, 512], F32) for i in range(4)]
    hab = [k.sbuf("h_hab%d" % i, [128, 512], BF16) for i in range(2)]
    ones_c = k.sbuf("h_ones_c", [128, 128], BF16)
    rowm = k.sbuf("h_rowm", [128, 1], F32)
    k.op("pool", [], [ones_c], lambda: nc.gpsimd.memset(ones_c[:, :], 1.0))
    k.op("pool", [], [rowm], lambda: nc.gpsimd.memset(rowm[:, :], 1.0))
    k.op("pool", [rowm], [rowm], lambda: nc.gpsimd.affine_select(out=rowm[:, :], in_=rowm[:, :], pattern=[[0, 1]], compare_op=ALU.is_ge,
                                                                 fill=0.0, base=-1, channel_multiplier=1))
    psN = [k.psum("h_psN%d" % i, [128, 512], F32) for i in range(4)]
    na = 0
    for tc in range(nt):
        dt_ = dec[tc % 2]
        k.dma("sp", dt_, dt_[:, :], W["hy_decay" + sfx], W["hy_decay" + sfx][tc * 128:(tc + 1) * 128, :])
        for cg in range(4):
            ps = psA[cg % 2]
            _mm(k, ps, ps[:, :], h2b, h2b[:, tc * 128:(tc + 1) * 128], w3b, w3b[:, cg * 512:(cg + 1) * 512], True, True)
            k.op("dve", [ps, dt_], [hd[cg]], lambda: v_.tensor_tensor(out=hd[cg][:, :], in0=ps[:, :], in1=dt_[:, :], op=ALU.mult))
            ab = hab[na % 2]
            na += 1
            k.op("act", [hd[cg]], [ab], lambda: nc.scalar.activation(out=ab[:, :], in_=hd[cg][:, :], func=AF.Abs))
            _mm(k, psN[cg], psN[cg][:, :], ones_c, ones_c[:, :], ab, ab[:, :], tc == 0, tc == nt - 1)
            if tc == 0 and cg % 2 == 1:
                k.op("dve", [hd[cg], rowm], [hd[cg]], lambda: v_.tensor_scalar(out=hd[cg][:, :], in0=hd[cg][:, :], scalar1=rowm[:, 0:1],
                                                                               scalar2=None, op0=ALU.mult))
        for o in range(2):
            k.op("pool", [hd[2 * o], hd[2 * o + 1]], [hp], lambda: nc.gpsimd.tensor_tensor(
                out=hp[:, tc, o * 512:(o + 1) * 512], in0=hd[2 * o][:, :], in1=hd[2 * o + 1][:, :], op=ALU.add))
            k.op("pool", [hd[2 * o], hd[2 * o + 1]], [hm], lambda: nc.gpsimd.tensor_tensor(
                out=hm[:, tc, o * 512:(o + 1) * 512], in0=hd[2 * o][:, :], in1=hd[2 * o + 1][:, :], op=ALU.subtract))
    nr = k.sbuf("h_nr", [128, 2048], F32)
    for cg in range(4):
        _copy(k, "dve", nr, nr[:, cg * 512:(cg + 1) * 512], psN[cg], psN[cg][:, :])
    rnb = k.sbuf("h_rnb", [128, 1024], F32)
    nrv = nr[:, :].rearrange("p (o d c) -> p o d c", o=2, d=2)
    k.op("dve", [nr], [rnb], lambda: v_.tensor_tensor(out=rnb[:, :].rearrange("p (o c) -> p o c", o=2), in0=nrv[:, :, 0, :], in1=nrv[:, :, 1, :],
                                                      op=ALU.add))
    k.op("dve", [rnb], [rnb], lambda: v_.reciprocal(out=rnb[:, :], in_=rnb[:, :]))
    wf = k.sbuf("h_wf", [128, nb], F32)
    k.dma("sp", wf, wf[:, :], W["hy_wf" + sfx], W["hy_wf" + sfx][:, :])
    npz = 0
    for fb in range(nb):
        c_t, s_t = load_blk(fb)
        for o in range(2):
            os_ = slice(o * 512, (o + 1) * 512)
            for (tab, src, dst) in ((c_t, hp, G), (s_t, hm, Bt)):
                ps = psN[npz % 4]
                npz += 1
                for tc in range(nt):
                    _mm(k, ps, ps[:, :], tab, tab[:, tc, :], src, src[:, tc, os_], tc == 0, tc == nt - 1)
                k.op("dve", [ps, wf, rnb], [dst], lambda: v_.scalar_tensor_tensor(out=dst[:, fb, os_], in0=ps[:, :], scalar=wf[:, fb:fb + 1],
                                                                                in1=rnb[:, os_], op0=ALU.mult, op1=ALU.mult))
    k.release(m1)
    import os as _os
    if _os.environ.get("HY_STOP") == "1":
        k.release(m0)
        return
    y = k.sbuf("h_y", [128, nt, 512], BF16)
    x1 = k.sbuf("h_x1", [128, nt, 512], BF16)
    x2 = k.sbuf("h_x2", [128, nt, 512], BF16)
    m2 = k.mark()
    cw = k.sbuf("h_cw", [128, 3, 1536], F32)
    for j in range(3):
        k.dma("sp", cw, cw[:, j, :], W["hyena_conv_w"], W["hyena_conv_w"][l, j:j + 1, :].broadcast_to([128, 1536]))
    cbias = _bc_load(k, "h_cbias", W["hyena_conv_b"], W["hyena_conv_b"][l:l + 1, :], 1536)
    ut = [[k.sbuf("h_u%d_%d" % (a, i), [128, 1536], BF16) for a in range(3)] for i in range(2)]
    za = k.sbuf("h_za", [128, 1536], F32)
    zb = k.sbuf("h_zb", [128, 1536], F32)
    for j in range(nt):
        um, uc, up = ut[j % 2]
        r0 = (tile0 + j) * 128
        hy = slice(C_HY, C_HY + 1536)
        if j == 0:
            k.op("pool", [], [um], lambda: nc.gpsimd.memset(um[:, :], 0.0))
            k.dma("sp", um, um[1:128, :], W["u"], W["u"][r0:r0 + 127, hy])
        else:
            k.dma("sp", um, um[:, :], W["u"], W["u"][r0 - 1:r0 + 127, hy])
        k.dma("sp", uc, uc[:, :], W["u"], W["u"][r0:r0 + 128, hy])
        if j == nt - 1:
            k.op("pool", [], [up], lambda: nc.gpsimd.memset(up[:, :], 0.0))
            k.dma("sp", up, up[0:127, :], W["u"], W["u"][r0 + 1:r0 + 128, hy])
        else:
            k.dma("sp", up, up[:, :], W["u"], W["u"][r0 + 1:r0 + 129, hy])
        k.op("dve", [um, cw], [za], lambda: v_.tensor_tensor(out=za[:, :], in0=um[:, :], in1=cw[:, 0, :], op=ALU.mult))
        k.op("pool", [uc, cw], [zb], lambda: nc.gpsimd.tensor_tensor(out=zb[:, :], in0=uc[:, :], in1=cw[:, 1, :], op=ALU.mult))
        k.op("dve", [za, zb], [za], lambda: v_.tensor_tensor(out=za[:, :], in0=za[:, :], in1=zb[:, :], op=ALU.add))
        k.op("pool", [up, cw], [zb], lambda: nc.gpsimd.tensor_tensor(out=zb[:, :], in0=up[:, :], in1=cw[:, 2, :], op=ALU.mult))
        k.op("pool", [zb, cbias], [zb], lambda: nc.gpsimd.tensor_tensor(out=zb[:, :], in0=zb[:, :], in1=cbias[:, :], op=ALU.add))
        for (dst, c0, eng) in ((x1, 0, "dve"), (x2, 512, "pool"), (y, 1024, "dve")):
            e_ = v_ if eng == "dve" else nc.gpsimd
            k.op(eng, [za, zb], [dst], lambda: e_.tensor_tensor(out=dst[:, j, :], in0=za[:, c0:c0 + 512], in1=zb[:, c0:c0 + 512], op=ALU.add))
    k.release(m2)
    ReY = k.sbuf("h_ReY", [128, nb, 512], BF16)
    ImY = k.sbuf("h_ImY", [128, nb, 512], BF16)
    hbias = k.sbuf("h_hbias", [128, 2, 512], F32)
    for o in range(2):
        k.dma("sp", hbias, hbias[:, o, :], W["hyena_bias"], W["hyena_bias"][l, o:o + 1, :].broadcast_to([128, 512]))
    tq = [k.sbuf("h_tq%d" % i, [128, 512], F32) for i in range(4)]
    ob = [k.sbuf("h_ob%d" % i, [128, 512], BF16) for i in range(2)]
    psP = [k.psum("h_psP%d" % i, [128, 512], F32) for i in range(2)]
    psQ = [k.psum("h_psQ%d" % i, [128, 512], F32) for i in range(2)]
    psO = [k.psum("h_psO%d" % i, [128, 512], F32) for i in range(2)]
    for o in range(2):
        os_ = slice(o * 512, (o + 1) * 512)
        xg = x1 if o == 0 else x2
        for fb in range(nb):
            c_t, s_t = load_blk(fb)
            pP, pQ = psP[fb % 2], psQ[fb % 2]
            for tc in range(nt):
                _mm(k, pP, pP[:, :], c_t, c_t[:, tc, :], y, y[:, tc, :], tc == 0, tc == nt - 1)
            for tc in range(nt):
                _mm(k, pQ, pQ[:, :], s_t, s_t[:, tc, :], y, y[:, tc, :], tc == 0, tc == nt - 1)
            k.op("dve", [pP, G], [tq[0]], lambda: v_.tensor_tensor(out=tq[0][:, :], in0=pP[:, :], in1=G[:, fb, os_], op=ALU.mult))
            k.op("dve", [pQ, Bt], [tq[1]], lambda: v_.tensor_tensor(out=tq[1][:, :], in0=pQ[:, :], in1=Bt[:, fb, os_], op=ALU.mult))
            k.op("pool", [tq[0], tq[1]], [ReY], lambda: nc.gpsimd.tensor_tensor(out=ReY[:, fb, :], in0=tq[0][:, :], in1=tq[1][:, :], op=ALU.subtract))
            k.op("dve", [pP, Bt], [tq[2]], lambda: v_.tensor_tensor(out=tq[2][:, :], in0=pP[:, :], in1=Bt[:, fb, os_], op=ALU.mult))
            k.op("dve", [pQ, G], [tq[3]], lambda: v_.tensor_tensor(out=tq[3][:, :], in0=pQ[:, :], in1=G[:, fb, os_], op=ALU.mult))
            k.op("pool", [tq[2], tq[3]], [ImY], lambda: nc.gpsimd.tensor_tensor(out=ImY[:, fb, :], in0=tq[2][:, :], in1=tq[3][:, :], op=ALU.add))
        for tc in range(nt):
            c_t, s_t = load_blk(tc)
            pO = psO[tc % 2]
            n_ = 0
            for fb in range(nb):
                for (tab, src) in ((c_t, ReY), (s_t, ImY)):
                    _mm(k, pO, pO[:, :], tab, tab[:, fb, :], src, src[:, fb, :], n_ == 0, n_ == 2 * nb - 1)
                    n_ += 1
            t0_, t1_ = tq[0], tq[1]
            k.op("pool", [y, hbias], [t0_], lambda: nc.gpsimd.tensor_tensor(out=t0_[:, :], in0=y[:, tc, :], in1=hbias[:, o, :], op=ALU.mult))
            k.op("dve", [pO, t0_], [t1_], lambda: v_.tensor_tensor(out=t1_[:, :], in0=pO[:, :], in1=t0_[:, :], op=ALU.add))
            if o == 0:
                k.op("pool", [xg, t1_], [y], lambda: nc.gpsimd.tensor_tensor(out=y[:, tc, :], in0=xg[:, tc, :], in1=t1_[:, :], op=ALU.mult))
            else:
                o_b = ob[tc % 2]
                k.op("pool", [xg, t1_], [o_b], lambda: nc.gpsimd.tensor_tensor(out=o_b[:, :], in0=xg[:, tc, :], in1=t1_[:, :], op=ALU.mult))
                r0 = (tile0 + tc) * 128
                k.dma("sp", W["mix"], W["mix"][r0:r0 + 128, 1024:1536], o_b, o_b[:, :])
    k.release(m0)


def stage_hyena(k, cx, W, l, update_ctx):
    if update_ctx:
        _hyena_seq(k, cx, W, l, LC, 0, "_s")
    _hyena_seq(k, cx, W, l, S, 2, "")


def build_full():
    nc, k, cx, W = new_program()
    stage_mod(k, cx, W)
    for l in range(DEPTH):
        upd = l < DEPTH - 1
        if l == 0:
            def xsrc(i):
                if i < 2:
                    return W["ctx"], W["ctx"][i * 128:(i + 1) * 128, :]
                return W["x"], W["x"][(i - 2) * 128:(i - 1) * 128, :]
        else:
            def xsrc(i):
                return W["xres"], W["xres"][i * 128:(i + 1) * 128, :]
        stage_inproj(k, cx, W, l, xsrc)
        stage_gla(k, cx, W, l, upd)
        stage_swa(k, cx, W, l, upd)
        stage_hyena(k, cx, W, l, upd)
        stage_diff(k, cx, W, l, upd)
        if upd:
            tiles = list(range(NT))
            passes = [list(range(0, 9)), list(range(9, 18))]
            dst = lambda i: (W["xres"], W["xres"][i * 128:(i + 1) * 128, :])
        else:
            tiles = list(range(2, NT))
            passes = [list(range(2, 10)), list(range(10, 18))]
            dst = lambda i: (W["out"], W["out"][(i - 2) * 128:(i - 1) * 128, :])
        stage_outproj_norm_router(k, cx, W, l, xsrc, tiles)
        for tl in passes:
            stage_experts(k, cx, W, l, tl, dst)
    end_program(k, W)
    return nc, k, W


_CACHE = {}


def kernel(**inputs):
    if "prog" not in _CACHE:
        _CACHE["prog"] = build_full()
        _CACHE["hc"] = host_consts()
    nc, k, W = _CACHE["prog"]
    hc = _CACHE["hc"]
    f32 = lambda a: np.ascontiguousarray(np.asarray(a, dtype=np.float32))
    shared = {}
    for n in W.inputs:
        if n in ("x", "ctx", "cvec"):
            continue
        if n in hc:
            shared[n] = hc[n]
        elif n == "router_bias":
            shared[n] = f32(inputs[n]).reshape(1, NE)
        elif n == "gla_gate_b":
            shared[n] = f32(inputs[n]).reshape(DEPTH, 512)
        elif n == "diff_lambda":
            shared[n] = f32(inputs[n]).reshape(DEPTH, 256)
        else:
            shared[n] = f32(inputs[n])
    x = f32(inputs["x"])
    ctx = f32(inputs["ctx"])
    c = f32(inputs["c"])
    c_ctx = f32(inputs["c_ctx"])
    nb = x.shape[0]
    in_maps = []
    for b in range(nb):
        m = dict(shared)
        m["x"] = x[b]
        m["ctx"] = ctx[b]
        m["cvec"] = np.ascontiguousarray(np.stack([c[b], c_ctx]))
        in_maps.append(m)
    res = run_bass_kernel_spmd(nc, in_maps, core_ids=list(range(nb)))
    return np.stack([np.asarray(r["out"], dtype=np.float32) for r in res.results])
```
